# Optimizing a Trainium2 kernel written in Bass

```python
import math
import jax, jax.numpy as jnp
from jax import lax
import numpy as np

D_MODEL = 1024
BATCH = 8
SEQ = 4096
DEPTH = 2

HEAD_DIM = 64
A_Q_HEADS = 8
A_KV_HEADS = 2
A_GROUP = A_Q_HEADS // A_KV_HEADS
WINDOW = 128
BLOCK = 128
NUM_BUCKETS = 32
MAX_DISTANCE = 128
B_HEADS = 8
A_WIDTH = A_Q_HEADS * HEAD_DIM
A_KV_WIDTH = A_KV_HEADS * HEAD_DIM
B_WIDTH = B_HEADS * HEAD_DIM
DECAY_LORA = 64
AAA_LORA = 64
GATE_LORA = 128
B_IN_WIDTH = 3 * B_WIDTH + DECAY_LORA + AAA_LORA + GATE_LORA
EVEN_IN_WIDTH = A_WIDTH + 2 * A_KV_WIDTH + B_IN_WIDTH
MIX_WIDTH = A_WIDTH + B_WIDTH
CONV_WIDTH = 3
CONV_DIM = D_MODEL
D_FF_DENSE = 2816
N_EXPERTS = 8
TOP_K = 2
D_FF_EXPERT = 3584
N_EVEN = (DEPTH + 1) // 2
N_ODD = DEPTH // 2
ALPHA = (2.0 * DEPTH) ** 0.25
BETA = (8.0 * DEPTH) ** -0.25
LN_EPS = 1e-5
GN_EPS = 64e-5
NEG_INF = -1e30
ATTN_SCALE = HEAD_DIM ** -0.5

kernel_name = "hybrid_swa_rwkv7_shortconv_moe_deepnorm"


def layer_norm(x, g, b):
    xf = x.astype(jnp.float32)
    mu = xf.mean(-1, keepdims=True)
    var = jnp.square(xf - mu).mean(-1, keepdims=True)
    y = (xf - mu) * lax.rsqrt(var + LN_EPS) * g.astype(jnp.float32) + b.astype(jnp.float32)
    return y.astype(x.dtype)


def token_shift(z):
    return jnp.pad(z, ((0, 0), (1, 0), (0, 0)))[:, :-1]


def t5_causal_bucket(dist):
    n = jnp.maximum(dist, 0)
    max_exact = NUM_BUCKETS // 2
    log_ratio = jnp.log(jnp.maximum(n, 1).astype(jnp.float32) / max_exact) / math.log(MAX_DISTANCE / max_exact)
    large = max_exact + (log_ratio * (NUM_BUCKETS - max_exact)).astype(jnp.int32)
    large = jnp.minimum(large, NUM_BUCKETS - 1)
    return jnp.where(n < max_exact, n, large)


def sliding_window_sink_attention(q, k, v, sinks, rel_bias_table):
    bsz, seq, _ = q.shape
    nb = seq // BLOCK
    qb = q.reshape(bsz, nb, BLOCK, A_KV_HEADS, A_GROUP, HEAD_DIM)
    kb = k.reshape(bsz, nb, BLOCK, A_KV_HEADS, HEAD_DIM)
    vb = v.reshape(bsz, nb, BLOCK, A_KV_HEADS, HEAD_DIM)
    pad = ((0, 0), (1, 0), (0, 0), (0, 0), (0, 0))
    kcat = jnp.concatenate([jnp.pad(kb, pad)[:, :-1], kb], axis=2)
    vcat = jnp.concatenate([jnp.pad(vb, pad)[:, :-1], vb], axis=2)
    qi = jnp.arange(BLOCK)[:, None]
    ki = jnp.arange(2 * BLOCK)[None, :]
    dist = qi + BLOCK - ki
    s_abs = jnp.arange(nb)[:, None, None] * BLOCK - BLOCK + ki[None]
    valid = (dist >= 0) & (dist < WINDOW) & (s_abs >= 0)
    bias = rel_bias_table[t5_causal_bucket(dist)].astype(jnp.float32)
    bias = jnp.transpose(bias, (2, 0, 1)).reshape(A_KV_HEADS, A_GROUP, BLOCK, 2 * BLOCK)
    scores = jnp.einsum('bnqhgd,bnshd->bnhgqs', qb, kcat).astype(jnp.float32) * ATTN_SCALE + bias
    scores = jnp.where(valid[None, :, None, None], scores, NEG_INF)
    sink = sinks.astype(jnp.float32).reshape(A_KV_HEADS, A_GROUP)[None, None, :, :, None]
    m = jnp.maximum(scores.max(-1), sink)
    p = jnp.exp(scores - m[..., None])
    denom = p.sum(-1) + jnp.exp(sink - m)
    p = p / denom[..., None]
    out = jnp.einsum('bnhgqs,bnshd->bnqhgd', p.astype(v.dtype), vcat)
    return out.reshape(bsz, seq, A_WIDTH)


def rwkv7_scan(r, w, k, v, a, b):
    bsz, _, nh, hd = r.shape
    xs = tuple(jnp.moveaxis(t, 1, 0) for t in (r, w, k, v, a, b))

    def step(state, inp):
        r_t, w_t, k_t, v_t, a_t, b_t = inp
        sa = jnp.einsum('bhij,bhj->bhi', state, a_t)
        state = state * w_t[:, :, None, :] + sa[..., None] * b_t[:, :, None, :] + v_t[..., None] * k_t[:, :, None, :]
        y = jnp.einsum('bhij,bhj->bhi', state, r_t)
        return state, y

    state0 = jnp.zeros((bsz, nh, hd, hd), jnp.float32)
    _, ys = lax.scan(step, state0, xs)
    return jnp.moveaxis(ys, 0, 1)


def rwkv7_time_mix(zb, mu, w0, w_up, a0, a_up, g_up, k_k, k_a, r_k, gn_g, gn_b):
    bsz, seq, _ = zb.shape
    f32 = jnp.float32
    zb = zb + (token_shift(zb) - zb) * mu
    splits = [B_WIDTH, 2 * B_WIDTH, 3 * B_WIDTH, 3 * B_WIDTH + DECAY_LORA, 3 * B_WIDTH + DECAY_LORA + AAA_LORA]
    zr, zk, zv, zwd, zad, zgd = jnp.split(zb, splits, axis=-1)
    w = -jax.nn.softplus(-(w0 + jnp.tanh(zwd) @ w_up).astype(f32)) - 0.5
    decay = jnp.exp(-jnp.exp(w))
    a = jax.nn.sigmoid((a0 + zad @ a_up).astype(f32))
    g = jax.nn.sigmoid(zgd) @ g_up

    def heads(t):
        return t.reshape(bsz, seq, B_HEADS, HEAD_DIM)

    zkf = zk.astype(f32)
    kk = heads(zkf * k_k.astype(f32))
    kk = kk / jnp.maximum(jnp.sqrt(jnp.sum(kk * kk, axis=-1, keepdims=True)), 1e-12)
    k = heads(zkf * (1.0 + (a - 1.0) * k_a.astype(f32)))
    r = heads(zr.astype(f32))
    v = heads(zv.astype(f32))
    a_h = heads(a)
    y = rwkv7_scan(r, heads(decay), k, v, -kk, kk * a_h)
    mean = y.mean(-1, keepdims=True)
    var = jnp.square(y - mean).mean(-1, keepdims=True)
    yn = ((y - mean) * lax.rsqrt(var + GN_EPS)).reshape(bsz, seq, B_WIDTH) * gn_g.astype(f32) + gn_b.astype(f32)
    bonus = (jnp.sum(r * k * r_k.astype(f32), axis=-1, keepdims=True) * v).reshape(bsz, seq, B_WIDTH)
    out = (yn + bonus) * g.astype(f32)
    return out.astype(zb.dtype)


def attention_rwkv_mixer(x, w_in, sinks, rel_bias_table, mu, w0, w_up, a0, a_up, g_up, k_k, k_a, r_k, gn_g, gn_b, w_out):
    z = x @ w_in
    q, k, v, zb = jnp.split(z, [A_WIDTH, A_WIDTH + A_KV_WIDTH, A_WIDTH + 2 * A_KV_WIDTH], axis=-1)
    ya = sliding_window_sink_attention(q, k, v, sinks, rel_bias_table)
    yb = rwkv7_time_mix(zb, mu, w0, w_up, a0, a_up, g_up, k_k, k_a, r_k, gn_g, gn_b)
    return jnp.concatenate([ya.astype(x.dtype), yb.astype(x.dtype)], axis=-1) @ w_out


def short_conv_mixer(x, w_in, conv_w, w_out):
    seq = x.shape[1]
    gb, gc, u = jnp.split(x @ w_in, 3, axis=-1)
    u = gc * u
    up = jnp.pad(u, ((0, 0), (CONV_WIDTH - 1, 0), (0, 0)))
    conv = up[:, 0:seq] * conv_w[0]
    for j in range(1, CONV_WIDTH):
        conv = conv + up[:, j:j + seq] * conv_w[j]
    return (gb * conv) @ w_out


def swiglu(x, w_gate, w_up, w_down):
    return (jax.nn.silu(x @ w_gate) * (x @ w_up)) @ w_down


def moe_swiglu(x, router_w, w_gate, w_up, w_down):
    bsz, seq, d = x.shape
    xt = x.reshape(-1, d)
    logits = (xt @ router_w).astype(jnp.float32)
    top_vals, top_idx = lax.top_k(logits, TOP_K)
    top_p = jax.nn.softmax(top_vals, axis=-1)
    combine = jnp.einsum('tk,tke->te', top_p, jax.nn.one_hot(top_idx, N_EXPERTS, dtype=jnp.float32))
    out = jnp.zeros_like(xt)
    for e in range(N_EXPERTS):
        y_e = swiglu(xt, w_gate[e], w_up[e], w_down[e])
        out = out + combine[:, e:e + 1].astype(x.dtype) * y_e
    return out.reshape(bsz, seq, d)


def setup_inputs(seed: int = 0) -> dict:
    key = jax.random.key(seed)
    ks = iter(jax.random.split(key, 48))

    def nrm(shape, scale):
        return jax.random.normal(next(ks), shape, jnp.float32) * scale

    def gain(shape):
        return 1.0 + nrm(shape, 0.05)

    def unif(shape, lo, hi):
        return jax.random.uniform(next(ks), shape, jnp.float32, lo, hi)

    D = D_MODEL
    return {
        "x": nrm((BATCH, SEQ, D), 1.0),
        "rel_bias_table": nrm((NUM_BUCKETS, A_Q_HEADS), 0.5),
        "even_w_in": nrm((N_EVEN, D, EVEN_IN_WIDTH), D ** -0.5),
        "even_sinks": nrm((N_EVEN, A_Q_HEADS), 0.5),
        "rwkv_mu": unif((N_EVEN, B_IN_WIDTH), 0.0, 1.0),
        "rwkv_w0": unif((N_EVEN, B_WIDTH), -6.0, 1.0),
        "rwkv_w_up": nrm((N_EVEN, DECAY_LORA, B_WIDTH), 0.5 * DECAY_LORA ** -0.5),
        "rwkv_a0": nrm((N_EVEN, B_WIDTH), 0.5),
        "rwkv_a_up": nrm((N_EVEN, AAA_LORA, B_WIDTH), 0.5 * AAA_LORA ** -0.5),
        "rwkv_g_up": nrm((N_EVEN, GATE_LORA, B_WIDTH), GATE_LORA ** -0.5),
        "rwkv_k_k": 0.85 + nrm((N_EVEN, B_WIDTH), 0.05),
        "rwkv_k_a": 1.0 + nrm((N_EVEN, B_WIDTH), 0.05),
        "rwkv_r_k": nrm((N_EVEN, B_HEADS, HEAD_DIM), 0.1),
        "rwkv_gn_g": gain((N_EVEN, B_WIDTH)),
        "rwkv_gn_b": nrm((N_EVEN, B_WIDTH), 0.02),
        "even_w_out": nrm((N_EVEN, MIX_WIDTH, D), BETA * MIX_WIDTH ** -0.5),
        "even_ln_mix_g": gain((N_EVEN, D)),
        "even_ln_mix_b": nrm((N_EVEN, D), 0.02),
        "dense_w_gate": nrm((N_EVEN, D, D_FF_DENSE), D ** -0.5),
        "dense_w_up": nrm((N_EVEN, D, D_FF_DENSE), D ** -0.5),
        "dense_w_down": nrm((N_EVEN, D_FF_DENSE, D), BETA * D_FF_DENSE ** -0.5),
        "even_ln_ffn_g": gain((N_EVEN, D)),
        "even_ln_ffn_b": nrm((N_EVEN, D), 0.02),
        "odd_w_in": nrm((N_ODD, D, 3 * CONV_DIM), D ** -0.5),
        "odd_conv_w": nrm((N_ODD, CONV_WIDTH, CONV_DIM), CONV_WIDTH ** -0.5),
        "odd_w_out": nrm((N_ODD, CONV_DIM, D), BETA * CONV_DIM ** -0.5),
        "odd_ln_mix_g": gain((N_ODD, D)),
        "odd_ln_mix_b": nrm((N_ODD, D), 0.02),
        "router_w": nrm((N_ODD, D, N_EXPERTS), D ** -0.5),
        "moe_w_gate": nrm((N_ODD, N_EXPERTS, D, D_FF_EXPERT), D ** -0.5),
        "moe_w_up": nrm((N_ODD, N_EXPERTS, D, D_FF_EXPERT), D ** -0.5),
        "moe_w_down": nrm((N_ODD, N_EXPERTS, D_FF_EXPERT, D), BETA * D_FF_EXPERT ** -0.5),
        "odd_ln_ffn_g": gain((N_ODD, D)),
        "odd_ln_ffn_b": nrm((N_ODD, D), 0.02),
    }


def reference(x, rel_bias_table, even_w_in, even_sinks, rwkv_mu, rwkv_w0, rwkv_w_up, rwkv_a0, rwkv_a_up, rwkv_g_up, rwkv_k_k, rwkv_k_a, rwkv_r_k, rwkv_gn_g, rwkv_gn_b, even_w_out, even_ln_mix_g, even_ln_mix_b, dense_w_gate, dense_w_up, dense_w_down, even_ln_ffn_g, even_ln_ffn_b, odd_w_in, odd_conv_w, odd_w_out, odd_ln_mix_g, odd_ln_mix_b, router_w, moe_w_gate, moe_w_up, moe_w_down, odd_ln_ffn_g, odd_ln_ffn_b):
    h = x
    for layer in range(DEPTH):
        i = layer // 2
        if layer % 2 == 0:
            mix = attention_rwkv_mixer(h, even_w_in[i], even_sinks[i], rel_bias_table, rwkv_mu[i], rwkv_w0[i], rwkv_w_up[i], rwkv_a0[i], rwkv_a_up[i], rwkv_g_up[i], rwkv_k_k[i], rwkv_k_a[i], rwkv_r_k[i], rwkv_gn_g[i], rwkv_gn_b[i], even_w_out[i])
            h = layer_norm(ALPHA * h + mix, even_ln_mix_g[i], even_ln_mix_b[i])
            ffn = swiglu(h, dense_w_gate[i], dense_w_up[i], dense_w_down[i])
            h = layer_norm(ALPHA * h + ffn, even_ln_ffn_g[i], even_ln_ffn_b[i])
        else:
            mix = short_conv_mixer(h, odd_w_in[i], odd_conv_w[i], odd_w_out[i])
            h = layer_norm(ALPHA * h + mix, odd_ln_mix_g[i], odd_ln_mix_b[i])
            ffn = moe_swiglu(h, router_w[i], moe_w_gate[i], moe_w_up[i], moe_w_down[i])
            h = layer_norm(ALPHA * h + ffn, odd_ln_ffn_g[i], odd_ln_ffn_b[i])
    return h
```

```python
import contextlib
import numpy as np
import concourse.bass as bass
import concourse.mybir as mybir
from concourse.bass_utils import run_bass_kernel_spmd

F32 = mybir.dt.float32
BF16 = mybir.dt.bfloat16
AF = mybir.ActivationFunctionType
ALU = mybir.AluOpType
AX = mybir.AxisListType

D = 1024
ALPHA = (2.0 * 2) ** 0.25
LN_EPS = 1e-5
GN_EPS = 64e-5
N_CORES = 8


def _dsize(dt):
    if dt == F32:
        return 4
    if dt == BF16:
        return 2
    s = str(dt)
    if "64" in s:
        return 8
    if "32" in s:
        return 4
    if "16" in s:
        return 2
    return 1


class Op:
    __slots__ = ("eng", "fn", "deps", "stream", "inc", "count", "isdma")

    def __init__(self, eng, fn, stream, isdma):
        self.eng = eng
        self.fn = fn
        self.deps = []
        self.stream = stream
        self.inc = False
        self.count = 0
        self.isdma = isdma


def region(ap):
    name = ap.name
    space = str(ap.space)
    off = int(ap.offset)
    pat = ap.ap
    esz = _dsize(ap.dtype)
    if "SB" in space or "PSUM" in space:
        shp = ap.tensor.shape
        rowsize = 1
        for s in list(shp)[1:]:
            rowsize *= int(s)
        p0 = off // rowsize
        f0 = off % rowsize
        npart = pat[0][1]
        f1 = f0 + 1
        for st, c in pat[1:]:
            if st > 0:
                f1 += (c - 1) * st
        if "PSUM" in space:
            b0 = (f0 * esz) // 2048
            b1 = (f1 * esz + 2047) // 2048
            return (name, 0, 128, b0 * 2048, b1 * 2048, True)
        return (name, p0, p0 + npart, f0 * esz, f1 * esz)
    lo = off
    hi = off + 1
    for st, c in pat:
        if st > 0:
            hi += (c - 1) * st
        elif st < 0:
            lo += (c - 1) * st
    return (name, 0, 1, lo * esz, hi * esz)


def _ovl(a, b):
    return a[1] < b[2] and b[1] < a[2] and a[3] < b[4] and b[3] < a[4]


def _contains(a, b):
    return a[1] <= b[1] and a[2] >= b[2] and a[3] <= b[3] and a[4] >= b[4]


class Sched:
    ENGS = ["pe", "act", "dve", "pool", "sp"]

    def __init__(self, nc, n_dma=32):
        self.nc = nc
        self.ops = {e: [] for e in self.ENGS}
        self.rec = {}
        self.n_dma = n_dma
        self.dma_last = [None] * n_dma
        self.dma_ops = [[] for _ in range(n_dma)]
        self.dma_rr = 0

    def _track(self, op, reads, writes):
        deps = op.deps
        st = op.stream
        rregs = [region(ap) for ap in reads]
        wregs = [region(ap) for ap in writes]
        for rg in rregs:
            lst = self.rec.get(rg[0])
            if lst:
                ps = len(rg) > 5
                for r in lst:
                    if (r[2] or (ps and r[1].stream != st)) and _ovl(r[0], rg):
                        p = r[1]
                        if p.isdma and p.stream == st:
                            continue
                        deps.append(p)
        for rg in wregs:
            lst = self.rec.get(rg[0])
            if lst:
                for r in lst:
                    if _ovl(r[0], rg):
                        p = r[1]
                        if p.stream == st:
                            continue
                        deps.append(p)
        for p in deps:
            p.inc = True
        for rg in rregs:
            lst = self.rec.setdefault(rg[0], [])
            lst[:] = [r for r in lst if not ((not r[2]) and r[1].stream == st and _contains(rg, r[0]))]
            lst.append([rg, op, False])
        for rg in wregs:
            lst = self.rec.setdefault(rg[0], [])
            lst[:] = [r for r in lst if not _contains(rg, r[0])]
            lst.append([rg, op, True])

    def add(self, eng, fn, reads=(), writes=()):
        op = Op(eng, fn, eng, False)
        self._track(op, reads, writes)
        self.ops[eng].append(op)
        return op

    def _pick_sem(self, eng):
        half = self.n_dma // 2
        if eng == "pool":
            self.dma_rr_sw = (getattr(self, "dma_rr_sw", -1) + 1) % half
            return half + self.dma_rr_sw
        self.dma_rr = (self.dma_rr + 1) % half
        return self.dma_rr

    def dma(self, eng, out, in_, sem=None, fn=None, reads=None, writes=None, **kw):
        if sem is None:
            sem = self._pick_sem(eng)
        stream = "dma%d" % sem
        if fn is None:
            fn = (lambda e, o=out, i=in_, k=kw: e.dma_start(out=o, in_=i, **k))
        op = Op(eng, fn, stream, True)
        op.inc = True
        prev = self.dma_last[sem]
        if prev is not None:
            op.deps.append(prev)
        self._track(op, [in_] if reads is None else reads, [out] if writes is None else writes)
        self.dma_last[sem] = op
        self.dma_ops[sem].append(op)
        self.ops[eng].append(op)
        return op

    def barrier(self):
        lasts = []
        for e in ["pe", "act", "dve", "pool"]:
            for op in reversed(self.ops[e]):
                if not op.isdma and op.fn is not None:
                    lasts.append(op)
                    break
        for s in range(self.n_dma):
            if self.dma_last[s] is not None:
                lasts.append(self.dma_last[s])
        for p in lasts:
            p.inc = True
        for e in self.ENGS:
            op = Op(e, None, e, False)
            op.deps = list(lasts)
            self.ops[e].append(op)
        self.rec = {}

    def emit(self):
        nc = self.nc
        for e in ["pe", "act", "dve", "pool"]:
            c = 0
            for op in self.ops[e]:
                if op.isdma or op.fn is None:
                    continue
                if op.inc:
                    c += 1
                op.count = c
        for s in range(self.n_dma):
            c = 0
            for op in self.dma_ops[s]:
                c += 16
                op.count = c
        with contextlib.ExitStack() as es:
            sems = {}
            for e in ["pe", "act", "dve", "pool"]:
                sems[e] = es.enter_context(nc.semaphore("s_" + e))
            for s in range(self.n_dma):
                if self.dma_ops[s]:
                    sems["dma%d" % s] = es.enter_context(nc.semaphore("s_dma%d" % s))
            block = es.enter_context(nc.Block())

            def run(engname):
                def body(engine):
                    known = {}
                    for op in self.ops[engname]:
                        need = {}
                        for p in op.deps:
                            v = p.count
                            if v > need.get(p.stream, 0):
                                need[p.stream] = v
                        for stn, v in need.items():
                            if v > known.get(stn, 0):
                                engine.wait_ge(sems[stn], v)
                                known[stn] = v
                        if op.fn is None:
                            continue
                        ins = op.fn(engine)
                        if op.inc:
                            ins.then_inc(sems[op.stream], 16 if op.isdma else 1)
                return body

            block.tensor(run("pe"))
            block.scalar(run("act"))
            block.vector(run("dve"))
            block.gpsimd(run("pool"))
            block.sync(run("sp"))


class Ctx:
    def __init__(self, nc, es, tag):
        self.nc = nc
        self.es = es
        self.tag = tag

    def sb(self, name, shape, dt):
        return self.es.enter_context(self.nc.sbuf_tensor(name + self.tag, list(shape), dt))

    def ps(self, name, shape, dt):
        return self.es.enter_context(self.nc.psum_tensor(name + self.tag, list(shape), dt))


def emit_ln(S, cx, pre, o, gb, bb, st, mv, sc, eps=LN_EPS):
    for c in range(2):
        S.add("dve", lambda e, c=c: e.bn_stats(st[:, c, :], pre[:, c * 512:(c + 1) * 512]),
              [pre[:, c * 512:(c + 1) * 512]], [st[:, c, :]])
    S.add("dve", lambda e: e.bn_aggr(mv[:], st[:]), [st[:]], [mv[:]])
    S.add("act", lambda e: e.activation(sc[:, 0:1], mv[:, 1:2], AF.Sqrt, bias=eps), [mv[:, 1:2]], [sc[:, 0:1]])
    S.add("dve", lambda e: e.reciprocal(sc[:, 0:1], sc[:, 0:1]), [sc[:, 0:1]], [sc[:, 0:1]])
    S.add("dve", lambda e: e.scalar_tensor_tensor(sc[:, 1:2], mv[:, 0:1], -1.0, sc[:, 0:1], ALU.mult, ALU.mult),
          [mv[:, 0:1], sc[:, 0:1]], [sc[:, 1:2]])
    S.add("act", lambda e: e.activation(o, pre, AF.Identity, bias=sc[:, 1:2], scale=sc[:, 0:1]),
          [pre, sc[:]], [o])
    S.add("dve", lambda e: e.tensor_tensor(o, o, gb[:], ALU.mult), [o, gb[:]], [o])
    S.add("pool", lambda e: e.tensor_tensor(o, o, bb[:], ALU.add), [o, bb[:]], [o])


def emit_ln_group(S, pairs, gb, bb, stg, mvg, scg, eps=LN_EPS):
    n = len(pairs)
    for k, (pre, o) in enumerate(pairs):
        for c in range(2):
            S.add("dve", lambda e, k=k, c=c, pre=pre: e.bn_stats(stg[:, k, c, :], pre[:, c * 512:(c + 1) * 512]),
                  [pre[:, c * 512:(c + 1) * 512]], [stg[:, k, c, :]])
    for k in range(n):
        S.add("dve", lambda e, k=k: e.bn_aggr(mvg[:, k, :], stg[:, k, :, :]), [stg[:, k, :, :]], [mvg[:, k, :]])
    S.add("act", lambda e: e.activation(scg[:, 0:n, 0], mvg[:, 0:n, 1], AF.Sqrt, bias=eps), [mvg[:, 0:n, :]], [scg[:, 0:n, 0]])
    S.add("dve", lambda e: e.reciprocal(scg[:, 0:n, 0], scg[:, 0:n, 0]), [scg[:, 0:n, 0]], [scg[:, 0:n, 0]])
    S.add("dve", lambda e: e.scalar_tensor_tensor(scg[:, 0:n, 1], mvg[:, 0:n, 0], -1.0, scg[:, 0:n, 0], ALU.mult, ALU.mult),
          [mvg[:, 0:n, :], scg[:, 0:n, 0]], [scg[:, 0:n, 1]])
    for k, (pre, o) in enumerate(pairs):
        S.add("act", lambda e, k=k, pre=pre, o=o: e.activation(o, pre, AF.Identity, bias=scg[:, k, 1:2], scale=scg[:, k, 0:1]),
              [pre, scg[:, k, :]], [o])
    for k, (pre, o) in enumerate(pairs):
        S.add("dve", lambda e, o=o: e.tensor_tensor(o, o, gb[:], ALU.mult), [o, gb[:]], [o])
    for k, (pre, o) in enumerate(pairs):
        S.add("dve", lambda e, o=o: e.tensor_tensor(o, o, bb[:], ALU.add), [o, bb[:]], [o])


def emit_load_transpose(S, hin_tile, ident, pbanks, xT_dst, xTf_dst=None):
    for b in range(2):
        pb = pbanks[b]
        for j in range(4):
            kc = b * 4 + j
            S.add("pe", lambda e, pb=pb, j=j, kc=kc: e.transpose(pb[:, j * 128:(j + 1) * 128],
                                                               hin_tile[:, kc * 128:(kc + 1) * 128], ident[:]),
                  [hin_tile[:, kc * 128:(kc + 1) * 128], ident[:]], [pb[:, j * 128:(j + 1) * 128]])
        src = pb[:, 0:512].rearrange("p (j t) -> p j t", j=4)
        dst = xT_dst[:, b * 4:(b + 1) * 4, :]
        S.add("act", lambda e, dst=dst, src=src: e.copy(dst, src), [pb[:, 0:512]], [dst])
        if xTf_dst is not None:
            dstf = xTf_dst[:, b * 4:(b + 1) * 4, :]
            S.add("dve", lambda e, dstf=dstf, src=src: e.tensor_copy(dstf, src), [pb[:, 0:512]], [dstf])


def ffn_phase(S, nc, T, hin, hout, wg, wu, wd, F, G, NE, rw, lng, lnb, ident_d, tag):
    HALF = min(T, 2048)
    NH = T // HALF
    TB = min(512, HALF)
    NB = HALF // TB
    NT = HALF // 128
    TPB = TB // 128
    GW = G * 128
    NG = F // GW
    assert NG * GW == F
    with contextlib.ExitStack() as es:
        cx = Ctx(nc, es, tag)
        ident = cx.sb("ident", [128, 128], F32)
        xT = cx.sb("xT", [128, 8, HALF], BF16)
        acc = cx.sb("acc", [128, NT, 1024], F32)
        wgb = [cx.sb("wg%d" % i, [128, 8, GW], BF16) for i in range(2)]
        wub = [cx.sb("wu%d" % i, [128, 8, GW], BF16) for i in range(2)]
        wdb = [cx.sb("wd%d" % i, [128, G, 1024], BF16) for i in range(2)]
        hinb = [cx.sb("hin%d" % i, [128, 1024], F32) for i in range(2)]
        sg = [cx.sb("sg%d" % i, [128, TB], F32) for i in range(2)]
        aT = [cx.sb("aT%d" % i, [128, G, TB], BF16) for i in range(2)]
        gb = cx.sb("gb", [128, 1024], F32)
        bb = cx.sb("bb", [128, 1024], F32)
        st = cx.sb("st", [128, 2, 6], F32)
        mv = cx.sb("mv", [128, 2], F32)
        sc = cx.sb("sc", [128, 2], F32)
        ob = [cx.sb("ob%d" % i, [128, 1024], F32) for i in range(4)]
        stg = cx.sb("stg", [128, 4, 2, 6], F32)
        mvg = cx.sb("mvg", [128, 4, 2], F32)
        scg = cx.sb("scg", [128, 4, 2], F32)
        pg = [cx.ps("pg%d" % i, [128, 512], F32) for i in range(2)]
        pu = [cx.ps("pu%d" % i, [128, 512], F32) for i in range(2)]
        pd = [cx.ps("pd%d" % i, [128, 1024], F32) for i in range(2)]
        if rw is not None:
            rwb = cx.sb("rwb", [128, 8, 8], F32)
            xTf = cx.sb("xTf", [128, 8, 128], F32)
            cw = cx.sb("cw", [128, NT, 8], F32)
            lg = cx.sb("lg", [128, 8], F32)
            lg2 = cx.sb("lg2", [128, 8], F32)
            eq1 = cx.sb("eq1", [128, 8], F32)
            eq2 = cx.sb("eq2", [128, 8], F32)
            sm = cx.sb("sm", [128, 8], F32)
            S.dma("sp", rwb[:], rw.rearrange("(k p) e -> p k e", p=128))
        S.dma("sp", ident[:], ident_d)
        S.dma("sp", gb[:], lng.to_broadcast([128, 1024]))
        S.dma("sp", bb[:], lnb.to_broadcast([128, 1024]))

        widx = 0
        for hf in range(NH):
            t0 = hf * HALF
            for tt in range(NT):
                hb = hinb[tt % 2]
                S.dma("sp", hb[:], hin[t0 + tt * 128:t0 + (tt + 1) * 128, :])
                emit_load_transpose(S, hb, ident, pg, xT[:, :, tt * 128:(tt + 1) * 128],
                                    xTf if rw is not None else None)
                a_t = acc[:, tt, :]
                S.add("act", lambda e, a_t=a_t, hb=hb: e.mul(a_t, hb[:], ALPHA), [hb[:]], [a_t])
                if rw is not None:
                    pl = pu[0][:, 0:8]
                    for kc in range(8):
                        S.add("pe", lambda e, kc=kc, pl=pl: e.matmul(pl, xTf[:, kc, :], rwb[:, kc, :],
                                                                   start=(kc == 0), stop=(kc == 7)),
                              [xTf[:, kc, :], rwb[:, kc, :]], [pl])
                    S.add("dve", lambda e, pl=pl: e.tensor_copy(lg[:], pl), [pl], [lg[:]])
                    S.add("dve", lambda e: e.tensor_reduce(sm[:, 0:1], lg[:], AX.X, ALU.max), [lg[:]], [sm[:, 0:1]])
                    S.add("dve", lambda e: e.tensor_scalar(eq1[:], lg[:], sm[:, 0:1], None, ALU.is_equal),
                          [lg[:], sm[:, 0:1]], [eq1[:]])
                    S.add("dve", lambda e: e.scalar_tensor_tensor(lg2[:], eq1[:], -1e30, lg[:], ALU.mult, ALU.add),
                          [eq1[:], lg[:]], [lg2[:]])
                    S.add("dve", lambda e: e.tensor_reduce(sm[:, 1:2], lg2[:], AX.X, ALU.max), [lg2[:]], [sm[:, 1:2]])
                    S.add("dve", lambda e: e.tensor_scalar(eq2[:], lg2[:], sm[:, 1:2], None, ALU.is_equal),
                          [lg2[:], sm[:, 1:2]], [eq2[:]])
                    S.add("dve", lambda e: e.tensor_tensor(sm[:, 2:3], sm[:, 1:2], sm[:, 0:1], ALU.subtract),
                          [sm[:, 0:2]], [sm[:, 2:3]])
                    S.add("act", lambda e: e.activation(sm[:, 3:4], sm[:, 2:3], AF.Exp), [sm[:, 2:3]], [sm[:, 3:4]])
                    S.add("dve", lambda e: e.tensor_scalar(sm[:, 4:5], sm[:, 3:4], 1.0, None, ALU.add),
                          [sm[:, 3:4]], [sm[:, 4:5]])
                    S.add("dve", lambda e: e.reciprocal(sm[:, 5:6], sm[:, 4:5]), [sm[:, 4:5]], [sm[:, 5:6]])
                    S.add("dve", lambda e: e.tensor_tensor(sm[:, 6:7], sm[:, 3:4], sm[:, 5:6], ALU.mult),
                          [sm[:, 3:4], sm[:, 5:6]], [sm[:, 6:7]])
                    cwt = cw[:, tt, :]
                    S.add("dve", lambda e, cwt=cwt: e.tensor_scalar(cwt, eq1[:], sm[:, 5:6], None, ALU.mult),
                          [eq1[:], sm[:, 5:6]], [cwt])
                    S.add("dve", lambda e, cwt=cwt: e.scalar_tensor_tensor(cwt, eq2[:], sm[:, 6:7], cwt, ALU.mult, ALU.add),
                          [eq2[:], sm[:, 6:7], cwt], [cwt])
            cnt = 0
            for ex in range(NE):
                for gi in range(NG):
                    slot = widx % 2
                    widx += 1
                    f0 = gi * GW
                    S.dma("pool", wgb[slot][:], wg[ex, :, f0:f0 + GW].rearrange("(k p) f -> p k f", p=128))
                    S.dma("pool", wub[slot][:], wu[ex, :, f0:f0 + GW].rearrange("(k p) f -> p k f", p=128))
                    S.dma("pool", wdb[slot][:], wd[ex, f0:f0 + GW, :].rearrange("(c p) d -> p c d", p=128))
                    for blk in range(NB):
                        c0 = blk * TB
                        ab = aT[cnt % 2]
                        for fc in range(G):
                            i2 = (cnt * G + fc) % 2
                            pgb, pub, sgb = pg[i2], pu[i2], sg[i2]
                            for kc in range(8):
                                S.add("pe", lambda e, pgb=pgb, kc=kc, fc=fc, slot=slot, c0=c0: e.matmul(
                                    pgb[:, 0:TB], wgb[slot][:, kc, fc * 128:(fc + 1) * 128], xT[:, kc, c0:c0 + TB],
                                    start=(kc == 0), stop=(kc == 7)),
                                    [wgb[slot][:, kc, fc * 128:(fc + 1) * 128], xT[:, kc, c0:c0 + TB]], [pgb[:, 0:TB]])
                            for kc in range(8):
                                S.add("pe", lambda e, pub=pub, kc=kc, fc=fc, slot=slot, c0=c0: e.matmul(
                                    pub[:, 0:TB], wub[slot][:, kc, fc * 128:(fc + 1) * 128], xT[:, kc, c0:c0 + TB],
                                    start=(kc == 0), stop=(kc == 7)),
                                    [wub[slot][:, kc, fc * 128:(fc + 1) * 128], xT[:, kc, c0:c0 + TB]], [pub[:, 0:TB]])
                            S.add("act", lambda e, sgb=sgb, pgb=pgb: e.activation(sgb[:], pgb[:, 0:TB], AF.Silu),
                                  [pgb[:, 0:TB]], [sgb[:]])
                            S.add("dve", lambda e, ab=ab, fc=fc, sgb=sgb, pub=pub: e.tensor_tensor(
                                ab[:, fc, :], sgb[:], pub[:, 0:TB], ALU.mult), [sgb[:], pub[:, 0:TB]], [ab[:, fc, :]])
                        for ti in range(TPB):
                            tt = blk * TPB + ti
                            pdb = pd[(cnt * TPB + ti) % 2]
                            for hd in range(2):
                                for fc in range(G):
                                    S.add("pe", lambda e, pdb=pdb, hd=hd, fc=fc, ab=ab, ti=ti, slot=slot: e.matmul(
                                        pdb[:, hd * 512:(hd + 1) * 512], ab[:, fc, ti * 128:(ti + 1) * 128],
                                        wdb[slot][:, fc, hd * 512:(hd + 1) * 512], start=(fc == 0), stop=(fc == G - 1)),
                                        [ab[:, fc, ti * 128:(ti + 1) * 128], wdb[slot][:, fc, hd * 512:(hd + 1) * 512]],
                                        [pdb[:, hd * 512:(hd + 1) * 512]])
                            a_t = acc[:, tt, :]
                            if rw is not None:
                                cws = cw[:, tt, ex:ex + 1]
                                S.add("dve", lambda e, a_t=a_t, pdb=pdb, cws=cws: e.scalar_tensor_tensor(
                                    a_t, pdb[:], cws, a_t, ALU.mult, ALU.add), [pdb[:], cws, a_t], [a_t])
                            else:
                                S.add("dve", lambda e, a_t=a_t, pdb=pdb: e.tensor_tensor(a_t, pdb[:], a_t, ALU.add),
                                      [pdb[:], a_t], [a_t])
                        cnt += 1
            for t4 in range(0, NT, 4):
                nn = min(4, NT - t4)
                emit_ln_group(S, [(acc[:, t4 + k, :], ob[k][:]) for k in range(nn)], gb, bb, stg, mvg, scg)
                for k in range(nn):
                    S.dma("sp", hout[t0 + (t4 + k) * 128:t0 + (t4 + k + 1) * 128, :], ob[k][:])
    S.barrier()


def conv_phase(S, nc, T, hin, hout, w_in, conv_w, w_out, lng, lnb, ident_d, tag):
    TB = min(512, T)
    NB = T // TB
    TPB = TB // 128
    with contextlib.ExitStack() as es:
        cx = Ctx(nc, es, tag)
        ident = cx.sb("ident", [128, 128], F32)
        win = cx.sb("win", [128, 8, 3072], BF16)
        wout = cx.sb("wout", [128, 8, 1024], BF16)
        cwt = cx.sb("cwt", [128, 8, 3], F32)
        xT = [cx.sb("xT%d" % i, [128, 8, TB], BF16) for i in range(2)]
        hinb = [cx.sb("hin%d" % i, [128, TPB, 1024], F32) for i in range(2)]
        up = [cx.sb("up%d" % i, [128, TB + 2], F32) for i in range(8)]
        gcs = [cx.sb("gcs%d" % i, [128, TB], F32) for i in range(2)]
        cv = [cx.sb("cv%d" % i, [128, TB], F32) for i in range(2)]
        vT = [cx.sb("vT%d" % i, [128, 8, TB], BF16) for i in range(2)]
        pre = [cx.sb("pre%d" % i, [128, 1024], F32) for i in range(4)]
        stg = cx.sb("stg", [128, 4, 2, 6], F32)
        mvg = cx.sb("mvg", [128, 4, 2], F32)
        scg = cx.sb("scg", [128, 4, 2], F32)
        gb = cx.sb("gb", [128, 1024], F32)
        bb = cx.sb("bb", [128, 1024], F32)
        st = cx.sb("st", [128, 2, 6], F32)
        mv = cx.sb("mv", [128, 2], F32)
        sc = cx.sb("sc", [128, 2], F32)
        ob = [cx.sb("ob%d" % i, [128, 1024], F32) for i in range(4)]
        pA = [cx.ps("pA%d" % i, [128, 512], F32) for i in range(2)]
        pB = [cx.ps("pB%d" % i, [128, 512], F32) for i in range(2)]
        pC = [cx.ps("pC%d" % i, [128, 512], F32) for i in range(2)]
        pO = cx.ps("pO", [128, 1024], F32)

        S.dma("sp", ident[:], ident_d)
        S.dma("sp", gb[:], lng.to_broadcast([128, 1024]))
        S.dma("sp", bb[:], lnb.to_broadcast([128, 1024]))
        S.dma("sp", cwt[:], conv_w)
        for j in range(3):
            S.dma("pool", win[:, :, j * 1024:(j + 1) * 1024],
                  w_in[:, j * 1024:(j + 1) * 1024].rearrange("(k p) f -> p k f", p=128))
        S.dma("pool", wout[:], w_out.rearrange("(k p) f -> p k f", p=128))
        for c in range(8):
            S.add("pool", lambda e, c=c: e.memset(up[c][:, 0:2], 0.0), [], [up[c][:, 0:2]])

        for blk in range(NB):
            t0 = blk * TB
            hb = hinb[blk % 2]
            xb = xT[blk % 2]
            vb = vT[blk % 2]
            for ti in range(TPB):
                S.dma("sp", hb[:, ti, :], hin[t0 + ti * 128:t0 + (ti + 1) * 128, :])
                emit_load_transpose(S, hb[:, ti, :], ident, pA, xb[:, :, ti * 128:(ti + 1) * 128])
            for c in range(8):
                i2 = c % 2
                for (pp, col0) in ((pA[i2], 0), (pB[i2], 1024), (pC[i2], 2048)):
                    for kc in range(8):
                        S.add("pe", lambda e, pp=pp, col0=col0, kc=kc, c=c, xb=xb: e.matmul(
                            pp[:, 0:TB], win[:, kc, col0 + c * 128:col0 + (c + 1) * 128], xb[:, kc, :],
                            start=(kc == 0), stop=(kc == 7)),
                            [win[:, kc, col0 + c * 128:col0 + (c + 1) * 128], xb[:, kc, :]], [pp[:, 0:TB]])
                g_s = gcs[i2]
                upc = up[c]
                cvb = cv[i2]
                if blk > 0:
                    S.add("pool", lambda e, upc=upc: e.tensor_copy(upc[:, 0:2], upc[:, TB:TB + 2]),
                          [upc[:, TB:TB + 2]], [upc[:, 0:2]])
                S.add("act", lambda e, g_s=g_s, i2=i2: e.copy(g_s[:], pB[i2][:, 0:TB]), [pB[i2][:, 0:TB]], [g_s[:]])
                S.add("dve", lambda e, upc=upc, g_s=g_s, i2=i2: e.tensor_tensor(upc[:, 2:TB + 2], g_s[:], pC[i2][:, 0:TB], ALU.mult),
                      [g_s[:], pC[i2][:, 0:TB]], [upc[:, 2:TB + 2]])
                S.add("act", lambda e, cvb=cvb, upc=upc, c=c: e.activation(cvb[:], upc[:, 0:TB], AF.Copy, scale=cwt[:, c, 0:1]),
                      [upc[:, 0:TB], cwt[:, c, 0:1]], [cvb[:]])
                S.add("dve", lambda e, cvb=cvb, upc=upc, c=c: e.scalar_tensor_tensor(
                    cvb[:], upc[:, 1:TB + 1], cwt[:, c, 1:2], cvb[:], ALU.mult, ALU.add),
                    [upc[:, 1:TB + 1], cwt[:, c, 1:2], cvb[:]], [cvb[:]])
                S.add("dve", lambda e, cvb=cvb, upc=upc, c=c: e.scalar_tensor_tensor(
                    cvb[:], upc[:, 2:TB + 2], cwt[:, c, 2:3], cvb[:], ALU.mult, ALU.add),
                    [upc[:, 2:TB + 2], cwt[:, c, 2:3], cvb[:]], [cvb[:]])
                S.add("dve", lambda e, cvb=cvb, vb=vb, c=c, i2=i2: e.tensor_tensor(vb[:, c, :], cvb[:], pA[i2][:, 0:TB], ALU.mult),
                      [cvb[:], pA[i2][:, 0:TB]], [vb[:, c, :]])
            for ti in range(TPB):
                for hd in range(2):
                    for kc in range(8):
                        S.add("pe", lambda e, hd=hd, kc=kc, ti=ti, vb=vb: e.matmul(
                            pO[:, hd * 512:(hd + 1) * 512], vb[:, kc, ti * 128:(ti + 1) * 128],
                            wout[:, kc, hd * 512:(hd + 1) * 512], start=(kc == 0), stop=(kc == 7)),
                            [vb[:, kc, ti * 128:(ti + 1) * 128], wout[:, kc, hd * 512:(hd + 1) * 512]],
                            [pO[:, hd * 512:(hd + 1) * 512]])
                pr = pre[ti % 4]
                S.add("dve", lambda e, pr=pr, hb=hb, ti=ti: e.scalar_tensor_tensor(
                    pr[:], hb[:, ti, :], ALPHA, pO[:], ALU.mult, ALU.add), [hb[:, ti, :], pO[:]], [pr[:]])
            emit_ln_group(S, [(pre[ti % 4][:], ob[ti % 4][:]) for ti in range(TPB)], gb, bb, stg, mvg, scg)
            for ti in range(TPB):
                S.dma("sp", hout[t0 + ti * 128:t0 + (ti + 1) * 128, :], ob[ti % 4][:])
    S.barrier()


WEIGHT_SPECS = {
    "dense_w_gate": [1, 1024, 2816], "dense_w_up": [1, 1024, 2816], "dense_w_down": [1, 2816, 1024],
    "even_ln_ffn_g": [1, 1024], "even_ln_ffn_b": [1, 1024],
    "odd_w_in": [1024, 3072], "odd_conv_w": [128, 8, 3], "odd_w_out": [1024, 1024],
    "odd_ln_mix_g": [1, 1024], "odd_ln_mix_b": [1, 1024],
    "router_w": [1024, 8],
    "moe_w_gate": [8, 1024, 3584], "moe_w_up": [8, 1024, 3584], "moe_w_down": [8, 3584, 1024],
    "odd_ln_ffn_g": [1, 1024], "odd_ln_ffn_b": [1, 1024],
    "c_ident": [128, 128],
    "even_w_in": [1024, 2560], "even_sinks": [1, 8], "c_bias": [128, 8, 256],
    "rwkv_w_up": [64, 512], "rwkv_a_up": [64, 512], "rwkv_g_up": [128, 512],
    "rwkv_gn_g": [1, 512], "rwkv_gn_b": [1, 512], "even_w_out": [1024, 1024],
    "even_ln_mix_g": [1, 1024], "even_ln_mix_b": [1, 1024],
    "moe_wg_l": [14336, 2048], "moe_wu_l": [14336, 2048], "moe_wd_l": [14336, 2048],
    "c_ltm": [128, 128], "c_slot512": [128, 32], "c_gp": [128, 14],
    "c_rwv": [64, 66], "c_mug": [128, 1], "c_msu": [64, 64], "c_miu": [64, 64], "c_msl": [64, 64],
}
PHASE_INPUTS = {
    "B2": ["dense_w_gate", "dense_w_up", "dense_w_down", "even_ln_ffn_g", "even_ln_ffn_b", "c_ident"],
    "E": ["router_w", "moe_wg_l", "moe_wu_l", "moe_wd_l", "odd_ln_ffn_g", "odd_ln_ffn_b", "c_ident", "c_ltm", "c_slot512", "c_gp"],
    "A1": ["even_w_in", "even_sinks", "c_bias", "c_ident"],
    "A": ["even_w_in", "even_sinks", "c_bias", "c_ident", "rwkv_w_up", "rwkv_a_up", "rwkv_g_up", "rwkv_gn_g", "rwkv_gn_b",
          "even_w_out", "even_ln_mix_g", "even_ln_mix_b", "c_rwv", "c_mug", "c_msu", "c_miu", "c_msl"],
    "B": ["dense_w_gate", "dense_w_up", "dense_w_down", "even_ln_ffn_g", "even_ln_ffn_b", "c_ident"],
    "C": ["odd_w_in", "odd_conv_w", "odd_w_out", "odd_ln_mix_g", "odd_ln_mix_b", "c_ident"],
    "D": ["router_w", "moe_w_gate", "moe_w_up", "moe_w_down", "odd_ln_ffn_g", "odd_ln_ffn_b", "c_ident"],
}


def build(T, phases):
    nc = bass.Bass("TRN2", target_bir_lowering=False)
    x = nc.dram_tensor("x", [T, D], F32, kind="ExternalInput").ap()
    ocols = 512 if phases == ["A1"] else D
    out = nc.dram_tensor("out", [T, ocols], F32, kind="ExternalOutput").ap()
    names = []
    for p in phases:
        for n in PHASE_INPUTS[p]:
            if n not in names:
                names.append(n)
    W = {n: nc.dram_tensor(n, WEIGHT_SPECS[n], F32, kind="ExternalInput").ap() for n in names}
    hs = [x]
    for i in range(len(phases) - 1):
        hs.append(nc.dram_tensor("hscr%d" % i, [T, D], F32).ap())
    hs.append(out)
    S = Sched(nc)
    for i, p in enumerate(phases):
        hi, ho = hs[i], hs[i + 1]
        if p == "A1":
            attn_phase(S, nc, T, hi, ho, W["even_w_in"], W["even_sinks"], W["c_bias"], W["c_ident"], "_A1")
        elif p == "A":
            ya_scr = nc.dram_tensor("ya_scr", [T, 512], F32).ap()
            attn_phase(S, nc, T, hi, ya_scr, W["even_w_in"], W["even_sinks"], W["c_bias"], W["c_ident"], "_A1")
            rwkv_phase(S, nc, T, hi, ya_scr, ho, W, "_A2")
        elif p == "B2":
            ffn_stream_phase(S, nc, T, hi, ho, W["dense_w_gate"], W["dense_w_up"], W["dense_w_down"], 2816,
                             W["even_ln_ffn_g"], W["even_ln_ffn_b"], W["c_ident"], "_B2")
        elif p == "B":
            ffn_phase(S, nc, T, hi, ho, W["dense_w_gate"], W["dense_w_up"], W["dense_w_down"], 2816, 2, 1, None,
                      W["even_ln_ffn_g"], W["even_ln_ffn_b"], W["c_ident"], "_B")
        elif p == "C":
            conv_phase(S, nc, T, hi, ho, W["odd_w_in"], W["odd_conv_w"], W["odd_w_out"],
                       W["odd_ln_mix_g"], W["odd_ln_mix_b"], W["c_ident"], "_C")
        elif p == "E":
            moe_sparse_phase(S, nc, T, hi, ho, W, "_E")
        elif p == "D":
            ffn_phase(S, nc, T, hi, ho, W["moe_w_gate"], W["moe_w_up"], W["moe_w_down"], 3584, 4, 8, W["router_w"],
                      W["odd_ln_ffn_g"], W["odd_ln_ffn_b"], W["c_ident"], "_D")
    S.emit()
    return nc, names


WANT_MOE_LAYOUT = [False]


def _t5_bucket(dist):
    n = np.maximum(dist, 0)
    max_exact = 16
    lr = np.log(np.maximum(n, 1).astype(np.float32) / np.float32(max_exact)) / np.float32(np.log(128 / 16))
    large = max_exact + (lr.astype(np.float32) * np.float32(32 - max_exact)).astype(np.int32)
    large = np.minimum(large, 31)
    return np.where(n < max_exact, n, large)


def host_consts(inputs):
    c = {"c_ident": np.eye(128, dtype=np.float32)}
    s_i = np.arange(64)[:, None]
    t_i = np.arange(64)[None, :]
    c["c_msu"] = (s_i < t_i).astype(np.float32)
    c["c_miu"] = (s_i <= t_i).astype(np.float32)
    c["c_msl"] = (s_i > t_i).astype(np.float32)
    pp_ = np.arange(128)
    c["c_ltm"] = (pp_[:, None] < pp_[None, :]).astype(np.float32)
    c["c_slot512"] = np.tile((np.arange(32) * 512).astype(np.float32)[None, :], (128, 1))
    c["c_gp"] = (np.arange(14)[None, :] * 128 + pp_[:, None]).astype(np.float32)
    if "moe_w_gate" in inputs and WANT_MOE_LAYOUT[0]:
        for src, dstn in (("moe_w_gate", "moe_wg_l"), ("moe_w_up", "moe_wu_l")):
            a = np.asarray(inputs[src], np.float32).reshape(8, 8, 128, 14, 256)
            c[dstn] = np.ascontiguousarray(a.transpose(0, 3, 2, 1, 4)).reshape(14336, 2048)
        a = np.asarray(inputs["moe_w_down"], np.float32).reshape(8, 14, 2, 128, 1024)
        c["moe_wd_l"] = np.ascontiguousarray(a.transpose(0, 1, 3, 2, 4)).reshape(14336, 2048)
    if "rel_bias_table" in inputs:
        tbl = np.asarray(inputs["rel_bias_table"], np.float32)
        qi = np.arange(128)[:, None]
        ki = np.arange(256)[None, :]
        dist = qi + 128 - ki
        valid = (dist >= 0) & (dist < 128)
        g = tbl[_t5_bucket(dist)]
        g = np.where(valid[:, :, None], g, np.float32(-1e30))
        c["c_bias"] = np.ascontiguousarray(np.transpose(g, (0, 2, 1))).astype(np.float32)
    if "rwkv_mu" in inputs:
        mu = np.asarray(inputs["rwkv_mu"], np.float32).reshape(-1)
        cols = [mu[0:1664].reshape(26, 64).T]
        for nm in ("rwkv_w0", "rwkv_a0", "rwkv_k_k", "rwkv_k_a", "rwkv_r_k"):
            cols.append(np.asarray(inputs[nm], np.float32).reshape(8, 64).T)
        c["c_rwv"] = np.ascontiguousarray(np.concatenate(cols, axis=1))
        c["c_mug"] = np.ascontiguousarray(mu[1664:1792].reshape(128, 1))
    return c


def prep_weights(inputs, names):
    WANT_MOE_LAYOUT[0] = "moe_wg_l" in names
    cons = host_consts(inputs)
    outw = {}
    for n in names:
        if n in cons:
            outw[n] = cons[n]
            continue
        a = np.asarray(inputs[n], dtype=np.float32)
        shp = WEIGHT_SPECS[n]
        if n == "odd_conv_w":
            a = a.reshape(3, 8, 128).transpose(2, 1, 0)
        outw[n] = np.ascontiguousarray(a.reshape(shp))
    return outw


def run_phases(xfull, inputs, phases, T):
    nc, names = build(T, phases)
    w = prep_weights(inputs, names)
    in_maps = []
    for c in range(xfull.shape[0]):
        m = {"x": np.ascontiguousarray(xfull[c])}
        m.update(w)
        in_maps.append(m)
    res = run_bass_kernel_spmd(nc, in_maps, core_ids=list(range(len(in_maps))))
    global LAST_RES
    LAST_RES = res.results
    return np.stack([np.asarray(r["out"]) for r in res.results], axis=0)


class Banks:
    def __init__(self, banks):
        self.banks = banks
        self.i = 0

    def nxt(self):
        b = self.banks[self.i % len(self.banks)]
        self.i += 1
        return b


def attn_phase(S, nc, T, xin_d, ya_d, w_in, sinks, bias_d, ident_d, tag):
    NTL = T // 128
    with contextlib.ExitStack() as es:
        cx = Ctx(nc, es, tag)
        ident = cx.sb("ident", [128, 128], F32)
        win = cx.sb("win", [128, 8, 768], BF16)
        biasb = cx.sb("biasb", [128, 8, 256], F32)
        sinkb = cx.sb("sinkb", [128, 8], F32)
        xinb = [cx.sb("xin%d" % i, [128, 1024], F32) for i in range(2)]
        xT = [cx.sb("xT%d" % i, [128, 8, 128], BF16) for i in range(2)]
        qT = [cx.sb("qT%d" % i, [64, 8, 128], BF16) for i in range(2)]
        kT = [cx.sb("kT%d" % i, [64, 2, 128], BF16) for i in range(2)]
        vt = [cx.sb("vt%d" % i, [128, 128], BF16) for i in range(2)]
        sc = [cx.sb("sc%d" % i, [128, 2, 256], F32) for i in range(2)]
        pp = [cx.sb("pp%d" % i, [128, 2, 256], F32) for i in range(2)]
        PT = [cx.sb("PT%d" % i, [128, 4, 128], BF16) for i in range(2)]
        mx = cx.sb("mx", [128, 8], F32)
        mm = cx.sb("mm", [128, 8], F32)
        negm = cx.sb("negm", [128, 8], F32)
        rs = cx.sb("rs", [128, 8], F32)
        esb = cx.sb("esb", [128, 8], F32)
        rden = [cx.sb("rden%d" % i, [128, 8], F32) for i in range(2)]
        ya = [cx.sb("ya%d" % i, [128, 512], F32) for i in range(2)]
        PB = Banks([cx.ps("ps%d" % i, [128, 512], F32) for i in range(6)])
        bo_banks = [cx.ps("pbo%d" % i, [128, 512], F32) for i in range(2)]

        S.dma("sp", ident[:], ident_d)
        S.dma("sp", biasb[:], bias_d)
        S.dma("sp", sinkb[:], sinks.to_broadcast([128, 8]))
        S.dma("pool", win[:], w_in[:, 0:768].rearrange("(k p) f -> p k f", p=128))
        for i in range(2):
            S.add("pool", lambda e, i=i: e.memset(pp[i][:], 0.0), [], [pp[i][:]])

        for n in range(NTL):
            p = n % 2
            xb = xinb[p]
            S.dma("sp", xb[:], xin_d[n * 128:(n + 1) * 128, :])
            emit_load_transpose(S, xb, ident, [PB.nxt(), PB.nxt()], xT[p][:])
            for b in range(2):
                bk = PB.nxt()
                for hh in range(4):
                    h = b * 4 + hh
                    for kc in range(8):
                        S.add("pe", lambda e, bk=bk, hh=hh, h=h, kc=kc, p=p: e.matmul(
                            bk[0:64, hh * 128:(hh + 1) * 128], win[:, kc, h * 64:(h + 1) * 64], xT[p][:, kc, :],
                            start=(kc == 0), stop=(kc == 7)),
                            [win[:, kc, h * 64:(h + 1) * 64], xT[p][:, kc, :]], [bk[0:64, hh * 128:(hh + 1) * 128]])
                dst = qT[p][:, b * 4:(b + 1) * 4, :]
                src = bk[0:64, :].rearrange("p (j t) -> p j t", j=4)
                S.add("act", lambda e, dst=dst, src=src: e.mul(dst, src, 0.125), [bk[0:64, :]], [dst])
            bk = PB.nxt()
            for g in range(2):
                for kc in range(8):
                    S.add("pe", lambda e, bk=bk, g=g, kc=kc, p=p: e.matmul(
                        bk[0:64, g * 128:(g + 1) * 128], win[:, kc, 512 + g * 64:512 + (g + 1) * 64], xT[p][:, kc, :],
                        start=(kc == 0), stop=(kc == 7)),
                        [win[:, kc, 512 + g * 64:512 + (g + 1) * 64], xT[p][:, kc, :]], [bk[0:64, g * 128:(g + 1) * 128]])
            src = bk[0:64, 0:256].rearrange("p (j t) -> p j t", j=2)
            S.add("act", lambda e, src=src, p=p: e.copy(kT[p][:], src), [bk[0:64, 0:256]], [kT[p][:]])
            bv = PB.nxt()
            for kc in range(8):
                S.add("pe", lambda e, bv=bv, kc=kc, p=p: e.matmul(
                    bv[:, 0:128], xT[p][:, kc, :], win[:, kc, 640:768], start=(kc == 0), stop=(kc == 7)),
                    [xT[p][:, kc, :], win[:, kc, 640:768]], [bv[:, 0:128]])
            S.add("dve", lambda e, bv=bv, p=p: e.tensor_copy(vt[p][:], bv[:, 0:128]), [bv[:, 0:128]], [vt[p][:]])

            import os
            DBG = int(os.environ.get("DBG_STOP", "9"))
            DBGN = int(os.environ.get("DBGN", "0"))
            if DBG <= 1:
                continue
            bo = bo_banks[p]
            k0 = 0 if n > 0 else 128
            for i in range(4):
                g = i // 2
                i2 = i % 2
                bs = PB.nxt()
                for hh in range(2):
                    h = 2 * i + hh
                    if n > 0:
                        S.add("pe", lambda e, bs=bs, hh=hh, h=h, g=g, p=p: e.matmul(
                            bs[:, hh * 256:hh * 256 + 128], qT[p][:, h, :], kT[1 - p][:, g, :], start=True, stop=True),
                            [qT[p][:, h, :], kT[1 - p][:, g, :]], [bs[:, hh * 256:hh * 256 + 128]])
                    S.add("pe", lambda e, bs=bs, hh=hh, h=h, g=g, p=p: e.matmul(
                        bs[:, hh * 256 + 128:hh * 256 + 256], qT[p][:, h, :], kT[p][:, g, :], start=True, stop=True),
                        [qT[p][:, h, :], kT[p][:, g, :]], [bs[:, hh * 256 + 128:hh * 256 + 256]])
                bsv = bs[:, :].rearrange("p (j t) -> p j t", j=2)[:, :, k0:256]
                scv = sc[i2][:, :, k0:256]
                ppv = pp[i2][:, :, k0:256]
                S.add("dve", lambda e, scv=scv, bsv=bsv, i=i, k0=k0: e.tensor_tensor(scv, bsv, biasb[:, 2 * i:2 * i + 2, k0:256], ALU.add),
                      [bs[:, :], biasb[:, 2 * i:2 * i + 2, :]], [sc[i2][:]])
                S.add("dve", lambda e, scv=scv, i=i: e.tensor_reduce(mx[:, 2 * i:2 * i + 2], scv, AX.X, ALU.max),
                      [sc[i2][:]], [mx[:, 2 * i:2 * i + 2]])
                S.add("dve", lambda e, i=i: e.tensor_tensor(mm[:, 2 * i:2 * i + 2], mx[:, 2 * i:2 * i + 2], sinkb[:, 2 * i:2 * i + 2], ALU.max),
                      [mx[:, 2 * i:2 * i + 2], sinkb[:, 2 * i:2 * i + 2]], [mm[:, 2 * i:2 * i + 2]])
                S.add("dve", lambda e, i=i: e.tensor_scalar(negm[:, 2 * i:2 * i + 2], mm[:, 2 * i:2 * i + 2], -1.0, None, ALU.mult),
                      [mm[:, 2 * i:2 * i + 2]], [negm[:, 2 * i:2 * i + 2]])
                if DBG <= 2:
                    continue
                for hh in range(2):
                    h = 2 * i + hh
                    S.add("act", lambda e, hh=hh, h=h, i2=i2, k0=k0: e.activation(
                        pp[i2][:, hh, k0:256], sc[i2][:, hh, k0:256], AF.Exp, bias=negm[:, h:h + 1], accum_out=rs[:, h:h + 1]),
                        [sc[i2][:, hh, :], negm[:, h:h + 1]], [pp[i2][:, hh, :], rs[:, h:h + 1]])
                S.add("dve", lambda e, i=i: e.tensor_tensor(esb[:, 2 * i:2 * i + 2], sinkb[:, 2 * i:2 * i + 2], mm[:, 2 * i:2 * i + 2], ALU.subtract),
                      [sinkb[:, 2 * i:2 * i + 2], mm[:, 2 * i:2 * i + 2]], [esb[:, 2 * i:2 * i + 2]])
                S.add("act", lambda e, i=i: e.activation(esb[:, 2 * i:2 * i + 2], esb[:, 2 * i:2 * i + 2], AF.Exp),
                      [esb[:, 2 * i:2 * i + 2]], [esb[:, 2 * i:2 * i + 2]])
                S.add("dve", lambda e, i=i: e.tensor_tensor(esb[:, 2 * i:2 * i + 2], esb[:, 2 * i:2 * i + 2], rs[:, 2 * i:2 * i + 2], ALU.add),
                      [esb[:, 2 * i:2 * i + 2], rs[:, 2 * i:2 * i + 2]], [esb[:, 2 * i:2 * i + 2]])
                S.add("dve", lambda e, i=i, p=p: e.reciprocal(rden[p][:, 2 * i:2 * i + 2], esb[:, 2 * i:2 * i + 2]),
                      [esb[:, 2 * i:2 * i + 2]], [rden[p][:, 2 * i:2 * i + 2]])
                if DBG <= 3:
                    continue
                bt = PB.nxt()
                kcs = (0, 1) if n > 0 else (1,)
                E3 = True
                for hh in range(2):
                    for kc in ((0, 1) if E3 else kcs):
                        j = hh * 2 + kc
                        S.add("pe", lambda e, bt=bt, j=j, hh=hh, kc=kc, i2=i2: e.transpose(
                            bt[:, j * 128:(j + 1) * 128], pp[i2][:, hh, kc * 128:(kc + 1) * 128], ident[:]),
                            [pp[i2][:, hh, kc * 128:(kc + 1) * 128], ident[:]], [bt[:, j * 128:(j + 1) * 128]])
                if DBG == 41:
                    continue
                if n > 0 or E3:
                    S.add("act", lambda e, bt=bt, i2=i2: e.copy(PT[i2][:], bt[:, :].rearrange("p (j t) -> p j t", j=4)),
                          [bt[:, :]], [PT[i2][:]])
                else:
                    for hh in range(2):
                        j = hh * 2 + 1
                        S.add("dve", lambda e, bt=bt, i2=i2, j=j: e.tensor_copy(PT[i2][:, j, :], bt[:, j * 128:(j + 1) * 128]),
                              [bt[:, j * 128:(j + 1) * 128]], [PT[i2][:, j, :]])
                if DBG <= 4 or DBG == 41:
                    continue
                if os.environ.get("DBG_DUMP") and n == DBGN and i == 0:
                    dd = lambda nm, shp, dt=F32: nc.dram_tensor("dbg_" + nm, shp, dt, kind="ExternalOutput").ap()
                    S.dma("sp", dd("sc", [128, 2, 256]), sc[i2][:])
                    S.dma("sp", dd("pp", [128, 2, 256]), pp[i2][:])
                    S.dma("sp", dd("PT", [128, 4, 128], BF16), PT[i2][:])
                    S.dma("sp", dd("qT", [64, 8, 128], BF16), qT[p][:])
                    S.dma("sp", dd("kT", [64, 2, 128], BF16), kT[p][:])
                    S.dma("sp", dd("vt", [128, 128], BF16), vt[p][:])
                    S.dma("sp", dd("rden", [128, 8]), rden[p][:])
                    S.dma("sp", dd("rs", [128, 8]), rs[:])
                    S.dma("sp", dd("mm", [128, 8]), mm[:])
                for hh in range(2):
                    h = 2 * i + hh
                    for kc in kcs:
                        vsrc = vt[1 - p] if kc == 0 else vt[p]
                        S.add("pe", lambda e, bo=bo, h=h, hh=hh, kc=kc, g=g, vsrc=vsrc, i2=i2, kcs=kcs: e.matmul(
                            bo[:, h * 64:(h + 1) * 64], PT[i2][:, hh * 2 + kc, :], vsrc[:, g * 64:(g + 1) * 64],
                            start=(kc == kcs[0]), stop=(kc == kcs[-1])),
                            [PT[i2][:, hh * 2 + kc, :], vsrc[:, g * 64:(g + 1) * 64]], [bo[:, h * 64:(h + 1) * 64]])
            if DBG <= 5 or DBG == 41:
                continue
            yav = ya[p][:, :].rearrange("p (h d) -> p h d", h=8)
            bov = bo[:, :].rearrange("p (h d) -> p h d", h=8)
            rb = rden[p][:, :].unsqueeze(2).to_broadcast([128, 8, 64])
            S.add("dve", lambda e, yav=yav, bov=bov, rb=rb: e.tensor_tensor(yav, bov, rb, ALU.mult),
                  [bo[:, :], rden[p][:]], [ya[p][:]])
            S.dma("sp", ya_d[n * 128:(n + 1) * 128, :], ya[p][:])
    S.barrier()


RW_DECAY_SCALE = -0.6065306597126334


def rwkv_phase(S, nc, T, xin_d, ya_d, hout, W, tag):
    NTL = T // 128
    w_in = W["even_w_in"]
    with contextlib.ExitStack() as es:
        cx = Ctx(nc, es, tag)
        ident = cx.sb("ident", [128, 128], F32)
        id64 = ident[0:64, 0:64]
        idb = cx.sb("idb", [64, 64], BF16)
        win = cx.sb("win", [128, 8, 1792], BF16)
        wout = cx.sb("wout", [128, 8, 1024], BF16)
        gb = cx.sb("gb", [128, 1024], F32)
        bb = cx.sb("bb", [128, 1024], F32)
        gng = cx.sb("gng", [64, 512], F32)
        gnb = cx.sb("gnb", [64, 512], F32)
        rwv = cx.sb("rwv", [64, 66], F32)
        omka = cx.sb("omka", [64, 8], F32)
        mug = cx.sb("mug", [128, 1], F32)
        wup = cx.sb("wup", [128, 512], F32)
        aup = cx.sb("aup", [128, 512], F32)
        gup = cx.sb("gup", [128, 512], F32)
        msu = cx.sb("msu", [64, 64], F32)
        miu = cx.sb("miu", [64, 64], F32)
        msl = cx.sb("msl", [64, 64], F32)
        ones = cx.sb("ones", [64, 128], F32)
        onesb = cx.sb("onesb", [64, 2], BF16)
        xinb = [cx.sb("xin%d" % i, [128, 1024], F32) for i in range(2)]
        xT = [cx.sb("xT%d" % i, [128, 8, 128], BF16) for i in range(2)]
        zraw = cx.sb("zraw", [64, 26, 129], F32)
        zgraw = cx.sb("zgraw", [128, 129], F32)
        zm = cx.sb("zm", [64, 26, 64], F32)
        zgm = cx.sb("zgm", [128, 64], F32)
        th = cx.sb("th", [128, 64], F32)
        zad = cx.sb("zad", [128, 64], F32)
        H = {}
        for nm in ("lw", "a", "kkn", "rn", "k", "cum", "en", "eA", "eC", "b", "rkf"):
            H[nm] = cx.sb("h_" + nm, [64, 8, 64], F32)
        for nm in ("Bt", "Kt", "Bh", "Kh", "zvb"):
            H[nm] = cx.sb("h_" + nm, [64, 8, 64], BF16)
        M = {}
        for nm in ("Nm", "NTm", "P", "Ma0", "Ma1", "MTa0", "MTa1", "Xs", "Us", "Tb0", "Tb1"):
            M[nm] = cx.sb("m_" + nm, [64, 512], BF16)
        DB = []
        for q in range(2):
            d = {}
            d["eR"] = cx.sb("d%d_eR" % q, [64, 8, 64], F32)
            for nm in ("At", "Rt", "rk"):
                d[nm] = cx.sb("d%d_%s" % (q, nm), [64, 8, 64], BF16)
            for nm in ("BhT", "KhT", "Vt", "AKTm", "RBTm", "RKTm", "Qb"):
                d[nm] = cx.sb("d%d_%s" % (q, nm), [64, 512], BF16)
            d["sgd"] = cx.sb("d%d_sgd" % q, [128, 64], F32)
            DB.append(d)
        for nm in ("Qf", "Tf0", "Tf1", "Ys", "sq", "yn", "tmp", "ybo"):
            M[nm] = cx.sb("m_" + nm, [64, 512], F32)
        gs = cx.sb("gs", [64, 8, 8], F32)
        yab = cx.sb("yab", [128, 512], F32)
        catT = [cx.sb("catT%d" % i, [128, 8, 128], BF16) for i in range(2)]
        pre = cx.sb("pre", [128, 1024], F32)
        ob = cx.sb("ob", [128, 1024], F32)
        st = cx.sb("st", [128, 2, 6], F32)
        mv = cx.sb("mv", [128, 2], F32)
        sc = cx.sb("sc", [128, 2], F32)
        pmix = cx.ps("pmix", [128, 1024], F32)
        pbt = cx.ps("pbt", [128, 1024], BF16)
        PB = Banks([cx.ps("ps%d" % i, [128, 512], F32) for i in range(3)])
        PB2 = Banks([cx.ps("pq%d" % i, [128, 512], F32) for i in range(2)])
        bt_i = [0]

        def v3(t):
            return t[:, :].rearrange("p (h d) -> p h d", h=8)

        def bc_h(ap2):
            return ap2.unsqueeze(2).to_broadcast([64, 8, 64])

        def bc_m(ap2):
            return ap2.unsqueeze(1).to_broadcast([64, 8, 64])

        S.dma("sp", ident[:], W["c_ident"])
        S.dma("sp", gb[:], W["even_ln_mix_g"].to_broadcast([128, 1024]))
        S.dma("sp", bb[:], W["even_ln_mix_b"].to_broadcast([128, 1024]))
        S.dma("sp", gng[:], W["rwkv_gn_g"].to_broadcast([64, 512]))
        S.dma("sp", gnb[:], W["rwkv_gn_b"].to_broadcast([64, 512]))
        S.dma("sp", rwv[:], W["c_rwv"])
        S.dma("sp", mug[:], W["c_mug"])
        S.add("pool", lambda e: e.memset(wup[:], 0.0), [], [wup[:]])
        S.add("pool", lambda e: e.memset(aup[:], 0.0), [], [aup[:]])
        S.dma("sp", wup[0:64, :], W["rwkv_w_up"])
        S.dma("sp", aup[0:64, :], W["rwkv_a_up"])
        S.dma("sp", gup[:], W["rwkv_g_up"])
        S.dma("sp", msu[:], W["c_msu"])
        S.dma("sp", miu[:], W["c_miu"])
        S.dma("sp", msl[:], W["c_msl"])
        for j in range(2):
            S.dma("pool", win[:, :, j * 896:(j + 1) * 896],
                  w_in[:, 768 + j * 896:768 + (j + 1) * 896].rearrange("(k p) f -> p k f", p=128))
        S.dma("pool", wout[:], W["even_w_out"].rearrange("(k p) f -> p k f", p=128))
        S.add("pool", lambda e: e.memset(ones[:], 1.0), [], [ones[:]])
        S.add("pool", lambda e: e.memset(onesb[:], 1.0), [], [onesb[:]])
        S.add("pool", lambda e: e.memset(th[:], 0.0), [], [th[:]])
        S.add("pool", lambda e: e.memset(zad[:], 0.0), [], [zad[:]])
        S.add("pool", lambda e: e.memset(zraw[:, :, 0:1], 0.0), [], [zraw[:, :, 0:1]])
        S.add("pool", lambda e: e.memset(zgraw[:, 0:1], 0.0), [], [zgraw[:, 0:1]])
        S.add("pool", lambda e: e.memset(M["Tf0"][:], 0.0), [], [M["Tf0"][:]])
        S.add("pool", lambda e: e.memset(M["Tb0"][:], 0.0), [], [M["Tb0"][:]])
        S.add("act", lambda e: e.copy(idb[:], id64), [id64], [idb[:]])
        S.add("dve", lambda e: e.tensor_scalar(omka[:], rwv[:, 50:58], -1.0, 1.0, ALU.mult, ALU.add), [rwv[:, 50:58]], [omka[:]])
        mu_b = rwv[:, 0:26].unsqueeze(2).to_broadcast([64, 26, 64])
        w0_b = bc_h(rwv[:, 26:34])
        a0_b = bc_h(rwv[:, 34:42])
        kk_b = bc_h(rwv[:, 42:50])
        ka_b = bc_h(rwv[:, 50:58])
        rk_b = bc_h(rwv[:, 58:66])
        omka_b = bc_h(omka[:, :])

        def TT(eng, out, in0, in1, op, reads, writes):
            S.add(eng, lambda e: e.tensor_tensor(out, in0, in1, op), reads, writes)

        def headmm(bank, lhs_fn, rhs_fn):
            for h in range(8):
                lh, rh = lhs_fn(h), rhs_fn(h)
                ob_ = bank[0:64, h * 64:(h + 1) * 64]
                S.add("pe", lambda e, ob_=ob_, lh=lh, rh=rh: e.matmul(ob_, lh, rh, start=True, stop=True),
                      [lh, rh], [ob_])

        def blk(t, h):
            return t[:, h * 64:(h + 1) * 64]

        import os
        RWSTOP = int(os.environ.get("RW_STOP", "99"))
        def proj(n):
            p = n % 2
            xb = xinb[p]
            S.dma("sp", xb[:], xin_d[n * 128:(n + 1) * 128, :])
            emit_load_transpose(S, xb, ident, [PB.nxt(), PB.nxt()], xT[p][:])
            if n > 0:
                S.add("pool", lambda e: e.tensor_copy(zraw[:, :, 0:1], zraw[:, :, 128:129]), [zraw[:, :, 128:129]], [zraw[:, :, 0:1]])
                S.add("pool", lambda e: e.tensor_copy(zgraw[:, 0:1], zgraw[:, 128:129]), [zgraw[:, 128:129]], [zgraw[:, 0:1]])
            for b in range(7):
                bk = PB.nxt()
                ng = 4 if b < 6 else 2
                for j in range(ng):
                    gi = b * 4 + j
                    col0 = gi * 64
                    for kc in range(8):
                        S.add("pe", lambda e, bk=bk, j=j, col0=col0, kc=kc, p=p: e.matmul(
                            bk[0:64, j * 128:(j + 1) * 128], win[:, kc, col0:col0 + 64], xT[p][:, kc, :],
                            start=(kc == 0), stop=(kc == 7)),
                            [win[:, kc, col0:col0 + 64], xT[p][:, kc, :]], [bk[0:64, j * 128:(j + 1) * 128]])
                dst = zraw[:, b * 4:b * 4 + ng, 1:129]
                src = bk[0:64, 0:ng * 128].rearrange("p (j t) -> p j t", j=ng)
                S.add("act", lambda e, dst=dst, src=src: e.copy(dst, src), [bk[0:64, 0:ng * 128]], [dst])
            bk = PB.nxt()
            for kc in range(8):
                S.add("pe", lambda e, bk=bk, kc=kc, p=p: e.matmul(
                    bk[:, 0:128], win[:, kc, 1664:1792], xT[p][:, kc, :], start=(kc == 0), stop=(kc == 7)),
                    [win[:, kc, 1664:1792], xT[p][:, kc, :]], [bk[:, 0:128]])
            S.add("act", lambda e, bk=bk: e.copy(zgraw[:, 1:129], bk[:, 0:128]), [bk[:, 0:128]], [zgraw[:, 1:129]])

        def stage1(ci, q):
            D_ = DB[q]
            c0 = ci * 64
            zc = zraw[:, :, c0 + 1:c0 + 65]
            zp = zraw[:, :, c0:c0 + 64]
            sgd = D_["sgd"]
            TT("pool", zm[:], zp, zc, ALU.subtract, [zraw[:]], [zm[:]]); yield
            TT("dve", zm[:], zm[:], mu_b, ALU.mult, [zm[:], rwv[:, 0:26]], [zm[:]]); yield
            TT("pool", zm[:], zm[:], zc, ALU.add, [zm[:], zraw[:]], [zm[:]]); yield
            TT("pool", zgm[:], zgraw[:, c0:c0 + 64], zgraw[:, c0 + 1:c0 + 65], ALU.subtract, [zgraw[:]], [zgm[:]]); yield
            S.add("dve", lambda e, c0=c0: e.scalar_tensor_tensor(zgm[:], zgm[:], mug[:, 0:1], zgraw[:, c0 + 1:c0 + 65], ALU.mult, ALU.add),
                  [zgm[:], mug[:], zgraw[:]], [zgm[:]]); yield
            zr = zm[:, 0:8, :]
            zk = zm[:, 8:16, :]
            S.add("act", lambda e: e.activation(th[0:64, :], zm[:, 24, :], AF.Tanh), [zm[:, 24, :]], [th[0:64, :]]); yield
            S.add("pool", lambda e: e.tensor_copy(zad[0:64, :], zm[:, 25, :]), [zm[:, 25, :]], [zad[0:64, :]]); yield
            S.add("act", lambda e: e.activation(sgd[:], zgm[:], AF.Sigmoid), [zgm[:]], [sgd[:]]); yield
            S.add("act", lambda e: e.copy(H["zvb"][:], zm[:, 16:24, :]), [zm[:, 16:24, :]], [H["zvb"][:]]); yield
            bW = PB.nxt()
            headmm(bW, lambda h: wup[:, h * 64:(h + 1) * 64], lambda h: th[:])
            lw, a_, kkn, rn, k_, cum = H["lw"], H["a"], H["kkn"], H["rn"], H["k"], H["cum"]
            en, eA, eC, b_, rkf = H["en"], H["eA"], H["eC"], H["b"], H["rkf"]
            Bt, Kt, Bh, Kh, zvb = H["Bt"], H["Kt"], H["Bh"], H["Kh"], H["zvb"]
            eR, At, Rt, rk = D_["eR"], D_["At"], D_["Rt"], D_["rk"]
            TT("dve", lw[:], v3(bW)[0:64], w0_b, ALU.add, [bW[0:64, :], rwv[:, 26:34]], [lw[:]]); yield
            bA = PB.nxt()
            headmm(bA, lambda h: aup[:, h * 64:(h + 1) * 64], lambda h: zad[:])
            TT("dve", a_[:], v3(bA)[0:64], a0_b, ALU.add, [bA[0:64, :], rwv[:, 34:42]], [a_[:]]); yield
            S.add("act", lambda e: e.activation(lw[:], lw[:], AF.Sigmoid), [lw[:]], [lw[:]]); yield
            S.add("act", lambda e: e.activation(a_[:], a_[:], AF.Sigmoid), [a_[:]], [a_[:]]); yield
            TT("dve", kkn[:], zk, kk_b, ALU.mult, [zm[:, 8:16, :], rwv[:, 42:50]], [kkn[:]]); yield
            TT("pool", rn[:], kkn[:], kkn[:], ALU.mult, [kkn[:]], [rn[:]]); yield
            bS = PB.nxt()
            rn2 = rn[:, :, :].rearrange("p h d -> p (h d)")
            S.add("pe", lambda e, bS=bS, rn2=rn2: e.matmul(bS[:, :], ones[:], rn2, start=True, stop=True),
                  [ones[:], rn[:]], [bS[:, :]])
            S.add("act", lambda e, bS=bS: e.activation(rn[:], v3(bS)[0:64], AF.Sqrt), [bS[0:64, :]], [rn[:]]); yield
            for h in range(8):
                S.add("dve", lambda e, h=h: e.tensor_tensor_scan(cum[:, h, :], ones[:, 0:64], lw[:, h, :], 0.0, ALU.mult, ALU.add),
                      [ones[:, 0:64], lw[:, h, :]], [cum[:, h, :]])
                yield
            S.add("dve", lambda e: e.tensor_scalar(rn[:], rn[:], 1e-12, None, ALU.max), [rn[:]], [rn[:]]); yield
            S.add("dve", lambda e: e.reciprocal(rn[:], rn[:]), [rn[:]], [rn[:]]); yield
            TT("dve", kkn[:], kkn[:], rn[:], ALU.mult, [kkn[:], rn[:]], [kkn[:]]); yield
            TT("pool", k_[:], a_[:], ka_b, ALU.mult, [a_[:], rwv[:, 50:58]], [k_[:]]); yield
            TT("pool", k_[:], k_[:], omka_b, ALU.add, [k_[:], omka[:]], [k_[:]]); yield
            TT("dve", k_[:], k_[:], zk, ALU.mult, [k_[:], zm[:, 8:16, :]], [k_[:]]); yield
            S.add("act", lambda e: e.activation(eR[:], cum[:], AF.Exp, scale=RW_DECAY_SCALE), [cum[:]], [eR[:]]); yield
            S.add("act", lambda e: e.activation(en[:], cum[:], AF.Exp, scale=-RW_DECAY_SCALE), [cum[:]], [en[:]]); yield
            TT("pool", eA[:], cum[:], lw[:], ALU.subtract, [cum[:], lw[:]], [eA[:]]); yield
            S.add("act", lambda e: e.activation(eA[:], eA[:], AF.Exp, scale=RW_DECAY_SCALE), [eA[:]], [eA[:]]); yield
            TT("pool", eC[:], cum[:, :, 63:64].to_broadcast([64, 8, 64]), cum[:], ALU.subtract, [cum[:]], [eC[:]]); yield
            S.add("act", lambda e: e.activation(eC[:], eC[:], AF.Exp, scale=RW_DECAY_SCALE), [eC[:]], [eC[:]]); yield
            S.add("dve", lambda e: e.scalar_tensor_tensor(At[:], kkn[:], -1.0, eA[:], ALU.mult, ALU.mult), [kkn[:], eA[:]], [At[:]]); yield
            TT("pool", b_[:], kkn[:], a_[:], ALU.mult, [kkn[:], a_[:]], [b_[:]]); yield
            TT("dve", Bt[:], b_[:], en[:], ALU.mult, [b_[:], en[:]], [Bt[:]]); yield
            TT("pool", Kt[:], k_[:], en[:], ALU.mult, [k_[:], en[:]], [Kt[:]]); yield
            TT("dve", Rt[:], zr, eR[:], ALU.mult, [zm[:, 0:8, :], eR[:]], [Rt[:]]); yield
            TT("pool", Bh[:], b_[:], eC[:], ALU.mult, [b_[:], eC[:]], [Bh[:]]); yield
            TT("dve", Kh[:], k_[:], eC[:], ALU.mult, [k_[:], eC[:]], [Kh[:]]); yield
            TT("pool", rkf[:], zr, k_[:], ALU.mult, [zm[:, 0:8, :], k_[:]], [rkf[:]]); yield
            TT("pool", rk[:], rkf[:], rk_b, ALU.mult, [rkf[:], rwv[:, 58:66]], [rk[:]]); yield
            for (src3, dstn) in ((Bh, "BhT"), (Kh, "KhT"), (zvb, "Vt")):
                half = bt_i[0] % 2
                bt_i[0] += 1
                for h in range(8):
                    in_ = src3[:, h, :]
                    ob_ = pbt[0:64, half * 512 + h * 64:half * 512 + (h + 1) * 64]
                    S.add("pe", lambda e, ob_=ob_, in_=in_: e.transpose(ob_, in_, idb[:]), [in_, idb[:]], [ob_])
                dst = D_[dstn]
                srcb = pbt[0:64, half * 512:(half + 1) * 512]
                S.add("act" if dstn == "KhT" else "dve", (lambda e, dst=dst, srcb=srcb: e.copy(dst[:], srcb)) if dstn == "KhT" else
                      (lambda e, dst=dst, srcb=srcb: e.tensor_copy(dst[:], srcb)), [srcb], [dst[:]])
                yield
            Nm, NTm, P, Qf = M["Nm"], M["NTm"], M["P"], M["Qf"]
            specs = ((Bt, At, Nm, msu), (At, Bt, NTm, msl), (Kt, At, D_["AKTm"], msu), (Bt, Rt, D_["RBTm"], miu), (Kt, Rt, D_["RKTm"], miu))
            for (L3, R3, dst, msk) in specs:
                bC = PB.nxt()
                headmm(bC, lambda h, L3=L3: L3[:, h, :], lambda h, R3=R3: R3[:, h, :])
                TT("dve", v3(dst), v3(bC)[0:64], bc_m(msk[:, :]), ALU.mult, [bC[0:64, :], msk[:]], [dst[:]]); yield
            TT("pool", v3(Qf), v3(Nm), bc_m(id64), ALU.add, [Nm[:], ident[0:64, 0:64]], [Qf[:]]); yield
            TT("pool", v3(P), v3(NTm), bc_m(id64), ALU.add, [NTm[:], ident[0:64, 0:64]], [P[:]]); yield
            cM, cMT = Nm, NTm
            for lvl in range(1, 6):
                nM = M["Ma%d" % (lvl % 2)]
                nMT = M["MTa%d" % (lvl % 2)]
                bM = PB.nxt()
                headmm(bM, lambda h, cMT=cMT: blk(cMT, h), lambda h, cM=cM: blk(cM, h))
                S.add("act", lambda e, nM=nM, bM=bM: e.copy(nM[:], bM[0:64, :]), [bM[0:64, :]], [nM[:]]); yield
                if lvl < 5:
                    bMT = PB.nxt()
                    headmm(bMT, lambda h, cM=cM: blk(cM, h), lambda h, cMT=cMT: blk(cMT, h))
                    S.add("dve", lambda e, nMT=nMT, bMT=bMT: e.tensor_copy(nMT[:], bMT[0:64, :]), [bMT[0:64, :]], [nMT[:]]); yield
                bQ = PB.nxt()
                headmm(bQ, lambda h: blk(P, h), lambda h, nM=nM: blk(nM, h))
                TT("dve", Qf[:], Qf[:], bQ[0:64, :], ALU.add, [Qf[:], bQ[0:64, :]], [Qf[:]]); yield
                if lvl < 5:
                    bP = PB.nxt()
                    headmm(bP, lambda h, nM=nM: blk(nM, h), lambda h: blk(P, h))
                    TT("dve", P[:], P[:], bP[0:64, :], ALU.add, [P[:], bP[0:64, :]], [P[:]]); yield
                cM, cMT = nM, nMT
            S.add("act", lambda e: e.copy(D_["Qb"][:], Qf[:]), [Qf[:]], [D_["Qb"][:]]); yield

        def stage2(n, ci, q, gcx):
            D_ = DB[q]
            p = n % 2
            c0 = ci * 64
            Tfp, Tfn = M["Tf%d" % (gcx % 2)], M["Tf%d" % ((gcx + 1) % 2)]
            Tbp, Tbn = M["Tb%d" % (gcx % 2)], M["Tb%d" % ((gcx + 1) % 2)]
            eR, At, Rt, rk, sgd = D_["eR"], D_["At"], D_["Rt"], D_["rk"], D_["sgd"]
            BhT, KhT, Vt, AKTm, RBTm, RKTm, Qb = D_["BhT"], D_["KhT"], D_["Vt"], D_["AKTm"], D_["RBTm"], D_["RKTm"], D_["Qb"]
            Xs, Us, Ys, sq, yn, tmp, ybo = M["Xs"], M["Us"], M["Ys"], M["sq"], M["yn"], M["tmp"], M["ybo"]
            bX = PB2.nxt()
            for h in range(8):
                ob_ = bX[0:64, h * 64:(h + 1) * 64]
                S.add("pe", lambda e, ob_=ob_, h=h: e.matmul(ob_, blk(AKTm, h), blk(Vt, h), start=True, stop=False),
                      [blk(AKTm, h), blk(Vt, h)], [ob_])
                S.add("pe", lambda e, ob_=ob_, h=h, Tbp=Tbp: e.matmul(ob_, At[:, h, :], blk(Tbp, h), start=False, stop=True),
                      [At[:, h, :], blk(Tbp, h)], [ob_])
            S.add("act", lambda e, bX=bX: e.copy(Xs[:], bX[0:64, :]), [bX[0:64, :]], [Xs[:]]); yield
            bU = PB2.nxt()
            headmm(bU, lambda h: blk(Qb, h), lambda h: blk(Xs, h))
            S.add("dve", lambda e, bU=bU: e.tensor_copy(Us[:], bU[0:64, :]), [bU[0:64, :]], [Us[:]]); yield
            bTn = PB2.nxt()
            for h in range(8):
                ob_ = bTn[0:64, h * 64:(h + 1) * 64]
                S.add("pe", lambda e, ob_=ob_, h=h: e.matmul(ob_, blk(BhT, h), blk(Us, h), start=True, stop=False),
                      [blk(BhT, h), blk(Us, h)], [ob_])
                S.add("pe", lambda e, ob_=ob_, h=h: e.matmul(ob_, blk(KhT, h), blk(Vt, h), start=False, stop=True),
                      [blk(KhT, h), blk(Vt, h)], [ob_])
            TT("pool", v3(Tfn), v3(Tfp), eR[:, :, 63:64].to_broadcast([64, 8, 64]), ALU.mult, [Tfp[:], eR[:]], [Tfn[:]]); yield
            TT("dve", Tfn[:], Tfn[:], bTn[0:64, :], ALU.add, [Tfn[:], bTn[0:64, :]], [Tfn[:]]); yield
            S.add("act", lambda e, Tbn=Tbn, Tfn=Tfn: e.copy(Tbn[:], Tfn[:]), [Tfn[:]], [Tbn[:]]); yield
            bY = PB2.nxt()
            for h in range(8):
                ob_ = bY[0:64, h * 64:(h + 1) * 64]
                S.add("pe", lambda e, ob_=ob_, h=h, Tbp=Tbp: e.matmul(ob_, Rt[:, h, :], blk(Tbp, h), start=True, stop=False),
                      [Rt[:, h, :], blk(Tbp, h)], [ob_])
                S.add("pe", lambda e, ob_=ob_, h=h: e.matmul(ob_, blk(RBTm, h), blk(Us, h), start=False, stop=False),
                      [blk(RBTm, h), blk(Us, h)], [ob_])
                S.add("pe", lambda e, ob_=ob_, h=h: e.matmul(ob_, blk(RKTm, h), blk(Vt, h), start=False, stop=True),
                      [blk(RKTm, h), blk(Vt, h)], [ob_])
            S.add("act", lambda e, bY=bY: e.copy(Ys[:], bY[0:64, :]), [bY[0:64, :]], [Ys[:]]); yield
            bB = PB2.nxt()
            for h in range(8):
                ob_ = bB[0:64, h:h + 1]
                S.add("pe", lambda e, ob_=ob_, h=h: e.matmul(ob_, rk[:, h, :], onesb[:, 0:1], start=True, stop=True),
                      [rk[:, h, :], onesb[:, 0:1]], [ob_])
            S.add("dve", lambda e, bB=bB: e.tensor_copy(gs[:, :, 4], bB[0:64, 0:8]), [bB[0:64, 0:8]], [gs[:, :, 4]]); yield
            S.add("dve", lambda e: e.tensor_reduce(gs[:, :, 0], v3(Ys), AX.X, ALU.add), [Ys[:]], [gs[:, :, 0]]); yield
            TT("pool", sq[:], Ys[:], Ys[:], ALU.mult, [Ys[:]], [sq[:]]); yield
            S.add("dve", lambda e: e.tensor_reduce(gs[:, :, 1], v3(sq), AX.X, ALU.add), [sq[:]], [gs[:, :, 1]]); yield
            S.add("dve", lambda e: e.tensor_scalar(gs[:, :, 0:2], gs[:, :, 0:2], 1.0 / 64.0, None, ALU.mult), [gs[:, :, 0:2]], [gs[:, :, 0:2]]); yield
            TT("dve", gs[:, :, 2], gs[:, :, 0], gs[:, :, 0], ALU.mult, [gs[:, :, 0]], [gs[:, :, 2]]); yield
            TT("dve", gs[:, :, 1], gs[:, :, 1], gs[:, :, 2], ALU.subtract, [gs[:, :, 1], gs[:, :, 2]], [gs[:, :, 1]]); yield
            S.add("act", lambda e: e.activation(gs[:, :, 3], gs[:, :, 1], AF.Sqrt, bias=GN_EPS), [gs[:, :, 1]], [gs[:, :, 3]]); yield
            S.add("dve", lambda e: e.reciprocal(gs[:, :, 3], gs[:, :, 3]), [gs[:, :, 3]], [gs[:, :, 3]]); yield
            TT("dve", v3(yn), v3(Ys), gs[:, :, 0:1].to_broadcast([64, 8, 64]), ALU.subtract, [Ys[:], gs[:, :, 0:1]], [yn[:]]); yield
            TT("dve", v3(yn), v3(yn), gs[:, :, 3:4].to_broadcast([64, 8, 64]), ALU.mult, [yn[:], gs[:, :, 3:4]], [yn[:]]); yield
            TT("pool", yn[:], yn[:], gng[:], ALU.mult, [yn[:], gng[:]], [yn[:]]); yield
            TT("pool", yn[:], yn[:], gnb[:], ALU.add, [yn[:], gnb[:]], [yn[:]]); yield
            TT("dve", v3(tmp), v3(Vt), gs[:, :, 4:5].to_broadcast([64, 8, 64]), ALU.mult, [Vt[:], gs[:, :, 4:5]], [tmp[:]]); yield
            TT("pool", yn[:], yn[:], tmp[:], ALU.add, [yn[:], tmp[:]], [yn[:]]); yield
            bG = PB2.nxt()
            S.add("pe", lambda e, bG=bG: e.matmul(bG[0:64, :], sgd[:], gup[:], start=True, stop=True),
                  [sgd[:], gup[:]], [bG[0:64, :]])
            TT("dve", ybo[:], yn[:], bG[0:64, :], ALU.mult, [yn[:], bG[0:64, :]], [ybo[:]]); yield
            bYT = PB2.nxt()
            for j in range(4):
                ob_ = bYT[:, j * 64:(j + 1) * 64]
                in_ = ybo[:, j * 128:(j + 1) * 128]
                S.add("pe", lambda e, ob_=ob_, in_=in_: e.transpose(ob_, in_, id64), [in_, id64], [ob_])
            dst = catT[p][:, 4:8, c0:c0 + 64]
            S.add("act", lambda e, dst=dst, bYT=bYT: e.copy(dst, bYT[:, 0:256].rearrange("p (j t) -> p j t", j=4)),
                  [bYT[:, 0:256]], [dst]); yield

        def outp(n):
            p = n % 2
            xb = xinb[p]
            S.dma("sp", yab[:], ya_d[n * 128:(n + 1) * 128, :])
            bAT = PB2.nxt()
            for j in range(4):
                S.add("pe", lambda e, bAT=bAT, j=j: e.transpose(bAT[:, j * 128:(j + 1) * 128], yab[:, j * 128:(j + 1) * 128], ident[:]),
                      [yab[:, j * 128:(j + 1) * 128], ident[:]], [bAT[:, j * 128:(j + 1) * 128]])
            S.add("act", lambda e, bAT=bAT, p=p: e.copy(catT[p][:, 0:4, :], bAT[:, :].rearrange("p (j t) -> p j t", j=4)),
                  [bAT[:, :]], [catT[p][:, 0:4, :]])
            for hd in range(2):
                for kc in range(8):
                    S.add("pe", lambda e, hd=hd, kc=kc, p=p: e.matmul(
                        pmix[:, hd * 512:(hd + 1) * 512], catT[p][:, kc, :], wout[:, kc, hd * 512:(hd + 1) * 512],
                        start=(kc == 0), stop=(kc == 7)),
                        [catT[p][:, kc, :], wout[:, kc, hd * 512:(hd + 1) * 512]], [pmix[:, hd * 512:(hd + 1) * 512]])
            S.add("dve", lambda e, xb=xb: e.scalar_tensor_tensor(pre[:], xb[:], ALPHA, pmix[:], ALU.mult, ALU.add),
                  [xb[:], pmix[:]], [pre[:]])
            emit_ln(S, cx, pre[:], ob[:], gb, bb, st, mv, sc)
            S.dma("sp", hout[n * 128:(n + 1) * 128, :], ob[:])

        def interleave(ga, gb_, ra=3):
            da = db = False
            while not (da and db):
                for _ in range(ra):
                    if not da:
                        try:
                            next(ga)
                        except StopIteration:
                            da = True
                if not db:
                    try:
                        next(gb_)
                    except StopIteration:
                        db = True

        import os
        NCH = 2 * NTL
        proj(0)
        for _ in stage1(0, 0):
            pass
        for g in range(1, NCH + 1):
            if g < NCH and g % 2 == 0:
                proj(g // 2)
            g2 = stage2((g - 1) // 2, (g - 1) % 2, (g - 1) % 2, g - 1)
            if g < NCH and os.environ.get("NOPIPE") == "1":
                for _ in g2:
                    pass
                for _ in stage1(g % 2, g % 2):
                    pass
            elif g < NCH:
                interleave(stage1(g % 2, g % 2), g2, int(os.environ.get("RATIO", "2")))
            else:
                for _ in g2:
                    pass
            if (g - 1) % 2 == 1:
                outp((g - 1) // 2)
    S.barrier()


def kernel(**inputs):
    x = np.ascontiguousarray(np.asarray(inputs["x"], dtype=np.float32))
    out = run_phases(x, inputs, ["A", "B2", "C", "E"], x.shape[1])
    return np.ascontiguousarray(out.astype(np.float32))


I32 = mybir.dt.int32
SLOT = 512


def moe_sparse_phase(S, nc, T, hin, hout, W, tag):
    NTL = T // 128
    NSLOT = (2 * T) // SLOT + 8
    NROWS = NSLOT * SLOT
    wg_l, wu_l, wd_l = W["moe_wg_l"], W["moe_wu_l"], W["moe_wd_l"]
    xs = nc.dram_tensor("moe_xs" + tag, [NROWS, 1024], F32).ap()
    ys = nc.dram_tensor("moe_ys" + tag, [NROWS, 1024], F32).ap()
    G = 4
    NG = 14
    NSG = 7
    with contextlib.ExitStack() as es:
        cx = Ctx(nc, es, tag)
        ident = cx.sb("ident", [128, 128], F32)
        ltm = cx.sb("ltm", [128, 128], F32)
        ones = cx.sb("ones", [128, 128], F32)
        slot512 = cx.sb("slot512", [128, NSLOT], F32)
        cgp = cx.sb("cgp", [128, NG], F32)
        rwb = cx.sb("rwb", [128, 8, 8], F32)
        gb = cx.sb("gb", [128, 1024], F32)
        bb = cx.sb("bb", [128, 1024], F32)
        hin4 = [cx.sb("hin4_%d" % i, [128, 4, 1024], F32) for i in range(2)]
        hinb = [hin4[0][:, 0, :], hin4[0][:, 1, :]]
        xTf = cx.sb("xTf", [128, 8, 128], F32)
        lga = cx.sb("lga", [128, NTL, 8], F32)
        lgb = cx.sb("lgb", [128, NTL, 8], F32)
        q1 = cx.sb("q1", [128, NTL, 8], F32)
        m12 = cx.sb("m12", [128, 4, NTL], F32)
        stg = cx.sb("stg", [128, 4, 2, 6], F32)
        mvg = cx.sb("mvg", [128, 4, 2], F32)
        scg = cx.sb("scg", [128, 4, 2], F32)
        E1 = cx.sb("E1", [128, 8, NTL], F32)
        E2 = cx.sb("E2", [128, 8, NTL], F32)
        MM = cx.sb("MM", [128, 8, NTL], F32)
        PW = cx.sb("PW", [128, NTL, 2], F32)
        within = cx.sb("within", [128, 8, NTL], F32)
        tot = cx.sb("tot", [128, 8, NTL], F32)
        incl = cx.sb("incl", [128, 8, NTL], F32)
        posall = cx.sb("posall", [128, 8, NTL], F32)
        ptmp = cx.sb("ptmp", [128, 8, NTL], F32)
        pos1 = cx.sb("pos1", [128, NTL], F32)
        pos2 = cx.sb("pos2", [128, NTL], F32)
        pos1i = cx.sb("pos1i", [128, NTL], I32)
        pos2i = cx.sb("pos2i", [128, NTL], I32)
        sv = cx.sb("sv", [128, 8, 6], F32)
        cmp_ = cx.sb("cmp", [128, NSLOT, 8], F32)
        esl = cx.sb("esl", [128, NSLOT], F32)
        idxf = cx.sb("idxf", [128, NSLOT, NG], F32)
        idxw = cx.sb("idxw", [128, NSLOT, NG], I32)
        xT = [cx.sb("xT%d" % i, [128, 8, SLOT], BF16) for i in range(2)]
        acc = [cx.sb("acc%d" % i, [128, 4, 1024], F32) for i in range(2)]
        NWB = 3
        wgb = [cx.sb("wg%d" % i, [128, 2, 8, 256], BF16) for i in range(NWB)]
        wub = [cx.sb("wu%d" % i, [128, 2, 8, 256], BF16) for i in range(NWB)]
        wdb = [cx.sb("wd%d" % i, [128, 2, 2, 1024], BF16) for i in range(NWB)]
        sg = [cx.sb("sg%d" % i, [128, SLOT], F32) for i in range(2)]
        aT = [cx.sb("aT%d" % i, [128, G, SLOT], BF16) for i in range(2)]
        st = cx.sb("st", [128, 2, 6], F32)
        mv = cx.sb("mv", [128, 2], F32)
        sc = cx.sb("sc", [128, 2], F32)
        pg = [cx.ps("pg%d" % i, [128, 512], F32) for i in range(2)]
        pu = [cx.ps("pu%d" % i, [128, 512], F32) for i in range(2)]
        pd = [cx.ps("pd%d" % i, [128, 1024], F32) for i in range(2)]

        S.dma("sp", ident[:], W["c_ident"])
        S.dma("sp", ltm[:], W["c_ltm"])
        S.dma("sp", slot512[:], W["c_slot512"][:, 0:NSLOT])
        S.dma("sp", cgp[:], W["c_gp"])
        S.dma("sp", rwb[:], W["router_w"].rearrange("(k p) e -> p k e", p=128))
        S.dma("sp", gb[:], W["odd_ln_ffn_g"].to_broadcast([128, 1024]))
        S.dma("sp", bb[:], W["odd_ln_ffn_b"].to_broadcast([128, 1024]))
        S.add("pool", lambda e: e.memset(ones[:], 1.0), [], [ones[:]])

        for tt in range(NTL):
            hb = hinb[tt % 2]
            S.dma("sp", hb[:], hin[tt * 128:(tt + 1) * 128, :])
            for b in range(2):
                pb = pg[b]
                for j in range(4):
                    kc = b * 4 + j
                    S.add("pe", lambda e, pb=pb, j=j, kc=kc, hb=hb: e.transpose(pb[:, j * 128:(j + 1) * 128], hb[:, kc * 128:(kc + 1) * 128], ident[:]),
                          [hb[:, kc * 128:(kc + 1) * 128], ident[:]], [pb[:, j * 128:(j + 1) * 128]])
                dstf = xTf[:, b * 4:(b + 1) * 4, :]
                src = pb[:, 0:512].rearrange("p (j t) -> p j t", j=4)
                S.add("act" if b == 0 else "dve", (lambda e, dstf=dstf, src=src: e.copy(dstf, src)) if b == 0 else
                      (lambda e, dstf=dstf, src=src: e.tensor_copy(dstf, src)), [pb[:, 0:512]], [dstf])
            pl = pu[0][:, 0:8]
            for kc in range(8):
                S.add("pe", lambda e, kc=kc, pl=pl: e.matmul(pl, xTf[:, kc, :], rwb[:, kc, :], start=(kc == 0), stop=(kc == 7)),
                      [xTf[:, kc, :], rwb[:, kc, :]], [pl])
            S.add("dve", lambda e, pl=pl, tt=tt: e.tensor_copy(lga[:, tt, :], pl), [pl], [lga[:, tt, :]])
        bcT = lambda ap2: ap2.unsqueeze(2).to_broadcast([128, NTL, 8])
        E1v = E1[:, :, :].rearrange("p e t -> p t e")
        E2v = E2[:, :, :].rearrange("p e t -> p t e")
        S.add("dve", lambda e: e.tensor_reduce(m12[:, 0, :], lga[:], AX.X, ALU.max), [lga[:]], [m12[:, 0, :]])
        S.add("dve", lambda e: e.tensor_tensor(q1[:], lga[:], bcT(m12[:, 0, :]), ALU.is_equal), [lga[:], m12[:, 0, :]], [q1[:]])
        S.add("dve", lambda e: e.scalar_tensor_tensor(lgb[:], q1[:], -1e30, lga[:], ALU.mult, ALU.add), [q1[:], lga[:]], [lgb[:]])
        S.add("pool", lambda e: e.tensor_copy(E1v, q1[:]), [q1[:]], [E1[:]])
        S.add("dve", lambda e: e.tensor_reduce(m12[:, 1, :], lgb[:], AX.X, ALU.max), [lgb[:]], [m12[:, 1, :]])
        S.add("dve", lambda e: e.tensor_tensor(E2v, lgb[:], bcT(m12[:, 1, :]), ALU.is_equal), [lgb[:], m12[:, 1, :]], [E2[:]])
        S.add("dve", lambda e: e.tensor_tensor(m12[:, 2, :], m12[:, 1, :], m12[:, 0, :], ALU.subtract), [m12[:, 0:2, :]], [m12[:, 2, :]])
        S.add("act", lambda e: e.activation(m12[:, 2, :], m12[:, 2, :], AF.Exp), [m12[:, 2, :]], [m12[:, 2, :]])
        S.add("dve", lambda e: e.tensor_scalar(m12[:, 3, :], m12[:, 2, :], 1.0, None, ALU.add), [m12[:, 2, :]], [m12[:, 3, :]])
        S.add("dve", lambda e: e.reciprocal(PW[:, :, 0], m12[:, 3, :]), [m12[:, 3, :]], [PW[:, :, 0]])
        S.add("dve", lambda e: e.tensor_tensor(PW[:, :, 1], m12[:, 2, :], PW[:, :, 0], ALU.mult), [m12[:, 2, :], PW[:, :, 0]], [PW[:, :, 1]])
        f2 = lambda t: t[:, :, :].rearrange("p e t -> p (e t)")
        S.add("dve", lambda e: e.tensor_tensor(MM[:], E1[:], E2[:], ALU.add), [E1[:], E2[:]], [MM[:]])
        NC_ = 8 * NTL
        S.add("pe", lambda e: e.matmul(pg[0][:, 0:NC_], ltm[:], f2(MM), start=True, stop=True), [ltm[:], MM[:]], [pg[0][:, 0:NC_]])
        S.add("pe", lambda e: e.matmul(pg[1][:, 0:NC_], ones[:], f2(MM), start=True, stop=True), [ones[:], MM[:]], [pg[1][:, 0:NC_]])
        S.add("dve", lambda e: e.tensor_copy(f2(within), pg[0][:, 0:NC_]), [pg[0][:, 0:NC_]], [within[:]])
        S.add("act", lambda e: e.copy(f2(tot), pg[1][:, 0:NC_]), [pg[1][:, 0:NC_]], [tot[:]])
        for ex in range(8):
            S.add("dve", lambda e, ex=ex: e.tensor_tensor_scan(incl[:, ex, :], ones[:, 0:NTL], tot[:, ex, :], 0.0, ALU.mult, ALU.add),
                  [ones[:, 0:NTL], tot[:, ex, :]], [incl[:, ex, :]])
        S.add("dve", lambda e: e.tensor_copy(sv[:, :, 0], incl[:, :, NTL - 1]), [incl[:]], [sv[:, :, 0]])
        S.add("dve", lambda e: e.tensor_tensor(cmp_[:, 0:8, :], sv[:, :, 0].unsqueeze(1).to_broadcast([128, 8, 8]),
                                               slot512[:, 0:8].unsqueeze(2).to_broadcast([128, 8, 8]), ALU.is_gt),
              [sv[:, :, 0], slot512[:, 0:8]], [cmp_[:, 0:8, :]])
        S.add("dve", lambda e: e.tensor_reduce(sv[:, :, 1], cmp_[:, 0:8, :].rearrange("p j e -> p e j"), AX.X, ALU.add),
              [cmp_[:, 0:8, :]], [sv[:, :, 1]])
        S.add("dve", lambda e: e.tensor_scalar(sv[:, :, 3], sv[:, :, 1], float(SLOT), None, ALU.mult), [sv[:, :, 1]], [sv[:, :, 3]])
        S.add("dve", lambda e: e.tensor_tensor_scan(sv[:, :, 4], ones[:, 0:8], sv[:, :, 3], 0.0, ALU.mult, ALU.add),
              [ones[:, 0:8], sv[:, :, 3]], [sv[:, :, 4]])
        S.add("dve", lambda e: e.tensor_tensor(sv[:, :, 5], sv[:, :, 4], sv[:, :, 3], ALU.subtract), [sv[:, :, 4], sv[:, :, 3]], [sv[:, :, 5]])
        S.add("dve", lambda e: e.tensor_tensor(cmp_[:], sv[:, :, 4].unsqueeze(1).to_broadcast([128, NSLOT, 8]),
                                               slot512[:, :].unsqueeze(2).to_broadcast([128, NSLOT, 8]), ALU.is_le),
              [sv[:, :, 4], slot512[:]], [cmp_[:]])
        S.add("dve", lambda e: e.tensor_reduce(esl[:], cmp_[:], AX.X, ALU.add), [cmp_[:]], [esl[:]])
        S.add("dve", lambda e: e.tensor_scalar(esl[:], esl[:], 7.0, None, ALU.min), [esl[:]], [esl[:]])
        S.add("dve", lambda e: e.scalar_tensor_tensor(idxf[:], esl[:, :].unsqueeze(2).to_broadcast([128, NSLOT, NG]), float(NG * 128),
                                                      cgp[:, :].unsqueeze(1).to_broadcast([128, NSLOT, NG]), ALU.mult, ALU.add),
              [esl[:], cgp[:]], [idxf[:]])
        S.add("dve", lambda e: e.tensor_copy(idxw[:], idxf[:]), [idxf[:]], [idxw[:]])
        S.add("dve", lambda e: e.tensor_tensor(posall[:], incl[:], tot[:], ALU.subtract), [incl[:], tot[:]], [posall[:]])
        S.add("dve", lambda e: e.tensor_tensor(posall[:], posall[:], within[:], ALU.add), [posall[:], within[:]], [posall[:]])
        S.add("dve", lambda e: e.tensor_tensor(posall[:], posall[:], sv[:, :, 5:6].to_broadcast([128, 8, NTL]), ALU.add),
              [posall[:], sv[:, :, 5:6]], [posall[:]])
        for (Ek, pk, pki) in ((E1, pos1, pos1i), (E2, pos2, pos2i)):
            S.add("dve", lambda e, Ek=Ek: e.tensor_tensor(ptmp[:], posall[:], Ek[:], ALU.mult), [posall[:], Ek[:]], [ptmp[:]])
            S.add("dve", lambda e, pk=pk: e.tensor_reduce(pk[:], ptmp[:, :, :].rearrange("p e t -> p t e"), AX.X, ALU.add), [ptmp[:]], [pk[:]])
            S.add("dve", lambda e, pk=pk, pki=pki: e.tensor_copy(pki[:], pk[:]), [pk[:]], [pki[:]])
        import os
        ESTOP = int(os.environ.get("E_STOP", "9"))
        if os.environ.get("DBG_E"):
            dd = lambda nm, shp, dt=F32: nc.dram_tensor("dbg_" + nm, shp, dt, kind="ExternalOutput").ap()
            S.dma("sp", dd("pos1i", [128, NTL], I32), pos1i[:])
            S.dma("sp", dd("pos2i", [128, NTL], I32), pos2i[:])
            S.dma("sp", dd("idxw", [128, NSLOT, NG], I32), idxw[:])
            S.dma("sp", dd("esl", [128, NSLOT]), esl[:])
            S.dma("sp", dd("sv", [128, 8, 6]), sv[:])
            S.dma("sp", dd("E1", [128, 8, NTL]), E1[:])
            S.dma("sp", dd("E2", [128, 8, NTL]), E2[:])
            S.dma("sp", dd("PW", [128, NTL, 2]), PW[:])
        for tt in range(NTL if ESTOP >= 2 else 0):
            hb = hin4[(tt // 4) % 2][:, tt % 4, :]
            S.dma("sp", hb, hin[tt * 128:(tt + 1) * 128, :])
            for pki in (pos1i, pos2i):
                S.dma("pool", xs, hb,
                      fn=lambda e, hb=hb, pki=pki, tt=tt: e.indirect_dma_start(
                          out=xs, out_offset=bass.IndirectOffsetOnAxis(ap=pki[:, tt:tt + 1], axis=0), in_=hb, in_offset=None),
                      reads=[hb, pki[:, tt:tt + 1]], writes=[xs])
        widx = 0
        cnt = 0
        def slot_prologue(s_):
            xb_ = xT[s_ % 2]
            for ti in range(4):
                hb = hin4[s_ % 2][:, ti, :]
                S.dma("sp", hb, xs[s_ * SLOT + ti * 128:s_ * SLOT + (ti + 1) * 128, :])
                emit_load_transpose(S, hb, ident, pg, xb_[:, :, ti * 128:(ti + 1) * 128])

        if ESTOP >= 3:
            slot_prologue(0)
        for s in range(NSLOT if ESTOP >= 3 else 0):
            xb = xT[s % 2]
            ac = acc[s % 2]
            for gi in range(NSG):
                if gi == 3 and s + 1 < NSLOT:
                    slot_prologue(s + 1)
                slot = widx % NWB
                widx += 1
                for sub in range(2):
                    ixa = idxw[:, s, 2 * gi + sub:2 * gi + sub + 1]
                    for (dst, srcw) in ((wgb[slot], wg_l), (wub[slot], wu_l), (wdb[slot], wd_l)):
                        dst2 = dst[:, sub].rearrange("p a b -> p (a b)")
                        S.dma("pool", dst2, srcw,
                              fn=lambda e, dst2=dst2, srcw=srcw, ixa=ixa: e.indirect_dma_start(
                                  out=dst2, out_offset=None, in_=srcw, in_offset=bass.IndirectOffsetOnAxis(ap=ixa, axis=0)),
                              reads=[srcw, ixa], writes=[dst2])
                ab = aT[cnt % 2]
                for fc in range(G):
                    sub, c = fc // 2, fc % 2
                    i2 = (cnt * G + fc) % 2
                    pgb, pub, sgb = pg[i2], pu[i2], sg[i2]
                    for kc in range(8):
                        S.add("pe", lambda e, pgb=pgb, kc=kc, sub=sub, c=c, slot=slot, xb=xb: e.matmul(
                            pgb[:, :], wgb[slot][:, sub, kc, c * 128:(c + 1) * 128], xb[:, kc, :], start=(kc == 0), stop=(kc == 7)),
                            [wgb[slot][:, sub, kc, c * 128:(c + 1) * 128], xb[:, kc, :]], [pgb[:, :]])
                    for kc in range(8):
                        S.add("pe", lambda e, pub=pub, kc=kc, sub=sub, c=c, slot=slot, xb=xb: e.matmul(
                            pub[:, :], wub[slot][:, sub, kc, c * 128:(c + 1) * 128], xb[:, kc, :], start=(kc == 0), stop=(kc == 7)),
                            [wub[slot][:, sub, kc, c * 128:(c + 1) * 128], xb[:, kc, :]], [pub[:, :]])
                    S.add("act", lambda e, sgb=sgb, pgb=pgb: e.activation(sgb[:], pgb[:, :], AF.Silu), [pgb[:, :]], [sgb[:]])
                    S.add("dve", lambda e, ab=ab, fc=fc, sgb=sgb, pub=pub: e.tensor_tensor(ab[:, fc, :], sgb[:], pub[:, :], ALU.mult),
                          [sgb[:], pub[:, :]], [ab[:, fc, :]])
                for ti in range(4):
                    pdb = pd[(cnt * 4 + ti) % 2]
                    for hd in range(2):
                        for fc in range(G):
                            S.add("pe", lambda e, pdb=pdb, hd=hd, fc=fc, ab=ab, ti=ti, slot=slot: e.matmul(
                                pdb[:, hd * 512:(hd + 1) * 512], ab[:, fc, ti * 128:(ti + 1) * 128],
                                wdb[slot][:, fc // 2, fc % 2, hd * 512:(hd + 1) * 512], start=(fc == 0), stop=(fc == G - 1)),
                                [ab[:, fc, ti * 128:(ti + 1) * 128], wdb[slot][:, fc // 2, fc % 2, hd * 512:(hd + 1) * 512]],
                                [pdb[:, hd * 512:(hd + 1) * 512]])
                    a_t = ac[:, ti, :]
                    if gi == 0:
                        S.add("act", lambda e, a_t=a_t, pdb=pdb: e.copy(a_t, pdb[:]), [pdb[:]], [a_t])
                    else:
                        S.add("dve", lambda e, a_t=a_t, pdb=pdb: e.tensor_tensor(a_t, pdb[:], a_t, ALU.add), [pdb[:], a_t], [a_t])
                cnt += 1
            S.dma("sp", ys[s * SLOT:(s + 1) * SLOT, :].rearrange("(t p) d -> p t d", p=128), ac[:])
        for g4 in range(0, NTL if ESTOP >= 4 else 0, 4):
            nn = min(4, NTL - g4)
            hq = hin4[(g4 // 4) % 2]
            for k in range(nn):
                tt = g4 + k
                S.dma("sp", hq[:, k, :], hin[tt * 128:(tt + 1) * 128, :])
                for (ab_, pki) in ((acc[0], pos1i), (acc[1], pos2i)):
                    yt = ab_[:, k, :]
                    S.dma("pool", yt, ys,
                          fn=lambda e, yt=yt, pki=pki, tt=tt: e.indirect_dma_start(
                              out=yt, out_offset=None, in_=ys, in_offset=bass.IndirectOffsetOnAxis(ap=pki[:, tt:tt + 1], axis=0)),
                          reads=[ys, pki[:, tt:tt + 1]], writes=[yt])
            for k in range(nn):
                S.add("act", lambda e, hq=hq, k=k: e.mul(hq[:, k, :], hq[:, k, :], ALPHA), [hq[:, k, :]], [hq[:, k, :]])
            for j, ab_ in enumerate((acc[0], acc[1])):
                for k in range(nn):
                    tt = g4 + k
                    S.add("dve", lambda e, hq=hq, k=k, ab_=ab_, tt=tt, j=j: e.scalar_tensor_tensor(
                        hq[:, k, :], ab_[:, k, :], PW[:, tt, j:j + 1], hq[:, k, :], ALU.mult, ALU.add),
                        [ab_[:, k, :], PW[:, tt, j:j + 1], hq[:, k, :]], [hq[:, k, :]])
            emit_ln_group(S, [(hq[:, k, :], acc[0][:, k, :]) for k in range(nn)], gb, bb, stg, mvg, scg)
            for k in range(nn):
                tt = g4 + k
                S.dma("sp", hout[tt * 128:(tt + 1) * 128, :], acc[0][:, k, :])
    S.barrier()


def ffn_stream_phase(S, nc, T, hin, hout, wg, wu, wd, F, lng, lnb, ident_d, tag):
    TB = min(512, T)
    NB = T // TB
    TPB = TB // 128
    GW = 256
    NG = F // GW
    assert NG * GW == F
    NWB = 3
    with contextlib.ExitStack() as es:
        cx = Ctx(nc, es, tag)
        ident = cx.sb("ident", [128, 128], F32)
        gb = cx.sb("gb", [128, 1024], F32)
        bb = cx.sb("bb", [128, 1024], F32)
        hin4 = [cx.sb("hin4_%d" % i, [128, TPB, 1024], F32) for i in range(2)]
        xT = [cx.sb("xT%d" % i, [128, 8, TB], BF16) for i in range(2)]
        acc = [cx.sb("acc%d" % i, [128, TPB, 1024], F32) for i in range(2)]
        wgb = [cx.sb("wg%d" % i, [128, 8, GW], BF16) for i in range(NWB)]
        wub = [cx.sb("wu%d" % i, [128, 8, GW], BF16) for i in range(NWB)]
        wdb = [cx.sb("wd%d" % i, [128, 2, 1024], BF16) for i in range(NWB)]
        sg = [cx.sb("sg%d" % i, [128, TB], F32) for i in range(2)]
        aT = [cx.sb("aT%d" % i, [128, 2, TB], BF16) for i in range(2)]
        stg = cx.sb("stg", [128, 4, 2, 6], F32)
        mvg = cx.sb("mvg", [128, 4, 2], F32)
        scg = cx.sb("scg", [128, 4, 2], F32)
        pg = [cx.ps("pg%d" % i, [128, 512], F32) for i in range(2)]
        pu = [cx.ps("pu%d" % i, [128, 512], F32) for i in range(2)]
        pd = [cx.ps("pd%d" % i, [128, 1024], F32) for i in range(2)]
        S.dma("sp", ident[:], ident_d)
        S.dma("sp", gb[:], lng.to_broadcast([128, 1024]))
        S.dma("sp", bb[:], lnb.to_broadcast([128, 1024]))

        def prologue(b_):
            for ti in range(TPB):
                hb = hin4[b_ % 2][:, ti, :]
                S.dma("sp", hb, hin[b_ * TB + ti * 128:b_ * TB + (ti + 1) * 128, :])
                emit_load_transpose(S, hb, ident, pg, xT[b_ % 2][:, :, ti * 128:(ti + 1) * 128])

        def epilogue(b_):
            hq, ac = hin4[b_ % 2], acc[b_ % 2]
            for ti in range(TPB):
                S.add("dve", lambda e, hq=hq, ac=ac, ti=ti: e.scalar_tensor_tensor(hq[:, ti, :], hq[:, ti, :], ALPHA, ac[:, ti, :], ALU.mult, ALU.add),
                      [hq[:, ti, :], ac[:, ti, :]], [hq[:, ti, :]])
            emit_ln_group(S, [(hq[:, ti, :], ac[:, ti, :]) for ti in range(TPB)], gb, bb, stg, mvg, scg)
            for ti in range(TPB):
                S.dma("sp", hout[b_ * TB + ti * 128:b_ * TB + (ti + 1) * 128, :], ac[:, ti, :])

        widx = 0
        cnt = 0
        pending = [None]
        prologue(0)
        for blk_ in range(NB):
            xb = xT[blk_ % 2]
            ac = acc[blk_ % 2]
            for gi in range(NG):
                if gi == NG // 2 and blk_ + 1 < NB:
                    prologue(blk_ + 1)
                if gi == 2 and blk_ > 0:
                    epilogue(blk_ - 1)
                slot = widx % NWB
                widx += 1
                f0 = gi * GW
                S.dma("pool", wgb[slot][:], wg[0, :, f0:f0 + GW].rearrange("(k p) f -> p k f", p=128))
                S.dma("pool", wub[slot][:], wu[0, :, f0:f0 + GW].rearrange("(k p) f -> p k f", p=128))
                S.dma("pool", wdb[slot][:], wd[0, f0:f0 + GW, :].rearrange("(c p) d -> p c d", p=128))
                ab = aT[cnt % 2]
                for fc in range(2):
                    i2 = (cnt * 2 + fc) % 2
                    pgb, pub, sgb = pg[i2], pu[i2], sg[i2]
                    for kc in range(8):
                        S.add("pe", lambda e, pgb=pgb, kc=kc, fc=fc, slot=slot, xb=xb: e.matmul(
                            pgb[:, 0:TB], wgb[slot][:, kc, fc * 128:(fc + 1) * 128], xb[:, kc, :], start=(kc == 0), stop=(kc == 7)),
                            [wgb[slot][:, kc, fc * 128:(fc + 1) * 128], xb[:, kc, :]], [pgb[:, 0:TB]])
                    for kc in range(8):
                        S.add("pe", lambda e, pub=pub, kc=kc, fc=fc, slot=slot, xb=xb: e.matmul(
                            pub[:, 0:TB], wub[slot][:, kc, fc * 128:(fc + 1) * 128], xb[:, kc, :], start=(kc == 0), stop=(kc == 7)),
                            [wub[slot][:, kc, fc * 128:(fc + 1) * 128], xb[:, kc, :]], [pub[:, 0:TB]])
                    S.add("act", lambda e, sgb=sgb, pgb=pgb: e.activation(sgb[:], pgb[:, 0:TB], AF.Silu), [pgb[:, 0:TB]], [sgb[:]])
                    S.add("dve", lambda e, ab=ab, fc=fc, sgb=sgb, pub=pub: e.tensor_tensor(ab[:, fc, :], sgb[:], pub[:, 0:TB], ALU.mult),
                          [sgb[:], pub[:, 0:TB]], [ab[:, fc, :]])

                def down_part(ab=ab, slot=slot, gi=gi, ac=ac, cnt=cnt):
                    for ti in range(TPB):
                        pdb = pd[(cnt * TPB + ti) % 2]
                        for hd in range(2):
                            for fc in range(2):
                                S.add("pe", lambda e, pdb=pdb, hd=hd, fc=fc, ab=ab, ti=ti, slot=slot: e.matmul(
                                    pdb[:, hd * 512:(hd + 1) * 512], ab[:, fc, ti * 128:(ti + 1) * 128],
                                    wdb[slot][:, fc, hd * 512:(hd + 1) * 512], start=(fc == 0), stop=(fc == 1)),
                                    [ab[:, fc, ti * 128:(ti + 1) * 128], wdb[slot][:, fc, hd * 512:(hd + 1) * 512]],
                                    [pdb[:, hd * 512:(hd + 1) * 512]])
                        a_t = ac[:, ti, :]
                        if gi == 0:
                            S.add("act", lambda e, a_t=a_t, pdb=pdb: e.copy(a_t, pdb[:]), [pdb[:]], [a_t])
                        else:
                            S.add("dve", lambda e, a_t=a_t, pdb=pdb: e.tensor_tensor(a_t, pdb[:], a_t, ALU.add), [pdb[:], a_t], [a_t])
                if pending[0] is not None:
                    pending[0]()
                pending[0] = down_part
                if gi == NG - 1:
                    pending[0]()
                    pending[0] = None
                cnt += 1
        epilogue(NB - 1)
    S.barrier()
```

```python
import contextlib
import numpy as np
import concourse.bass as bass
import concourse.mybir as mybir
from concourse.bass_utils import run_bass_kernel_spmd

F32 = mybir.dt.float32
BF16 = mybir.dt.bfloat16
AF = mybir.ActivationFunctionType
ALU = mybir.AluOpType
AX = mybir.AxisListType

D = 1024
ALPHA = (2.0 * 2) ** 0.25
LN_EPS = 1e-5
GN_EPS = 64e-5
N_CORES = 8


def _dsize(dt):
    if dt == F32:
        return 4
    if dt == BF16:
        return 2
    s = str(dt)
    if "64" in s:
        return 8
    if "32" in s:
        return 4
    if "16" in s:
        return 2
    return 1


class Op:
    __slots__ = ("eng", "fn", "deps", "stream", "inc", "count", "isdma")

    def __init__(self, eng, fn, stream, isdma):
        self.eng = eng
        self.fn = fn
        self.deps = []
        self.stream = stream
        self.inc = False
        self.count = 0
        self.isdma = isdma


def region(ap):
    name = ap.name
    space = str(ap.space)
    off = int(ap.offset)
    pat = ap.ap
    esz = _dsize(ap.dtype)
    if "SB" in space or "PSUM" in space:
        shp = ap.tensor.shape
        rowsize = 1
        for s in list(shp)[1:]:
            rowsize *= int(s)
        p0 = off // rowsize
        f0 = off % rowsize
        npart = pat[0][1]
        f1 = f0 + 1
        for st, c in pat[1:]:
            if st > 0:
                f1 += (c - 1) * st
        if "PSUM" in space:
            b0 = (f0 * esz) // 2048
            b1 = (f1 * esz + 2047) // 2048
            return (name, 0, 128, b0 * 2048, b1 * 2048, True)
        return (name, p0, p0 + npart, f0 * esz, f1 * esz)
    lo = off
    hi = off + 1
    for st, c in pat:
        if st > 0:
            hi += (c - 1) * st
        elif st < 0:
            lo += (c - 1) * st
    return (name, 0, 1, lo * esz, hi * esz)


def _ovl(a, b):
    return a[1] < b[2] and b[1] < a[2] and a[3] < b[4] and b[3] < a[4]


def _contains(a, b):
    return a[1] <= b[1] and a[2] >= b[2] and a[3] <= b[3] and a[4] >= b[4]


class Sched:
    ENGS = ["pe", "act", "dve", "pool", "sp"]

    def __init__(self, nc, n_dma=32):
        self.nc = nc
        self.ops = {e: [] for e in self.ENGS}
        self.rec = {}
        self.n_dma = n_dma
        self.dma_last = [None] * n_dma
        self.dma_ops = [[] for _ in range(n_dma)]
        self.dma_rr = 0

    def _track(self, op, reads, writes):
        deps = op.deps
        st = op.stream
        rregs = [region(ap) for ap in reads]
        wregs = [region(ap) for ap in writes]
        for rg in rregs:
            lst = self.rec.get(rg[0])
            if lst:
                ps = len(rg) > 5
                for r in lst:
                    if (r[2] or (ps and r[1].stream != st)) and _ovl(r[0], rg):
                        p = r[1]
                        if p.isdma and p.stream == st:
                            continue
                        deps.append(p)
        for rg in wregs:
            lst = self.rec.get(rg[0])
            if lst:
                for r in lst:
                    if _ovl(r[0], rg):
                        p = r[1]
                        if p.stream == st:
                            continue
                        deps.append(p)
        for p in deps:
            p.inc = True
        for rg in rregs:
            lst = self.rec.setdefault(rg[0], [])
            lst[:] = [r for r in lst if not ((not r[2]) and r[1].stream == st and _contains(rg, r[0]))]
            lst.append([rg, op, False])
        for rg in wregs:
            lst = self.rec.setdefault(rg[0], [])
            lst[:] = [r for r in lst if not _contains(rg, r[0])]
            lst.append([rg, op, True])

    def add(self, eng, fn, reads=(), writes=()):
        op = Op(eng, fn, eng, False)
        self._track(op, reads, writes)
        self.ops[eng].append(op)
        return op

    def _pick_sem(self, eng):
        half = self.n_dma // 2
        if eng == "pool":
            self.dma_rr_sw = (getattr(self, "dma_rr_sw", -1) + 1) % half
            return half + self.dma_rr_sw
        self.dma_rr = (self.dma_rr + 1) % half
        return self.dma_rr

    def dma(self, eng, out, in_, sem=None, fn=None, reads=None, writes=None, **kw):
        if sem is None:
            sem = self._pick_sem(eng)
        stream = "dma%d" % sem
        if fn is None:
            fn = (lambda e, o=out, i=in_, k=kw: e.dma_start(out=o, in_=i, **k))
        op = Op(eng, fn, stream, True)
        op.inc = True
        prev = self.dma_last[sem]
        if prev is not None:
            op.deps.append(prev)
        self._track(op, [in_] if reads is None else reads, [out] if writes is None else writes)
        self.dma_last[sem] = op
        self.dma_ops[sem].append(op)
        self.ops[eng].append(op)
        return op

    def barrier(self):
        lasts = []
        for e in ["pe", "act", "dve", "pool"]:
            for op in reversed(self.ops[e]):
                if not op.isdma and op.fn is not None:
                    lasts.append(op)
                    break
        for s in range(self.n_dma):
            if self.dma_last[s] is not None:
                lasts.append(self.dma_last[s])
        for p in lasts:
            p.inc = True
        for e in self.ENGS:
            op = Op(e, None, e, False)
            op.deps = list(lasts)
            self.ops[e].append(op)
        self.rec = {}

    def emit(self):
        nc = self.nc
        for e in ["pe", "act", "dve", "pool"]:
            c = 0
            for op in self.ops[e]:
                if op.isdma or op.fn is None:
                    continue
                if op.inc:
                    c += 1
                op.count = c
        for s in range(self.n_dma):
            c = 0
            for op in self.dma_ops[s]:
                c += 16
                op.count = c
        with contextlib.ExitStack() as es:
            sems = {}
            for e in ["pe", "act", "dve", "pool"]:
                sems[e] = es.enter_context(nc.semaphore("s_" + e))
            for s in range(self.n_dma):
                if self.dma_ops[s]:
                    sems["dma%d" % s] = es.enter_context(nc.semaphore("s_dma%d" % s))
            block = es.enter_context(nc.Block())

            def run(engname):
                def body(engine):
                    known = {}
                    for op in self.ops[engname]:
                        need = {}
                        for p in op.deps:
                            v = p.count
                            if v > need.get(p.stream, 0):
                                need[p.stream] = v
                        for stn, v in need.items():
                            if v > known.get(stn, 0):
                                engine.wait_ge(sems[stn], v)
                                known[stn] = v
                        if op.fn is None:
                            continue
                        ins = op.fn(engine)
                        if op.inc:
                            ins.then_inc(sems[op.stream], 16 if op.isdma else 1)
                return body

            block.tensor(run("pe"))
            block.scalar(run("act"))
            block.vector(run("dve"))
            block.gpsimd(run("pool"))
            block.sync(run("sp"))


class Ctx:
    def __init__(self, nc, es, tag):
        self.nc = nc
        self.es = es
        self.tag = tag

    def sb(self, name, shape, dt):
        return self.es.enter_context(self.nc.sbuf_tensor(name + self.tag, list(shape), dt))

    def ps(self, name, shape, dt):
        return self.es.enter_context(self.nc.psum_tensor(name + self.tag, list(shape), dt))


def emit_ln(S, cx, pre, o, gb, bb, st, mv, sc, eps=LN_EPS):
    for c in range(2):
        S.add("dve", lambda e, c=c: e.bn_stats(st[:, c, :], pre[:, c * 512:(c + 1) * 512]),
              [pre[:, c * 512:(c + 1) * 512]], [st[:, c, :]])
    S.add("dve", lambda e: e.bn_aggr(mv[:], st[:]), [st[:]], [mv[:]])
    S.add("act", lambda e: e.activation(sc[:, 0:1], mv[:, 1:2], AF.Sqrt, bias=eps), [mv[:, 1:2]], [sc[:, 0:1]])
    S.add("dve", lambda e: e.reciprocal(sc[:, 0:1], sc[:, 0:1]), [sc[:, 0:1]], [sc[:, 0:1]])
    S.add("dve", lambda e: e.scalar_tensor_tensor(sc[:, 1:2], mv[:, 0:1], -1.0, sc[:, 0:1], ALU.mult, ALU.mult),
          [mv[:, 0:1], sc[:, 0:1]], [sc[:, 1:2]])
    S.add("act", lambda e: e.activation(o, pre, AF.Identity, bias=sc[:, 1:2], scale=sc[:, 0:1]),
          [pre, sc[:]], [o])
    S.add("dve", lambda e: e.tensor_tensor(o, o, gb[:], ALU.mult), [o, gb[:]], [o])
    S.add("pool", lambda e: e.tensor_tensor(o, o, bb[:], ALU.add), [o, bb[:]], [o])


def emit_ln_group(S, pairs, gb, bb, stg, mvg, scg, eps=LN_EPS):
    n = len(pairs)
    for k, (pre, o) in enumerate(pairs):
        for c in range(2):
            S.add("dve", lambda e, k=k, c=c, pre=pre: e.bn_stats(stg[:, k, c, :], pre[:, c * 512:(c + 1) * 512]),
                  [pre[:, c * 512:(c + 1) * 512]], [stg[:, k, c, :]])
    for k in range(n):
        S.add("dve", lambda e, k=k: e.bn_aggr(mvg[:, k, :], stg[:, k, :, :]), [stg[:, k, :, :]], [mvg[:, k, :]])
    S.add("act", lambda e: e.activation(scg[:, 0:n, 0], mvg[:, 0:n, 1], AF.Sqrt, bias=eps), [mvg[:, 0:n, :]], [scg[:, 0:n, 0]])
    S.add("dve", lambda e: e.reciprocal(scg[:, 0:n, 0], scg[:, 0:n, 0]), [scg[:, 0:n, 0]], [scg[:, 0:n, 0]])
    S.add("dve", lambda e: e.scalar_tensor_tensor(scg[:, 0:n, 1], mvg[:, 0:n, 0], -1.0, scg[:, 0:n, 0], ALU.mult, ALU.mult),
          [mvg[:, 0:n, :], scg[:, 0:n, 0]], [scg[:, 0:n, 1]])
    for k, (pre, o) in enumerate(pairs):
        S.add("act", lambda e, k=k, pre=pre, o=o: e.activation(o, pre, AF.Identity, bias=scg[:, k, 1:2], scale=scg[:, k, 0:1]),
              [pre, scg[:, k, :]], [o])
    for k, (pre, o) in enumerate(pairs):
        S.add("dve", lambda e, o=o: e.tensor_tensor(o, o, gb[:], ALU.mult), [o, gb[:]], [o])
    for k, (pre, o) in enumerate(pairs):
        S.add("dve", lambda e, o=o: e.tensor_tensor(o, o, bb[:], ALU.add), [o, bb[:]], [o])


def emit_load_transpose(S, hin_tile, ident, pbanks, xT_dst, xTf_dst=None):
    for b in range(2):
        pb = pbanks[b]
        for j in range(4):
            kc = b * 4 + j
            S.add("pe", lambda e, pb=pb, j=j, kc=kc: e.transpose(pb[:, j * 128:(j + 1) * 128],
                                                               hin_tile[:, kc * 128:(kc + 1) * 128], ident[:]),
                  [hin_tile[:, kc * 128:(kc + 1) * 128], ident[:]], [pb[:, j * 128:(j + 1) * 128]])
        src = pb[:, 0:512].rearrange("p (j t) -> p j t", j=4)
        dst = xT_dst[:, b * 4:(b + 1) * 4, :]
        S.add("act", lambda e, dst=dst, src=src: e.copy(dst, src), [pb[:, 0:512]], [dst])
        if xTf_dst is not None:
            dstf = xTf_dst[:, b * 4:(b + 1) * 4, :]
            S.add("dve", lambda e, dstf=dstf, src=src: e.tensor_copy(dstf, src), [pb[:, 0:512]], [dstf])


def ffn_phase(S, nc, T, hin, hout, wg, wu, wd, F, G, NE, rw, lng, lnb, ident_d, tag):
    HALF = min(T, 2048)
    NH = T // HALF
    TB = min(512, HALF)
    NB = HALF // TB
    NT = HALF // 128
    TPB = TB // 128
    GW = G * 128
    NG = F // GW
    assert NG * GW == F
    with contextlib.ExitStack() as es:
        cx = Ctx(nc, es, tag)
        ident = cx.sb("ident", [128, 128], F32)
        xT = cx.sb("xT", [128, 8, HALF], BF16)
        acc = cx.sb("acc", [128, NT, 1024], F32)
        wgb = [cx.sb("wg%d" % i, [128, 8, GW], BF16) for i in range(2)]
        wub = [cx.sb("wu%d" % i, [128, 8, GW], BF16) for i in range(2)]
        wdb = [cx.sb("wd%d" % i, [128, G, 1024], BF16) for i in range(2)]
        hinb = [cx.sb("hin%d" % i, [128, 1024], F32) for i in range(2)]
        sg = [cx.sb("sg%d" % i, [128, TB], F32) for i in range(2)]
        aT = [cx.sb("aT%d" % i, [128, G, TB], BF16) for i in range(2)]
        gb = cx.sb("gb", [128, 1024], F32)
        bb = cx.sb("bb", [128, 1024], F32)
        st = cx.sb("st", [128, 2, 6], F32)
        mv = cx.sb("mv", [128, 2], F32)
        sc = cx.sb("sc", [128, 2], F32)
        ob = [cx.sb("ob%d" % i, [128, 1024], F32) for i in range(4)]
        stg = cx.sb("stg", [128, 4, 2, 6], F32)
        mvg = cx.sb("mvg", [128, 4, 2], F32)
        scg = cx.sb("scg", [128, 4, 2], F32)
        pg = [cx.ps("pg%d" % i, [128, 512], F32) for i in range(2)]
        pu = [cx.ps("pu%d" % i, [128, 512], F32) for i in range(2)]
        pd = [cx.ps("pd%d" % i, [128, 1024], F32) for i in range(2)]
        if rw is not None:
            rwb = cx.sb("rwb", [128, 8, 8], F32)
            xTf = cx.sb("xTf", [128, 8, 128], F32)
            cw = cx.sb("cw", [128, NT, 8], F32)
            lg = cx.sb("lg", [128, 8], F32)
            lg2 = cx.sb("lg2", [128, 8], F32)
            eq1 = cx.sb("eq1", [128, 8], F32)
            eq2 = cx.sb("eq2", [128, 8], F32)
            sm = cx.sb("sm", [128, 8], F32)
            S.dma("sp", rwb[:], rw.rearrange("(k p) e -> p k e", p=128))
        S.dma("sp", ident[:], ident_d)
        S.dma("sp", gb[:], lng.to_broadcast([128, 1024]))
        S.dma("sp", bb[:], lnb.to_broadcast([128, 1024]))

        widx = 0
        for hf in range(NH):
            t0 = hf * HALF
            for tt in range(NT):
                hb = hinb[tt % 2]
                S.dma("sp", hb[:], hin[t0 + tt * 128:t0 + (tt + 1) * 128, :])
                emit_load_transpose(S, hb, ident, pg, xT[:, :, tt * 128:(tt + 1) * 128],
                                    xTf if rw is not None else None)
                a_t = acc[:, tt, :]
                S.add("act", lambda e, a_t=a_t, hb=hb: e.mul(a_t, hb[:], ALPHA), [hb[:]], [a_t])
                if rw is not None:
                    pl = pu[0][:, 0:8]
                    for kc in range(8):
                        S.add("pe", lambda e, kc=kc, pl=pl: e.matmul(pl, xTf[:, kc, :], rwb[:, kc, :],
                                                                   start=(kc == 0), stop=(kc == 7)),
                              [xTf[:, kc, :], rwb[:, kc, :]], [pl])
                    S.add("dve", lambda e, pl=pl: e.tensor_copy(lg[:], pl), [pl], [lg[:]])
                    S.add("dve", lambda e: e.tensor_reduce(sm[:, 0:1], lg[:], AX.X, ALU.max), [lg[:]], [sm[:, 0:1]])
                    S.add("dve", lambda e: e.tensor_scalar(eq1[:], lg[:], sm[:, 0:1], None, ALU.is_equal),
                          [lg[:], sm[:, 0:1]], [eq1[:]])
                    S.add("dve", lambda e: e.scalar_tensor_tensor(lg2[:], eq1[:], -1e30, lg[:], ALU.mult, ALU.add),
                          [eq1[:], lg[:]], [lg2[:]])
                    S.add("dve", lambda e: e.tensor_reduce(sm[:, 1:2], lg2[:], AX.X, ALU.max), [lg2[:]], [sm[:, 1:2]])
                    S.add("dve", lambda e: e.tensor_scalar(eq2[:], lg2[:], sm[:, 1:2], None, ALU.is_equal),
                          [lg2[:], sm[:, 1:2]], [eq2[:]])
                    S.add("dve", lambda e: e.tensor_tensor(sm[:, 2:3], sm[:, 1:2], sm[:, 0:1], ALU.subtract),
                          [sm[:, 0:2]], [sm[:, 2:3]])
                    S.add("act", lambda e: e.activation(sm[:, 3:4], sm[:, 2:3], AF.Exp), [sm[:, 2:3]], [sm[:, 3:4]])
                    S.add("dve", lambda e: e.tensor_scalar(sm[:, 4:5], sm[:, 3:4], 1.0, None, ALU.add),
                          [sm[:, 3:4]], [sm[:, 4:5]])
                    S.add("dve", lambda e: e.reciprocal(sm[:, 5:6], sm[:, 4:5]), [sm[:, 4:5]], [sm[:, 5:6]])
                    S.add("dve", lambda e: e.tensor_tensor(sm[:, 6:7], sm[:, 3:4], sm[:, 5:6], ALU.mult),
                          [sm[:, 3:4], sm[:, 5:6]], [sm[:, 6:7]])
                    cwt = cw[:, tt, :]
                    S.add("dve", lambda e, cwt=cwt: e.tensor_scalar(cwt, eq1[:], sm[:, 5:6], None, ALU.mult),
                          [eq1[:], sm[:, 5:6]], [cwt])
                    S.add("dve", lambda e, cwt=cwt: e.scalar_tensor_tensor(cwt, eq2[:], sm[:, 6:7], cwt, ALU.mult, ALU.add),
                          [eq2[:], sm[:, 6:7], cwt], [cwt])
            cnt = 0
            for ex in range(NE):
                for gi in range(NG):
                    slot = widx % 2
                    widx += 1
                    f0 = gi * GW
                    S.dma("pool", wgb[slot][:], wg[ex, :, f0:f0 + GW].rearrange("(k p) f -> p k f", p=128))
                    S.dma("pool", wub[slot][:], wu[ex, :, f0:f0 + GW].rearrange("(k p) f -> p k f", p=128))
                    S.dma("pool", wdb[slot][:], wd[ex, f0:f0 + GW, :].rearrange("(c p) d -> p c d", p=128))
                    for blk in range(NB):
                        c0 = blk * TB
                        ab = aT[cnt % 2]
                        for fc in range(G):
                            i2 = (cnt * G + fc) % 2
                            pgb, pub, sgb = pg[i2], pu[i2], sg[i2]
                            for kc in range(8):
                                S.add("pe", lambda e, pgb=pgb, kc=kc, fc=fc, slot=slot, c0=c0: e.matmul(
                                    pgb[:, 0:TB], wgb[slot][:, kc, fc * 128:(fc + 1) * 128], xT[:, kc, c0:c0 + TB],
                                    start=(kc == 0), stop=(kc == 7)),
                                    [wgb[slot][:, kc, fc * 128:(fc + 1) * 128], xT[:, kc, c0:c0 + TB]], [pgb[:, 0:TB]])
                            for kc in range(8):
                                S.add("pe", lambda e, pub=pub, kc=kc, fc=fc, slot=slot, c0=c0: e.matmul(
                                    pub[:, 0:TB], wub[slot][:, kc, fc * 128:(fc + 1) * 128], xT[:, kc, c0:c0 + TB],
                                    start=(kc == 0), stop=(kc == 7)),
                                    [wub[slot][:, kc, fc * 128:(fc + 1) * 128], xT[:, kc, c0:c0 + TB]], [pub[:, 0:TB]])
                            S.add("act", lambda e, sgb=sgb, pgb=pgb: e.activation(sgb[:], pgb[:, 0:TB], AF.Silu),
                                  [pgb[:, 0:TB]], [sgb[:]])
                            S.add("dve", lambda e, ab=ab, fc=fc, sgb=sgb, pub=pub: e.tensor_tensor(
                                ab[:, fc, :], sgb[:], pub[:, 0:TB], ALU.mult), [sgb[:], pub[:, 0:TB]], [ab[:, fc, :]])
                        for ti in range(TPB):
                            tt = blk * TPB + ti
                            pdb = pd[(cnt * TPB + ti) % 2]
                            for hd in range(2):
                                for fc in range(G):
                                    S.add("pe", lambda e, pdb=pdb, hd=hd, fc=fc, ab=ab, ti=ti, slot=slot: e.matmul(
                                        pdb[:, hd * 512:(hd + 1) * 512], ab[:, fc, ti * 128:(ti + 1) * 128],
                                        wdb[slot][:, fc, hd * 512:(hd + 1) * 512], start=(fc == 0), stop=(fc == G - 1)),
                                        [ab[:, fc, ti * 128:(ti + 1) * 128], wdb[slot][:, fc, hd * 512:(hd + 1) * 512]],
                                        [pdb[:, hd * 512:(hd + 1) * 512]])
                            a_t = acc[:, tt, :]
                            if rw is not None:
                                cws = cw[:, tt, ex:ex + 1]
                                S.add("dve", lambda e, a_t=a_t, pdb=pdb, cws=cws: e.scalar_tensor_tensor(
                                    a_t, pdb[:], cws, a_t, ALU.mult, ALU.add), [pdb[:], cws, a_t], [a_t])
                            else:
                                S.add("dve", lambda e, a_t=a_t, pdb=pdb: e.tensor_tensor(a_t, pdb[:], a_t, ALU.add),
                                      [pdb[:], a_t], [a_t])
                        cnt += 1
            for t4 in range(0, NT, 4):
                nn = min(4, NT - t4)
                emit_ln_group(S, [(acc[:, t4 + k, :], ob[k][:]) for k in range(nn)], gb, bb, stg, mvg, scg)
                for k in range(nn):
                    S.dma("sp", hout[t0 + (t4 + k) * 128:t0 + (t4 + k + 1) * 128, :], ob[k][:])
    S.barrier()


def conv_phase(S, nc, T, hin, hout, w_in, conv_w, w_out, lng, lnb, ident_d, tag):
    TB = min(512, T)
    NB = T // TB
    TPB = TB // 128
    with contextlib.ExitStack() as es:
        cx = Ctx(nc, es, tag)
        ident = cx.sb("ident", [128, 128], F32)
        win = cx.sb("win", [128, 8, 3072], BF16)
        wout = cx.sb("wout", [128, 8, 1024], BF16)
        cwt = cx.sb("cwt", [128, 8, 3], F32)
        xT = [cx.sb("xT%d" % i, [128, 8, TB], BF16) for i in range(2)]
        hinb = [cx.sb("hin%d" % i, [128, TPB, 1024], F32) for i in range(2)]
        up = [cx.sb("up%d" % i, [128, TB + 2], F32) for i in range(8)]
        gcs = [cx.sb("gcs%d" % i, [128, TB], F32) for i in range(2)]
        cv = [cx.sb("cv%d" % i, [128, TB], F32) for i in range(2)]
        vT = [cx.sb("vT%d" % i, [128, 8, TB], BF16) for i in range(2)]
        pre = [cx.sb("pre%d" % i, [128, 1024], F32) for i in range(4)]
        stg = cx.sb("stg", [128, 4, 2, 6], F32)
        mvg = cx.sb("mvg", [128, 4, 2], F32)
        scg = cx.sb("scg", [128, 4, 2], F32)
        gb = cx.sb("gb", [128, 1024], F32)
        bb = cx.sb("bb", [128, 1024], F32)
        st = cx.sb("st", [128, 2, 6], F32)
        mv = cx.sb("mv", [128, 2], F32)
        sc = cx.sb("sc", [128, 2], F32)
        ob = [cx.sb("ob%d" % i, [128, 1024], F32) for i in range(4)]
        pA = [cx.ps("pA%d" % i, [128, 512], F32) for i in range(2)]
        pB = [cx.ps("pB%d" % i, [128, 512], F32) for i in range(2)]
        pC = [cx.ps("pC%d" % i, [128, 512], F32) for i in range(2)]
        pO = cx.ps("pO", [128, 1024], F32)

        S.dma("sp", ident[:], ident_d)
        S.dma("sp", gb[:], lng.to_broadcast([128, 1024]))
        S.dma("sp", bb[:], lnb.to_broadcast([128, 1024]))
        S.dma("sp", cwt[:], conv_w)
        for j in range(3):
            S.dma("pool", win[:, :, j * 1024:(j + 1) * 1024],
                  w_in[:, j * 1024:(j + 1) * 1024].rearrange("(k p) f -> p k f", p=128))
        S.dma("pool", wout[:], w_out.rearrange("(k p) f -> p k f", p=128))
        for c in range(8):
            S.add("pool", lambda e, c=c: e.memset(up[c][:, 0:2], 0.0), [], [up[c][:, 0:2]])

        for blk in range(NB):
            t0 = blk * TB
            hb = hinb[blk % 2]
            xb = xT[blk % 2]
            vb = vT[blk % 2]
            for ti in range(TPB):
                S.dma("sp", hb[:, ti, :], hin[t0 + ti * 128:t0 + (ti + 1) * 128, :])
                emit_load_transpose(S, hb[:, ti, :], ident, pA, xb[:, :, ti * 128:(ti + 1) * 128])
            for c in range(8):
                i2 = c % 2
                for (pp, col0) in ((pA[i2], 0), (pB[i2], 1024), (pC[i2], 2048)):
                    for kc in range(8):
                        S.add("pe", lambda e, pp=pp, col0=col0, kc=kc, c=c, xb=xb: e.matmul(
                            pp[:, 0:TB], win[:, kc, col0 + c * 128:col0 + (c + 1) * 128], xb[:, kc, :],
                            start=(kc == 0), stop=(kc == 7)),
                            [win[:, kc, col0 + c * 128:col0 + (c + 1) * 128], xb[:, kc, :]], [pp[:, 0:TB]])
                g_s = gcs[i2]
                upc = up[c]
                cvb = cv[i2]
                if blk > 0:
                    S.add("pool", lambda e, upc=upc: e.tensor_copy(upc[:, 0:2], upc[:, TB:TB + 2]),
                          [upc[:, TB:TB + 2]], [upc[:, 0:2]])
                S.add("act", lambda e, g_s=g_s, i2=i2: e.copy(g_s[:], pB[i2][:, 0:TB]), [pB[i2][:, 0:TB]], [g_s[:]])
                S.add("dve", lambda e, upc=upc, g_s=g_s, i2=i2: e.tensor_tensor(upc[:, 2:TB + 2], g_s[:], pC[i2][:, 0:TB], ALU.mult),
                      [g_s[:], pC[i2][:, 0:TB]], [upc[:, 2:TB + 2]])
                S.add("act", lambda e, cvb=cvb, upc=upc, c=c: e.activation(cvb[:], upc[:, 0:TB], AF.Copy, scale=cwt[:, c, 0:1]),
                      [upc[:, 0:TB], cwt[:, c, 0:1]], [cvb[:]])
                S.add("dve", lambda e, cvb=cvb, upc=upc, c=c: e.scalar_tensor_tensor(
                    cvb[:], upc[:, 1:TB + 1], cwt[:, c, 1:2], cvb[:], ALU.mult, ALU.add),
                    [upc[:, 1:TB + 1], cwt[:, c, 1:2], cvb[:]], [cvb[:]])
                S.add("dve", lambda e, cvb=cvb, upc=upc, c=c: e.scalar_tensor_tensor(
                    cvb[:], upc[:, 2:TB + 2], cwt[:, c, 2:3], cvb[:], ALU.mult, ALU.add),
                    [upc[:, 2:TB + 2], cwt[:, c, 2:3], cvb[:]], [cvb[:]])
                S.add("dve", lambda e, cvb=cvb, vb=vb, c=c, i2=i2: e.tensor_tensor(vb[:, c, :], cvb[:], pA[i2][:, 0:TB], ALU.mult),
                      [cvb[:], pA[i2][:, 0:TB]], [vb[:, c, :]])
            for ti in range(TPB):
                for hd in range(2):
                    for kc in range(8):
                        S.add("pe", lambda e, hd=hd, kc=kc, ti=ti, vb=vb: e.matmul(
                            pO[:, hd * 512:(hd + 1) * 512], vb[:, kc, ti * 128:(ti + 1) * 128],
                            wout[:, kc, hd * 512:(hd + 1) * 512], start=(kc == 0), stop=(kc == 7)),
                            [vb[:, kc, ti * 128:(ti + 1) * 128], wout[:, kc, hd * 512:(hd + 1) * 512]],
                            [pO[:, hd * 512:(hd + 1) * 512]])
                pr = pre[ti % 4]
                S.add("dve", lambda e, pr=pr, hb=hb, ti=ti: e.scalar_tensor_tensor(
                    pr[:], hb[:, ti, :], ALPHA, pO[:], ALU.mult, ALU.add), [hb[:, ti, :], pO[:]], [pr[:]])
            emit_ln_group(S, [(pre[ti % 4][:], ob[ti % 4][:]) for ti in range(TPB)], gb, bb, stg, mvg, scg)
            for ti in range(TPB):
                S.dma("sp", hout[t0 + ti * 128:t0 + (ti + 1) * 128, :], ob[ti % 4][:])
    S.barrier()


WEIGHT_SPECS = {
    "dense_w_gate": [1, 1024, 2816], "dense_w_up": [1, 1024, 2816], "dense_w_down": [1, 2816, 1024],
    "even_ln_ffn_g": [1, 1024], "even_ln_ffn_b": [1, 1024],
    "odd_w_in": [1024, 3072], "odd_conv_w": [128, 8, 3], "odd_w_out": [1024, 1024],
    "odd_ln_mix_g": [1, 1024], "odd_ln_mix_b": [1, 1024],
    "router_w": [1024, 8],
    "moe_w_gate": [8, 1024, 3584], "moe_w_up": [8, 1024, 3584], "moe_w_down": [8, 3584, 1024],
    "odd_ln_ffn_g": [1, 1024], "odd_ln_ffn_b": [1, 1024],
    "c_ident": [128, 128],
    "even_w_in": [1024, 2560], "even_sinks": [1, 8], "c_bias": [128, 8, 256],
    "rwkv_w_up": [64, 512], "rwkv_a_up": [64, 512], "rwkv_g_up": [128, 512],
    "rwkv_gn_g": [1, 512], "rwkv_gn_b": [1, 512], "even_w_out": [1024, 1024],
    "even_ln_mix_g": [1, 1024], "even_ln_mix_b": [1, 1024],
    "moe_wg_l": [14336, 2048], "moe_wu_l": [14336, 2048], "moe_wd_l": [14336, 2048],
    "c_ltm": [128, 128], "c_slot512": [128, 32], "c_gp": [128, 14],
    "c_rwv": [64, 66], "c_mug": [128, 1], "c_msu": [64, 64], "c_miu": [64, 64], "c_msl": [64, 64],
}
PHASE_INPUTS = {
    "B2": ["dense_w_gate", "dense_w_up", "dense_w_down", "even_ln_ffn_g", "even_ln_ffn_b", "c_ident"],
    "E": ["router_w", "moe_wg_l", "moe_wu_l", "moe_wd_l", "odd_ln_ffn_g", "odd_ln_ffn_b", "c_ident", "c_ltm", "c_slot512", "c_gp"],
    "A1": ["even_w_in", "even_sinks", "c_bias", "c_ident"],
    "A": ["even_w_in", "even_sinks", "c_bias", "c_ident", "rwkv_w_up", "rwkv_a_up", "rwkv_g_up", "rwkv_gn_g", "rwkv_gn_b",
          "even_w_out", "even_ln_mix_g", "even_ln_mix_b", "c_rwv", "c_mug", "c_msu", "c_miu", "c_msl"],
    "B": ["dense_w_gate", "dense_w_up", "dense_w_down", "even_ln_ffn_g", "even_ln_ffn_b", "c_ident"],
    "C": ["odd_w_in", "odd_conv_w", "odd_w_out", "odd_ln_mix_g", "odd_ln_mix_b", "c_ident"],
    "D": ["router_w", "moe_w_gate", "moe_w_up", "moe_w_down", "odd_ln_ffn_g", "odd_ln_ffn_b", "c_ident"],
}


def build(T, phases):
    nc = bass.Bass("TRN2", target_bir_lowering=False)
    x = nc.dram_tensor("x", [T, D], F32, kind="ExternalInput").ap()
    ocols = 512 if phases == ["A1"] else D
    out = nc.dram_tensor("out", [T, ocols], F32, kind="ExternalOutput").ap()
    names = []
    for p in phases:
        for n in PHASE_INPUTS[p]:
            if n not in names:
                names.append(n)
    W = {n: nc.dram_tensor(n, WEIGHT_SPECS[n], F32, kind="ExternalInput").ap() for n in names}
    hs = [x]
    for i in range(len(phases) - 1):
        hs.append(nc.dram_tensor("hscr%d" % i, [T, D], F32).ap())
    hs.append(out)
    S = Sched(nc)
    for i, p in enumerate(phases):
        hi, ho = hs[i], hs[i + 1]
        if p == "A1":
            attn_phase(S, nc, T, hi, ho, W["even_w_in"], W["even_sinks"], W["c_bias"], W["c_ident"], "_A1")
        elif p == "A":
            ya_scr = nc.dram_tensor("ya_scr", [T, 512], F32).ap()
            attn_phase(S, nc, T, hi, ya_scr, W["even_w_in"], W["even_sinks"], W["c_bias"], W["c_ident"], "_A1")
            rwkv_phase(S, nc, T, hi, ya_scr, ho, W, "_A2")
        elif p == "B2":
            ffn_stream_phase(S, nc, T, hi, ho, W["dense_w_gate"], W["dense_w_up"], W["dense_w_down"], 2816,
                             W["even_ln_ffn_g"], W["even_ln_ffn_b"], W["c_ident"], "_B2")
        elif p == "B":
            ffn_phase(S, nc, T, hi, ho, W["dense_w_gate"], W["dense_w_up"], W["dense_w_down"], 2816, 2, 1, None,
                      W["even_ln_ffn_g"], W["even_ln_ffn_b"], W["c_ident"], "_B")
        elif p == "C":
            conv_phase(S, nc, T, hi, ho, W["odd_w_in"], W["odd_conv_w"], W["odd_w_out"],
                       W["odd_ln_mix_g"], W["odd_ln_mix_b"], W["c_ident"], "_C")
        elif p == "E":
            moe_sparse_phase(S, nc, T, hi, ho, W, "_E")
        elif p == "D":
            ffn_phase(S, nc, T, hi, ho, W["moe_w_gate"], W["moe_w_up"], W["moe_w_down"], 3584, 4, 8, W["router_w"],
                      W["odd_ln_ffn_g"], W["odd_ln_ffn_b"], W["c_ident"], "_D")
    S.emit()
    return nc, names


WANT_MOE_LAYOUT = [False]


def _t5_bucket(dist):
    n = np.maximum(dist, 0)
    max_exact = 16
    lr = np.log(np.maximum(n, 1).astype(np.float32) / np.float32(max_exact)) / np.float32(np.log(128 / 16))
    large = max_exact + (lr.astype(np.float32) * np.float32(32 - max_exact)).astype(np.int32)
    large = np.minimum(large, 31)
    return np.where(n < max_exact, n, large)


def host_consts(inputs):
    c = {"c_ident": np.eye(128, dtype=np.float32)}
    s_i = np.arange(64)[:, None]
    t_i = np.arange(64)[None, :]
    c["c_msu"] = (s_i < t_i).astype(np.float32)
    c["c_miu"] = (s_i <= t_i).astype(np.float32)
    c["c_msl"] = (s_i > t_i).astype(np.float32)
    pp_ = np.arange(128)
    c["c_ltm"] = (pp_[:, None] < pp_[None, :]).astype(np.float32)
    c["c_slot512"] = np.tile((np.arange(32) * 512).astype(np.float32)[None, :], (128, 1))
    c["c_gp"] = (np.arange(14)[None, :] * 128 + pp_[:, None]).astype(np.float32)
    if "moe_w_gate" in inputs and WANT_MOE_LAYOUT[0]:
        for src, dstn in (("moe_w_gate", "moe_wg_l"), ("moe_w_up", "moe_wu_l")):
            a = np.asarray(inputs[src], np.float32).reshape(8, 8, 128, 14, 256)
            c[dstn] = np.ascontiguousarray(a.transpose(0, 3, 2, 1, 4)).reshape(14336, 2048)
        a = np.asarray(inputs["moe_w_down"], np.float32).reshape(8, 14, 2, 128, 1024)
        c["moe_wd_l"] = np.ascontiguousarray(a.transpose(0, 1, 3, 2, 4)).reshape(14336, 2048)
    if "rel_bias_table" in inputs:
        tbl = np.asarray(inputs["rel_bias_table"], np.float32)
        qi = np.arange(128)[:, None]
        ki = np.arange(256)[None, :]
        dist = qi + 128 - ki
        valid = (dist >= 0) & (dist < 128)
        g = tbl[_t5_bucket(dist)]
        g = np.where(valid[:, :, None], g, np.float32(-1e30))
        c["c_bias"] = np.ascontiguousarray(np.transpose(g, (0, 2, 1))).astype(np.float32)
    if "rwkv_mu" in inputs:
        mu = np.asarray(inputs["rwkv_mu"], np.float32).reshape(-1)
        cols = [mu[0:1664].reshape(26, 64).T]
        for nm in ("rwkv_w0", "rwkv_a0", "rwkv_k_k", "rwkv_k_a", "rwkv_r_k"):
            cols.append(np.asarray(inputs[nm], np.float32).reshape(8, 64).T)
        c["c_rwv"] = np.ascontiguousarray(np.concatenate(cols, axis=1))
        c["c_mug"] = np.ascontiguousarray(mu[1664:1792].reshape(128, 1))
    return c


def prep_weights(inputs, names):
    WANT_MOE_LAYOUT[0] = "moe_wg_l" in names
    cons = host_consts(inputs)
    outw = {}
    for n in names:
        if n in cons:
            outw[n] = cons[n]
            continue
        a = np.asarray(inputs[n], dtype=np.float32)
        shp = WEIGHT_SPECS[n]
        if n == "odd_conv_w":
            a = a.reshape(3, 8, 128).transpose(2, 1, 0)
        outw[n] = np.ascontiguousarray(a.reshape(shp))
    return outw


def run_phases(xfull, inputs, phases, T):
    nc, names = build(T, phases)
    w = prep_weights(inputs, names)
    in_maps = []
    for c in range(xfull.shape[0]):
        m = {"x": np.ascontiguousarray(xfull[c])}
        m.update(w)
        in_maps.append(m)
    res = run_bass_kernel_spmd(nc, in_maps, core_ids=list(range(len(in_maps))))
    global LAST_RES
    LAST_RES = res.results
    return np.stack([np.asarray(r["out"]) for r in res.results], axis=0)


class Banks:
    def __init__(self, banks):
        self.banks = banks
        self.i = 0

    def nxt(self):
        b = self.banks[self.i % len(self.banks)]
        self.i += 1
        return b


def attn_phase(S, nc, T, xin_d, ya_d, w_in, sinks, bias_d, ident_d, tag):
    NTL = T // 128
    with contextlib.ExitStack() as es:
        cx = Ctx(nc, es, tag)
        ident = cx.sb("ident", [128, 128], F32)
        win = cx.sb("win", [128, 8, 768], BF16)
        biasb = cx.sb("biasb", [128, 8, 256], F32)
        sinkb = cx.sb("sinkb", [128, 8], F32)
        xinb = [cx.sb("xin%d" % i, [128, 1024], F32) for i in range(2)]
        xT = [cx.sb("xT%d" % i, [128, 8, 128], BF16) for i in range(2)]
        qT = [cx.sb("qT%d" % i, [64, 8, 128], BF16) for i in range(2)]
        kT = [cx.sb("kT%d" % i, [64, 2, 128], BF16) for i in range(2)]
        vt = [cx.sb("vt%d" % i, [128, 128], BF16) for i in range(2)]
        sc = [cx.sb("sc%d" % i, [128, 2, 256], F32) for i in range(2)]
        pp = [cx.sb("pp%d" % i, [128, 2, 256], F32) for i in range(2)]
        PT = [cx.sb("PT%d" % i, [128, 4, 128], BF16) for i in range(2)]
        mx = cx.sb("mx", [128, 8], F32)
        mm = cx.sb("mm", [128, 8], F32)
        negm = cx.sb("negm", [128, 8], F32)
        rs = cx.sb("rs", [128, 8], F32)
        esb = cx.sb("esb", [128, 8], F32)
        rden = [cx.sb("rden%d" % i, [128, 8], F32) for i in range(2)]
        ya = [cx.sb("ya%d" % i, [128, 512], F32) for i in range(2)]
        PB = Banks([cx.ps("ps%d" % i, [128, 512], F32) for i in range(6)])
        bo_banks = [cx.ps("pbo%d" % i, [128, 512], F32) for i in range(2)]

        S.dma("sp", ident[:], ident_d)
        S.dma("sp", biasb[:], bias_d)
        S.dma("sp", sinkb[:], sinks.to_broadcast([128, 8]))
        S.dma("pool", win[:], w_in[:, 0:768].rearrange("(k p) f -> p k f", p=128))
        for i in range(2):
            S.add("pool", lambda e, i=i: e.memset(pp[i][:], 0.0), [], [pp[i][:]])

        for n in range(NTL):
            p = n % 2
            xb = xinb[p]
            S.dma("sp", xb[:], xin_d[n * 128:(n + 1) * 128, :])
            emit_load_transpose(S, xb, ident, [PB.nxt(), PB.nxt()], xT[p][:])
            for b in range(2):
                bk = PB.nxt()
                for hh in range(4):
                    h = b * 4 + hh
                    for kc in range(8):
                        S.add("pe", lambda e, bk=bk, hh=hh, h=h, kc=kc, p=p: e.matmul(
                            bk[0:64, hh * 128:(hh + 1) * 128], win[:, kc, h * 64:(h + 1) * 64], xT[p][:, kc, :],
                            start=(kc == 0), stop=(kc == 7)),
                            [win[:, kc, h * 64:(h + 1) * 64], xT[p][:, kc, :]], [bk[0:64, hh * 128:(hh + 1) * 128]])
                dst = qT[p][:, b * 4:(b + 1) * 4, :]
                src = bk[0:64, :].rearrange("p (j t) -> p j t", j=4)
                S.add("act", lambda e, dst=dst, src=src: e.mul(dst, src, 0.125), [bk[0:64, :]], [dst])
            bk = PB.nxt()
            for g in range(2):
                for kc in range(8):
                    S.add("pe", lambda e, bk=bk, g=g, kc=kc, p=p: e.matmul(
                        bk[0:64, g * 128:(g + 1) * 128], win[:, kc, 512 + g * 64:512 + (g + 1) * 64], xT[p][:, kc, :],
                        start=(kc == 0), stop=(kc == 7)),
                        [win[:, kc, 512 + g * 64:512 + (g + 1) * 64], xT[p][:, kc, :]], [bk[0:64, g * 128:(g + 1) * 128]])
            src = bk[0:64, 0:256].rearrange("p (j t) -> p j t", j=2)
            S.add("act", lambda e, src=src, p=p: e.copy(kT[p][:], src), [bk[0:64, 0:256]], [kT[p][:]])
            bv = PB.nxt()
            for kc in range(8):
                S.add("pe", lambda e, bv=bv, kc=kc, p=p: e.matmul(
                    bv[:, 0:128], xT[p][:, kc, :], win[:, kc, 640:768], start=(kc == 0), stop=(kc == 7)),
                    [xT[p][:, kc, :], win[:, kc, 640:768]], [bv[:, 0:128]])
            S.add("dve", lambda e, bv=bv, p=p: e.tensor_copy(vt[p][:], bv[:, 0:128]), [bv[:, 0:128]], [vt[p][:]])

            import os
            DBG = int(os.environ.get("DBG_STOP", "9"))
            DBGN = int(os.environ.get("DBGN", "0"))
            if DBG <= 1:
                continue
            bo = bo_banks[p]
            k0 = 0 if n > 0 else 128
            for i in range(4):
                g = i // 2
                i2 = i % 2
                bs = PB.nxt()
                for hh in range(2):
                    h = 2 * i + hh
                    if n > 0:
                        S.add("pe", lambda e, bs=bs, hh=hh, h=h, g=g, p=p: e.matmul(
                            bs[:, hh * 256:hh * 256 + 128], qT[p][:, h, :], kT[1 - p][:, g, :], start=True, stop=True),
                            [qT[p][:, h, :], kT[1 - p][:, g, :]], [bs[:, hh * 256:hh * 256 + 128]])
                    S.add("pe", lambda e, bs=bs, hh=hh, h=h, g=g, p=p: e.matmul(
                        bs[:, hh * 256 + 128:hh * 256 + 256], qT[p][:, h, :], kT[p][:, g, :], start=True, stop=True),
                        [qT[p][:, h, :], kT[p][:, g, :]], [bs[:, hh * 256 + 128:hh * 256 + 256]])
                bsv = bs[:, :].rearrange("p (j t) -> p j t", j=2)[:, :, k0:256]
                scv = sc[i2][:, :, k0:256]
                ppv = pp[i2][:, :, k0:256]
                S.add("dve", lambda e, scv=scv, bsv=bsv, i=i, k0=k0: e.tensor_tensor(scv, bsv, biasb[:, 2 * i:2 * i + 2, k0:256], ALU.add),
                      [bs[:, :], biasb[:, 2 * i:2 * i + 2, :]], [sc[i2][:]])
                S.add("dve", lambda e, scv=scv, i=i: e.tensor_reduce(mx[:, 2 * i:2 * i + 2], scv, AX.X, ALU.max),
                      [sc[i2][:]], [mx[:, 2 * i:2 * i + 2]])
                S.add("dve", lambda e, i=i: e.tensor_tensor(mm[:, 2 * i:2 * i + 2], mx[:, 2 * i:2 * i + 2], sinkb[:, 2 * i:2 * i + 2], ALU.max),
                      [mx[:, 2 * i:2 * i + 2], sinkb[:, 2 * i:2 * i + 2]], [mm[:, 2 * i:2 * i + 2]])
                S.add("dve", lambda e, i=i: e.tensor_scalar(negm[:, 2 * i:2 * i + 2], mm[:, 2 * i:2 * i + 2], -1.0, None, ALU.mult),
                      [mm[:, 2 * i:2 * i + 2]], [negm[:, 2 * i:2 * i + 2]])
                if DBG <= 2:
                    continue
                for hh in range(2):
                    h = 2 * i + hh
                    S.add("act", lambda e, hh=hh, h=h, i2=i2, k0=k0: e.activation(
                        pp[i2][:, hh, k0:256], sc[i2][:, hh, k0:256], AF.Exp, bias=negm[:, h:h + 1], accum_out=rs[:, h:h + 1]),
                        [sc[i2][:, hh, :], negm[:, h:h + 1]], [pp[i2][:, hh, :], rs[:, h:h + 1]])
                S.add("dve", lambda e, i=i: e.tensor_tensor(esb[:, 2 * i:2 * i + 2], sinkb[:, 2 * i:2 * i + 2], mm[:, 2 * i:2 * i + 2], ALU.subtract),
                      [sinkb[:, 2 * i:2 * i + 2], mm[:, 2 * i:2 * i + 2]], [esb[:, 2 * i:2 * i + 2]])
                S.add("act", lambda e, i=i: e.activation(esb[:, 2 * i:2 * i + 2], esb[:, 2 * i:2 * i + 2], AF.Exp),
                      [esb[:, 2 * i:2 * i + 2]], [esb[:, 2 * i:2 * i + 2]])
                S.add("dve", lambda e, i=i: e.tensor_tensor(esb[:, 2 * i:2 * i + 2], esb[:, 2 * i:2 * i + 2], rs[:, 2 * i:2 * i + 2], ALU.add),
                      [esb[:, 2 * i:2 * i + 2], rs[:, 2 * i:2 * i + 2]], [esb[:, 2 * i:2 * i + 2]])
                S.add("dve", lambda e, i=i, p=p: e.reciprocal(rden[p][:, 2 * i:2 * i + 2], esb[:, 2 * i:2 * i + 2]),
                      [esb[:, 2 * i:2 * i + 2]], [rden[p][:, 2 * i:2 * i + 2]])
                if DBG <= 3:
                    continue
                bt = PB.nxt()
                kcs = (0, 1) if n > 0 else (1,)
                E3 = True
                for hh in range(2):
                    for kc in ((0, 1) if E3 else kcs):
                        j = hh * 2 + kc
                        S.add("pe", lambda e, bt=bt, j=j, hh=hh, kc=kc, i2=i2: e.transpose(
                            bt[:, j * 128:(j + 1) * 128], pp[i2][:, hh, kc * 128:(kc + 1) * 128], ident[:]),
                            [pp[i2][:, hh, kc * 128:(kc + 1) * 128], ident[:]], [bt[:, j * 128:(j + 1) * 128]])
                if DBG == 41:
                    continue
                if n > 0 or E3:
                    S.add("act", lambda e, bt=bt, i2=i2: e.copy(PT[i2][:], bt[:, :].rearrange("p (j t) -> p j t", j=4)),
                          [bt[:, :]], [PT[i2][:]])
                else:
                    for hh in range(2):
                        j = hh * 2 + 1
                        S.add("dve", lambda e, bt=bt, i2=i2, j=j: e.tensor_copy(PT[i2][:, j, :], bt[:, j * 128:(j + 1) * 128]),
                              [bt[:, j * 128:(j + 1) * 128]], [PT[i2][:, j, :]])
                if DBG <= 4 or DBG == 41:
                    continue
                if os.environ.get("DBG_DUMP") and n == DBGN and i == 0:
                    dd = lambda nm, shp, dt=F32: nc.dram_tensor("dbg_" + nm, shp, dt, kind="ExternalOutput").ap()
                    S.dma("sp", dd("sc", [128, 2, 256]), sc[i2][:])
                    S.dma("sp", dd("pp", [128, 2, 256]), pp[i2][:])
                    S.dma("sp", dd("PT", [128, 4, 128], BF16), PT[i2][:])
                    S.dma("sp", dd("qT", [64, 8, 128], BF16), qT[p][:])
                    S.dma("sp", dd("kT", [64, 2, 128], BF16), kT[p][:])
                    S.dma("sp", dd("vt", [128, 128], BF16), vt[p][:])
                    S.dma("sp", dd("rden", [128, 8]), rden[p][:])
                    S.dma("sp", dd("rs", [128, 8]), rs[:])
                    S.dma("sp", dd("mm", [128, 8]), mm[:])
                for hh in range(2):
                    h = 2 * i + hh
                    for kc in kcs:
                        vsrc = vt[1 - p] if kc == 0 else vt[p]
                        S.add("pe", lambda e, bo=bo, h=h, hh=hh, kc=kc, g=g, vsrc=vsrc, i2=i2, kcs=kcs: e.matmul(
                            bo[:, h * 64:(h + 1) * 64], PT[i2][:, hh * 2 + kc, :], vsrc[:, g * 64:(g + 1) * 64],
                            start=(kc == kcs[0]), stop=(kc == kcs[-1])),
                            [PT[i2][:, hh * 2 + kc, :], vsrc[:, g * 64:(g + 1) * 64]], [bo[:, h * 64:(h + 1) * 64]])
            if DBG <= 5 or DBG == 41:
                continue
            yav = ya[p][:, :].rearrange("p (h d) -> p h d", h=8)
            bov = bo[:, :].rearrange("p (h d) -> p h d", h=8)
            rb = rden[p][:, :].unsqueeze(2).to_broadcast([128, 8, 64])
            S.add("dve", lambda e, yav=yav, bov=bov, rb=rb: e.tensor_tensor(yav, bov, rb, ALU.mult),
                  [bo[:, :], rden[p][:]], [ya[p][:]])
            S.dma("sp", ya_d[n * 128:(n + 1) * 128, :], ya[p][:])
    S.barrier()


RW_DECAY_SCALE = -0.6065306597126334


def rwkv_phase(S, nc, T, xin_d, ya_d, hout, W, tag):
    NTL = T // 128
    w_in = W["even_w_in"]
    with contextlib.ExitStack() as es:
        cx = Ctx(nc, es, tag)
        ident = cx.sb("ident", [128, 128], F32)
        id64 = ident[0:64, 0:64]
        idb = cx.sb("idb", [64, 64], BF16)
        win = cx.sb("win", [128, 8, 1792], BF16)
        wout = cx.sb("wout", [128, 8, 1024], BF16)
        gb = cx.sb("gb", [128, 1024], F32)
        bb = cx.sb("bb", [128, 1024], F32)
        gng = cx.sb("gng", [64, 512], F32)
        gnb = cx.sb("gnb", [64, 512], F32)
        rwv = cx.sb("rwv", [64, 66], F32)
        omka = cx.sb("omka", [64, 8], F32)
        mug = cx.sb("mug", [128, 1], F32)
        wup = cx.sb("wup", [128, 512], F32)
        aup = cx.sb("aup", [128, 512], F32)
        gup = cx.sb("gup", [128, 512], F32)
        msu = cx.sb("msu", [64, 64], F32)
        miu = cx.sb("miu", [64, 64], F32)
        msl = cx.sb("msl", [64, 64], F32)
        ones = cx.sb("ones", [64, 128], F32)
        onesb = cx.sb("onesb", [64, 2], BF16)
        xinb = [cx.sb("xin%d" % i, [128, 1024], F32) for i in range(2)]
        xT = [cx.sb("xT%d" % i, [128, 8, 128], BF16) for i in range(2)]
        zraw = cx.sb("zraw", [64, 26, 129], F32)
        zgraw = cx.sb("zgraw", [128, 129], F32)
        zm = cx.sb("zm", [64, 26, 64], F32)
        zgm = cx.sb("zgm", [128, 64], F32)
        th = cx.sb("th", [128, 64], F32)
        zad = cx.sb("zad", [128, 64], F32)
        H = {}
        for nm in ("lw", "a", "kkn", "rn", "k", "cum", "en", "eA", "eC", "b", "rkf"):
            H[nm] = cx.sb("h_" + nm, [64, 8, 64], F32)
        for nm in ("Bt", "Kt", "Bh", "Kh", "zvb"):
            H[nm] = cx.sb("h_" + nm, [64, 8, 64], BF16)
        M = {}
        for nm in ("Nm", "NTm", "P", "Ma0", "Ma1", "MTa0", "MTa1", "Xs", "Us", "Tb0", "Tb1"):
            M[nm] = cx.sb("m_" + nm, [64, 512], BF16)
        DB = []
        for q in range(2):
            d = {}
            d["eR"] = cx.sb("d%d_eR" % q, [64, 8, 64], F32)
            for nm in ("At", "Rt", "rk"):
                d[nm] = cx.sb("d%d_%s" % (q, nm), [64, 8, 64], BF16)
            for nm in ("BhT", "KhT", "Vt", "AKTm", "RBTm", "RKTm", "Qb"):
                d[nm] = cx.sb("d%d_%s" % (q, nm), [64, 512], BF16)
            d["sgd"] = cx.sb("d%d_sgd" % q, [128, 64], F32)
            DB.append(d)
        for nm in ("Qf", "Tf0", "Tf1", "Ys", "sq", "yn", "tmp", "ybo"):
            M[nm] = cx.sb("m_" + nm, [64, 512], F32)
        gs = cx.sb("gs", [64, 8, 8], F32)
        yab = cx.sb("yab", [128, 512], F32)
        catT = [cx.sb("catT%d" % i, [128, 8, 128], BF16) for i in range(2)]
        pre = cx.sb("pre", [128, 1024], F32)
        ob = cx.sb("ob", [128, 1024], F32)
        st = cx.sb("st", [128, 2, 6], F32)
        mv = cx.sb("mv", [128, 2], F32)
        sc = cx.sb("sc", [128, 2], F32)
        pmix = cx.ps("pmix", [128, 1024], F32)
        pbt = cx.ps("pbt", [128, 1024], BF16)
        PB = Banks([cx.ps("ps%d" % i, [128, 512], F32) for i in range(3)])
        PB2 = Banks([cx.ps("pq%d" % i, [128, 512], F32) for i in range(2)])
        bt_i = [0]

        def v3(t):
            return t[:, :].rearrange("p (h d) -> p h d", h=8)

        def bc_h(ap2):
            return ap2.unsqueeze(2).to_broadcast([64, 8, 64])

        def bc_m(ap2):
            return ap2.unsqueeze(1).to_broadcast([64, 8, 64])

        S.dma("sp", ident[:], W["c_ident"])
        S.dma("sp", gb[:], W["even_ln_mix_g"].to_broadcast([128, 1024]))
        S.dma("sp", bb[:], W["even_ln_mix_b"].to_broadcast([128, 1024]))
        S.dma("sp", gng[:], W["rwkv_gn_g"].to_broadcast([64, 512]))
        S.dma("sp", gnb[:], W["rwkv_gn_b"].to_broadcast([64, 512]))
        S.dma("sp", rwv[:], W["c_rwv"])
        S.dma("sp", mug[:], W["c_mug"])
        S.add("pool", lambda e: e.memset(wup[:], 0.0), [], [wup[:]])
        S.add("pool", lambda e: e.memset(aup[:], 0.0), [], [aup[:]])
        S.dma("sp", wup[0:64, :], W["rwkv_w_up"])
        S.dma("sp", aup[0:64, :], W["rwkv_a_up"])
        S.dma("sp", gup[:], W["rwkv_g_up"])
        S.dma("sp", msu[:], W["c_msu"])
        S.dma("sp", miu[:], W["c_miu"])
        S.dma("sp", msl[:], W["c_msl"])
        for j in range(2):
            S.dma("pool", win[:, :, j * 896:(j + 1) * 896],
                  w_in[:, 768 + j * 896:768 + (j + 1) * 896].rearrange("(k p) f -> p k f", p=128))
        S.dma("pool", wout[:], W["even_w_out"].rearrange("(k p) f -> p k f", p=128))
        S.add("pool", lambda e: e.memset(ones[:], 1.0), [], [ones[:]])
        S.add("pool", lambda e: e.memset(onesb[:], 1.0), [], [onesb[:]])
        S.add("pool", lambda e: e.memset(th[:], 0.0), [], [th[:]])
        S.add("pool", lambda e: e.memset(zad[:], 0.0), [], [zad[:]])
        S.add("pool", lambda e: e.memset(zraw[:, :, 0:1], 0.0), [], [zraw[:, :, 0:1]])
        S.add("pool", lambda e: e.memset(zgraw[:, 0:1], 0.0), [], [zgraw[:, 0:1]])
        S.add("pool", lambda e: e.memset(M["Tf0"][:], 0.0), [], [M["Tf0"][:]])
        S.add("pool", lambda e: e.memset(M["Tb0"][:], 0.0), [], [M["Tb0"][:]])
        S.add("act", lambda e: e.copy(idb[:], id64), [id64], [idb[:]])
        S.add("dve", lambda e: e.tensor_scalar(omka[:], rwv[:, 50:58], -1.0, 1.0, ALU.mult, ALU.add), [rwv[:, 50:58]], [omka[:]])
        mu_b = rwv[:, 0:26].unsqueeze(2).to_broadcast([64, 26, 64])
        w0_b = bc_h(rwv[:, 26:34])
        a0_b = bc_h(rwv[:, 34:42])
        kk_b = bc_h(rwv[:, 42:50])
        ka_b = bc_h(rwv[:, 50:58])
        rk_b = bc_h(rwv[:, 58:66])
        omka_b = bc_h(omka[:, :])

        def TT(eng, out, in0, in1, op, reads, writes):
            S.add(eng, lambda e: e.tensor_tensor(out, in0, in1, op), reads, writes)

        def headmm(bank, lhs_fn, rhs_fn):
            for h in range(8):
                lh, rh = lhs_fn(h), rhs_fn(h)
                ob_ = bank[0:64, h * 64:(h + 1) * 64]
                S.add("pe", lambda e, ob_=ob_, lh=lh, rh=rh: e.matmul(ob_, lh, rh, start=True, stop=True),
                      [lh, rh], [ob_])

        def blk(t, h):
            return t[:, h * 64:(h + 1) * 64]

        import os
        RWSTOP = int(os.environ.get("RW_STOP", "99"))
        def proj(n):
            p = n % 2
            xb = xinb[p]
            S.dma("sp", xb[:], xin_d[n * 128:(n + 1) * 128, :])
            emit_load_transpose(S, xb, ident, [PB.nxt(), PB.nxt()], xT[p][:])
            if n > 0:
                S.add("pool", lambda e: e.tensor_copy(zraw[:, :, 0:1], zraw[:, :, 128:129]), [zraw[:, :, 128:129]], [zraw[:, :, 0:1]])
                S.add("pool", lambda e: e.tensor_copy(zgraw[:, 0:1], zgraw[:, 128:129]), [zgraw[:, 128:129]], [zgraw[:, 0:1]])
            for b in range(7):
                bk = PB.nxt()
                ng = 4 if b < 6 else 2
                for j in range(ng):
                    gi = b * 4 + j
                    col0 = gi * 64
                    for kc in range(8):
                        S.add("pe", lambda e, bk=bk, j=j, col0=col0, kc=kc, p=p: e.matmul(
                            bk[0:64, j * 128:(j + 1) * 128], win[:, kc, col0:col0 + 64], xT[p][:, kc, :],
                            start=(kc == 0), stop=(kc == 7)),
                            [win[:, kc, col0:col0 + 64], xT[p][:, kc, :]], [bk[0:64, j * 128:(j + 1) * 128]])
                dst = zraw[:, b * 4:b * 4 + ng, 1:129]
                src = bk[0:64, 0:ng * 128].rearrange("p (j t) -> p j t", j=ng)
                S.add("act", lambda e, dst=dst, src=src: e.copy(dst, src), [bk[0:64, 0:ng * 128]], [dst])
            bk = PB.nxt()
            for kc in range(8):
                S.add("pe", lambda e, bk=bk, kc=kc, p=p: e.matmul(
                    bk[:, 0:128], win[:, kc, 1664:1792], xT[p][:, kc, :], start=(kc == 0), stop=(kc == 7)),
                    [win[:, kc, 1664:1792], xT[p][:, kc, :]], [bk[:, 0:128]])
            S.add("act", lambda e, bk=bk: e.copy(zgraw[:, 1:129], bk[:, 0:128]), [bk[:, 0:128]], [zgraw[:, 1:129]])

        def stage1(ci, q):
            D_ = DB[q]
            c0 = ci * 64
            zc = zraw[:, :, c0 + 1:c0 + 65]
            zp = zraw[:, :, c0:c0 + 64]
            sgd = D_["sgd"]
            TT("pool", zm[:], zp, zc, ALU.subtract, [zraw[:]], [zm[:]]); yield
            TT("dve", zm[:], zm[:], mu_b, ALU.mult, [zm[:], rwv[:, 0:26]], [zm[:]]); yield
            TT("pool", zm[:], zm[:], zc, ALU.add, [zm[:], zraw[:]], [zm[:]]); yield
            TT("pool", zgm[:], zgraw[:, c0:c0 + 64], zgraw[:, c0 + 1:c0 + 65], ALU.subtract, [zgraw[:]], [zgm[:]]); yield
            S.add("dve", lambda e, c0=c0: e.scalar_tensor_tensor(zgm[:], zgm[:], mug[:, 0:1], zgraw[:, c0 + 1:c0 + 65], ALU.mult, ALU.add),
                  [zgm[:], mug[:], zgraw[:]], [zgm[:]]); yield
            zr = zm[:, 0:8, :]
            zk = zm[:, 8:16, :]
            S.add("act", lambda e: e.activation(th[0:64, :], zm[:, 24, :], AF.Tanh), [zm[:, 24, :]], [th[0:64, :]]); yield
            S.add("pool", lambda e: e.tensor_copy(zad[0:64, :], zm[:, 25, :]), [zm[:, 25, :]], [zad[0:64, :]]); yield
            S.add("act", lambda e: e.activation(sgd[:], zgm[:], AF.Sigmoid), [zgm[:]], [sgd[:]]); yield
            S.add("act", lambda e: e.copy(H["zvb"][:], zm[:, 16:24, :]), [zm[:, 16:24, :]], [H["zvb"][:]]); yield
            bW = PB.nxt()
            headmm(bW, lambda h: wup[:, h * 64:(h + 1) * 64], lambda h: th[:])
            lw, a_, kkn, rn, k_, cum = H["lw"], H["a"], H["kkn"], H["rn"], H["k"], H["cum"]
            en, eA, eC, b_, rkf = H["en"], H["eA"], H["eC"], H["b"], H["rkf"]
            Bt, Kt, Bh, Kh, zvb = H["Bt"], H["Kt"], H["Bh"], H["Kh"], H["zvb"]
            eR, At, Rt, rk = D_["eR"], D_["At"], D_["Rt"], D_["rk"]
            TT("dve", lw[:], v3(bW)[0:64], w0_b, ALU.add, [bW[0:64, :], rwv[:, 26:34]], [lw[:]]); yield
            bA = PB.nxt()
            headmm(bA, lambda h: aup[:, h * 64:(h + 1) * 64], lambda h: zad[:])
            TT("dve", a_[:], v3(bA)[0:64], a0_b, ALU.add, [bA[0:64, :], rwv[:, 34:42]], [a_[:]]); yield
            S.add("act", lambda e: e.activation(lw[:], lw[:], AF.Sigmoid), [lw[:]], [lw[:]]); yield
            S.add("act", lambda e: e.activation(a_[:], a_[:], AF.Sigmoid), [a_[:]], [a_[:]]); yield
            TT("dve", kkn[:], zk, kk_b, ALU.mult, [zm[:, 8:16, :], rwv[:, 42:50]], [kkn[:]]); yield
            TT("pool", rn[:], kkn[:], kkn[:], ALU.mult, [kkn[:]], [rn[:]]); yield
            bS = PB.nxt()
            rn2 = rn[:, :, :].rearrange("p h d -> p (h d)")
            S.add("pe", lambda e, bS=bS, rn2=rn2: e.matmul(bS[:, :], ones[:], rn2, start=True, stop=True),
                  [ones[:], rn[:]], [bS[:, :]])
            S.add("act", lambda e, bS=bS: e.activation(rn[:], v3(bS)[0:64], AF.Sqrt), [bS[0:64, :]], [rn[:]]); yield
            for h in range(8):
                S.add("dve", lambda e, h=h: e.tensor_tensor_scan(cum[:, h, :], ones[:, 0:64], lw[:, h, :], 0.0, ALU.mult, ALU.add),
                      [ones[:, 0:64], lw[:, h, :]], [cum[:, h, :]])
                yield
            S.add("dve", lambda e: e.tensor_scalar(rn[:], rn[:], 1e-12, None, ALU.max), [rn[:]], [rn[:]]); yield
            S.add("dve", lambda e: e.reciprocal(rn[:], rn[:]), [rn[:]], [rn[:]]); yield
            TT("dve", kkn[:], kkn[:], rn[:], ALU.mult, [kkn[:], rn[:]], [kkn[:]]); yield
            TT("pool", k_[:], a_[:], ka_b, ALU.mult, [a_[:], rwv[:, 50:58]], [k_[:]]); yield
            TT("pool", k_[:], k_[:], omka_b, ALU.add, [k_[:], omka[:]], [k_[:]]); yield
            TT("dve", k_[:], k_[:], zk, ALU.mult, [k_[:], zm[:, 8:16, :]], [k_[:]]); yield
            S.add("act", lambda e: e.activation(eR[:], cum[:], AF.Exp, scale=RW_DECAY_SCALE), [cum[:]], [eR[:]]); yield
            S.add("act", lambda e: e.activation(en[:], cum[:], AF.Exp, scale=-RW_DECAY_SCALE), [cum[:]], [en[:]]); yield
            TT("pool", eA[:], cum[:], lw[:], ALU.subtract, [cum[:], lw[:]], [eA[:]]); yield
            S.add("act", lambda e: e.activation(eA[:], eA[:], AF.Exp, scale=RW_DECAY_SCALE), [eA[:]], [eA[:]]); yield
            TT("pool", eC[:], cum[:, :, 63:64].to_broadcast([64, 8, 64]), cum[:], ALU.subtract, [cum[:]], [eC[:]]); yield
            S.add("act", lambda e: e.activation(eC[:], eC[:], AF.Exp, scale=RW_DECAY_SCALE), [eC[:]], [eC[:]]); yield
            S.add("dve", lambda e: e.scalar_tensor_tensor(At[:], kkn[:], -1.0, eA[:], ALU.mult, ALU.mult), [kkn[:], eA[:]], [At[:]]); yield
            TT("pool", b_[:], kkn[:], a_[:], ALU.mult, [kkn[:], a_[:]], [b_[:]]); yield
            TT("dve", Bt[:], b_[:], en[:], ALU.mult, [b_[:], en[:]], [Bt[:]]); yield
            TT("pool", Kt[:], k_[:], en[:], ALU.mult, [k_[:], en[:]], [Kt[:]]); yield
            TT("dve", Rt[:], zr, eR[:], ALU.mult, [zm[:, 0:8, :], eR[:]], [Rt[:]]); yield
            TT("pool", Bh[:], b_[:], eC[:], ALU.mult, [b_[:], eC[:]], [Bh[:]]); yield
            TT("dve", Kh[:], k_[:], eC[:], ALU.mult, [k_[:], eC[:]], [Kh[:]]); yield
            TT("pool", rkf[:], zr, k_[:], ALU.mult, [zm[:, 0:8, :], k_[:]], [rkf[:]]); yield
            TT("pool", rk[:], rkf[:], rk_b, ALU.mult, [rkf[:], rwv[:, 58:66]], [rk[:]]); yield
            for (src3, dstn) in ((Bh, "BhT"), (Kh, "KhT"), (zvb, "Vt")):
                half = bt_i[0] % 2
                bt_i[0] += 1
                for h in range(8):
                    in_ = src3[:, h, :]
                    ob_ = pbt[0:64, half * 512 + h * 64:half * 512 + (h + 1) * 64]
                    S.add("pe", lambda e, ob_=ob_, in_=in_: e.transpose(ob_, in_, idb[:]), [in_, idb[:]], [ob_])
                dst = D_[dstn]
                srcb = pbt[0:64, half * 512:(half + 1) * 512]
                S.add("act" if dstn == "KhT" else "dve", (lambda e, dst=dst, srcb=srcb: e.copy(dst[:], srcb)) if dstn == "KhT" else
                      (lambda e, dst=dst, srcb=srcb: e.tensor_copy(dst[:], srcb)), [srcb], [dst[:]])
                yield
            Nm, NTm, P, Qf = M["Nm"], M["NTm"], M["P"], M["Qf"]
            specs = ((Bt, At, Nm, msu), (At, Bt, NTm, msl), (Kt, At, D_["AKTm"], msu), (Bt, Rt, D_["RBTm"], miu), (Kt, Rt, D_["RKTm"], miu))
            for (L3, R3, dst, msk) in specs:
                bC = PB.nxt()
                headmm(bC, lambda h, L3=L3: L3[:, h, :], lambda h, R3=R3: R3[:, h, :])
                TT("dve", v3(dst), v3(bC)[0:64], bc_m(msk[:, :]), ALU.mult, [bC[0:64, :], msk[:]], [dst[:]]); yield
            TT("pool", v3(Qf), v3(Nm), bc_m(id64), ALU.add, [Nm[:], ident[0:64, 0:64]], [Qf[:]]); yield
            TT("pool", v3(P), v3(NTm), bc_m(id64), ALU.add, [NTm[:], ident[0:64, 0:64]], [P[:]]); yield
            cM, cMT = Nm, NTm
            for lvl in range(1, 6):
                nM = M["Ma%d" % (lvl % 2)]
                nMT = M["MTa%d" % (lvl % 2)]
                bM = PB.nxt()
                headmm(bM, lambda h, cMT=cMT: blk(cMT, h), lambda h, cM=cM: blk(cM, h))
                S.add("act", lambda e, nM=nM, bM=bM: e.copy(nM[:], bM[0:64, :]), [bM[0:64, :]], [nM[:]]); yield
                if lvl < 5:
                    bMT = PB.nxt()
                    headmm(bMT, lambda h, cM=cM: blk(cM, h), lambda h, cMT=cMT: blk(cMT, h))
                    S.add("dve", lambda e, nMT=nMT, bMT=bMT: e.tensor_copy(nMT[:], bMT[0:64, :]), [bMT[0:64, :]], [nMT[:]]); yield
                bQ = PB.nxt()
                headmm(bQ, lambda h: blk(P, h), lambda h, nM=nM: blk(nM, h))
                TT("dve", Qf[:], Qf[:], bQ[0:64, :], ALU.add, [Qf[:], bQ[0:64, :]], [Qf[:]]); yield
                if lvl < 5:
                    bP = PB.nxt()
                    headmm(bP, lambda h, nM=nM: blk(nM, h), lambda h: blk(P, h))
                    TT("dve", P[:], P[:], bP[0:64, :], ALU.add, [P[:], bP[0:64, :]], [P[:]]); yield
                cM, cMT = nM, nMT
            S.add("act", lambda e: e.copy(D_["Qb"][:], Qf[:]), [Qf[:]], [D_["Qb"][:]]); yield

        def stage2(n, ci, q, gcx):
            D_ = DB[q]
            p = n % 2
            c0 = ci * 64
            Tfp, Tfn = M["Tf%d" % (gcx % 2)], M["Tf%d" % ((gcx + 1) % 2)]
            Tbp, Tbn = M["Tb%d" % (gcx % 2)], M["Tb%d" % ((gcx + 1) % 2)]
            eR, At, Rt, rk, sgd = D_["eR"], D_["At"], D_["Rt"], D_["rk"], D_["sgd"]
            BhT, KhT, Vt, AKTm, RBTm, RKTm, Qb = D_["BhT"], D_["KhT"], D_["Vt"], D_["AKTm"], D_["RBTm"], D_["RKTm"], D_["Qb"]
            Xs, Us, Ys, sq, yn, tmp, ybo = M["Xs"], M["Us"], M["Ys"], M["sq"], M["yn"], M["tmp"], M["ybo"]
            bX = PB2.nxt()
            for h in range(8):
                ob_ = bX[0:64, h * 64:(h + 1) * 64]
                S.add("pe", lambda e, ob_=ob_, h=h: e.matmul(ob_, blk(AKTm, h), blk(Vt, h), start=True, stop=False),
                      [blk(AKTm, h), blk(Vt, h)], [ob_])
                S.add("pe", lambda e, ob_=ob_, h=h, Tbp=Tbp: e.matmul(ob_, At[:, h, :], blk(Tbp, h), start=False, stop=True),
                      [At[:, h, :], blk(Tbp, h)], [ob_])
            S.add("act", lambda e, bX=bX: e.copy(Xs[:], bX[0:64, :]), [bX[0:64, :]], [Xs[:]]); yield
            bU = PB2.nxt()
            headmm(bU, lambda h: blk(Qb, h), lambda h: blk(Xs, h))
            S.add("dve", lambda e, bU=bU: e.tensor_copy(Us[:], bU[0:64, :]), [bU[0:64, :]], [Us[:]]); yield
            bTn = PB2.nxt()
            for h in range(8):
                ob_ = bTn[0:64, h * 64:(h + 1) * 64]
                S.add("pe", lambda e, ob_=ob_, h=h: e.matmul(ob_, blk(BhT, h), blk(Us, h), start=True, stop=False),
                      [blk(BhT, h), blk(Us, h)], [ob_])
                S.add("pe", lambda e, ob_=ob_, h=h: e.matmul(ob_, blk(KhT, h), blk(Vt, h), start=False, stop=True),
                      [blk(KhT, h), blk(Vt, h)], [ob_])
            TT("pool", v3(Tfn), v3(Tfp), eR[:, :, 63:64].to_broadcast([64, 8, 64]), ALU.mult, [Tfp[:], eR[:]], [Tfn[:]]); yield
            TT("dve", Tfn[:], Tfn[:], bTn[0:64, :], ALU.add, [Tfn[:], bTn[0:64, :]], [Tfn[:]]); yield
            S.add("act", lambda e, Tbn=Tbn, Tfn=Tfn: e.copy(Tbn[:], Tfn[:]), [Tfn[:]], [Tbn[:]]); yield
            bY = PB2.nxt()
            for h in range(8):
                ob_ = bY[0:64, h * 64:(h + 1) * 64]
                S.add("pe", lambda e, ob_=ob_, h=h, Tbp=Tbp: e.matmul(ob_, Rt[:, h, :], blk(Tbp, h), start=True, stop=False),
                      [Rt[:, h, :], blk(Tbp, h)], [ob_])
                S.add("pe", lambda e, ob_=ob_, h=h: e.matmul(ob_, blk(RBTm, h), blk(Us, h), start=False, stop=False),
                      [blk(RBTm, h), blk(Us, h)], [ob_])
                S.add("pe", lambda e, ob_=ob_, h=h: e.matmul(ob_, blk(RKTm, h), blk(Vt, h), start=False, stop=True),
                      [blk(RKTm, h), blk(Vt, h)], [ob_])
            S.add("act", lambda e, bY=bY: e.copy(Ys[:], bY[0:64, :]), [bY[0:64, :]], [Ys[:]]); yield
            bB = PB2.nxt()
            for h in range(8):
                ob_ = bB[0:64, h:h + 1]
                S.add("pe", lambda e, ob_=ob_, h=h: e.matmul(ob_, rk[:, h, :], onesb[:, 0:1], start=True, stop=True),
                      [rk[:, h, :], onesb[:, 0:1]], [ob_])
            S.add("dve", lambda e, bB=bB: e.tensor_copy(gs[:, :, 4], bB[0:64, 0:8]), [bB[0:64, 0:8]], [gs[:, :, 4]]); yield
            S.add("dve", lambda e: e.tensor_reduce(gs[:, :, 0], v3(Ys), AX.X, ALU.add), [Ys[:]], [gs[:, :, 0]]); yield
            TT("pool", sq[:], Ys[:], Ys[:], ALU.mult, [Ys[:]], [sq[:]]); yield
            S.add("dve", lambda e: e.tensor_reduce(gs[:, :, 1], v3(sq), AX.X, ALU.add), [sq[:]], [gs[:, :, 1]]); yield
            S.add("dve", lambda e: e.tensor_scalar(gs[:, :, 0:2], gs[:, :, 0:2], 1.0 / 64.0, None, ALU.mult), [gs[:, :, 0:2]], [gs[:, :, 0:2]]); yield
            TT("dve", gs[:, :, 2], gs[:, :, 0], gs[:, :, 0], ALU.mult, [gs[:, :, 0]], [gs[:, :, 2]]); yield
            TT("dve", gs[:, :, 1], gs[:, :, 1], gs[:, :, 2], ALU.subtract, [gs[:, :, 1], gs[:, :, 2]], [gs[:, :, 1]]); yield
            S.add("act", lambda e: e.activation(gs[:, :, 3], gs[:, :, 1], AF.Sqrt, bias=GN_EPS), [gs[:, :, 1]], [gs[:, :, 3]]); yield
            S.add("dve", lambda e: e.reciprocal(gs[:, :, 3], gs[:, :, 3]), [gs[:, :, 3]], [gs[:, :, 3]]); yield
            TT("dve", v3(yn), v3(Ys), gs[:, :, 0:1].to_broadcast([64, 8, 64]), ALU.subtract, [Ys[:], gs[:, :, 0:1]], [yn[:]]); yield
            TT("dve", v3(yn), v3(yn), gs[:, :, 3:4].to_broadcast([64, 8, 64]), ALU.mult, [yn[:], gs[:, :, 3:4]], [yn[:]]); yield
            TT("pool", yn[:], yn[:], gng[:], ALU.mult, [yn[:], gng[:]], [yn[:]]); yield
            TT("pool", yn[:], yn[:], gnb[:], ALU.add, [yn[:], gnb[:]], [yn[:]]); yield
            TT("dve", v3(tmp), v3(Vt), gs[:, :, 4:5].to_broadcast([64, 8, 64]), ALU.mult, [Vt[:], gs[:, :, 4:5]], [tmp[:]]); yield
            TT("pool", yn[:], yn[:], tmp[:], ALU.add, [yn[:], tmp[:]], [yn[:]]); yield
            bG = PB2.nxt()
            S.add("pe", lambda e, bG=bG: e.matmul(bG[0:64, :], sgd[:], gup[:], start=True, stop=True),
                  [sgd[:], gup[:]], [bG[0:64, :]])
            TT("dve", ybo[:], yn[:], bG[0:64, :], ALU.mult, [yn[:], bG[0:64, :]], [ybo[:]]); yield
            bYT = PB2.nxt()
            for j in range(4):
                ob_ = bYT[:, j * 64:(j + 1) * 64]
                in_ = ybo[:, j * 128:(j + 1) * 128]
                S.add("pe", lambda e, ob_=ob_, in_=in_: e.transpose(ob_, in_, id64), [in_, id64], [ob_])
            dst = catT[p][:, 4:8, c0:c0 + 64]
            S.add("act", lambda e, dst=dst, bYT=bYT: e.copy(dst, bYT[:, 0:256].rearrange("p (j t) -> p j t", j=4)),
                  [bYT[:, 0:256]], [dst]); yield

        def outp(n):
            p = n % 2
            xb = xinb[p]
            S.dma("sp", yab[:], ya_d[n * 128:(n + 1) * 128, :])
            bAT = PB2.nxt()
            for j in range(4):
                S.add("pe", lambda e, bAT=bAT, j=j: e.transpose(bAT[:, j * 128:(j + 1) * 128], yab[:, j * 128:(j + 1) * 128], ident[:]),
                      [yab[:, j * 128:(j + 1) * 128], ident[:]], [bAT[:, j * 128:(j + 1) * 128]])
            S.add("act", lambda e, bAT=bAT, p=p: e.copy(catT[p][:, 0:4, :], bAT[:, :].rearrange("p (j t) -> p j t", j=4)),
                  [bAT[:, :]], [catT[p][:, 0:4, :]])
            for hd in range(2):
                for kc in range(8):
                    S.add("pe", lambda e, hd=hd, kc=kc, p=p: e.matmul(
                        pmix[:, hd * 512:(hd + 1) * 512], catT[p][:, kc, :], wout[:, kc, hd * 512:(hd + 1) * 512],
                        start=(kc == 0), stop=(kc == 7)),
                        [catT[p][:, kc, :], wout[:, kc, hd * 512:(hd + 1) * 512]], [pmix[:, hd * 512:(hd + 1) * 512]])
            S.add("dve", lambda e, xb=xb: e.scalar_tensor_tensor(pre[:], xb[:], ALPHA, pmix[:], ALU.mult, ALU.add),
                  [xb[:], pmix[:]], [pre[:]])
            emit_ln(S, cx, pre[:], ob[:], gb, bb, st, mv, sc)
            S.dma("sp", hout[n * 128:(n + 1) * 128, :], ob[:])

        def interleave(ga, gb_, ra=3):
            da = db = False
            while not (da and db):
                for _ in range(ra):
                    if not da:
                        try:
                            next(ga)
                        except StopIteration:
                            da = True
                if not db:
                    try:
                        next(gb_)
                    except StopIteration:
                        db = True

        import os
        NCH = 2 * NTL
        proj(0)
        for _ in stage1(0, 0):
            pass
        for g in range(1, NCH + 1):
            if g < NCH and g % 2 == 0:
                proj(g // 2)
            g2 = stage2((g - 1) // 2, (g - 1) % 2, (g - 1) % 2, g - 1)
            if g < NCH and os.environ.get("NOPIPE") == "1":
                for _ in g2:
                    pass
                for _ in stage1(g % 2, g % 2):
                    pass
            elif g < NCH:
                interleave(stage1(g % 2, g % 2), g2, int(os.environ.get("RATIO", "2")))
            else:
                for _ in g2:
                    pass
            if (g - 1) % 2 == 1:
                outp((g - 1) // 2)
    S.barrier()


def kernel(**inputs):
    x = np.ascontiguousarray(np.asarray(inputs["x"], dtype=np.float32))
    out = run_phases(x, inputs, ["A", "B2", "C", "E"], x.shape[1])
    return np.ascontiguousarray(out.astype(np.float32))


I32 = mybir.dt.int32
SLOT = 512


def moe_sparse_phase(S, nc, T, hin, hout, W, tag):
    NTL = T // 128
    NSLOT = (2 * T) // SLOT + 8
    NROWS = NSLOT * SLOT
    wg_l, wu_l, wd_l = W["moe_wg_l"], W["moe_wu_l"], W["moe_wd_l"]
    xs = nc.dram_tensor("moe_xs" + tag, [NROWS, 1024], F32).ap()
    ys = nc.dram_tensor("moe_ys" + tag, [NROWS, 1024], F32).ap()
    G = 4
    NG = 14
    NSG = 7
    with contextlib.ExitStack() as es:
        cx = Ctx(nc, es, tag)
        ident = cx.sb("ident", [128, 128], F32)
        ltm = cx.sb("ltm", [128, 128], F32)
        ones = cx.sb("ones", [128, 128], F32)
        slot512 = cx.sb("slot512", [128, NSLOT], F32)
        cgp = cx.sb("cgp", [128, NG], F32)
        rwb = cx.sb("rwb", [128, 8, 8], F32)
        gb = cx.sb("gb", [128, 1024], F32)
        bb = cx.sb("bb", [128, 1024], F32)
        hin4 = [cx.sb("hin4_%d" % i, [128, 4, 1024], F32) for i in range(2)]
        hinb = [hin4[0][:, 0, :], hin4[0][:, 1, :]]
        xTf = cx.sb("xTf", [128, 8, 128], F32)
        lga = cx.sb("lga", [128, NTL, 8], F32)
        lgb = cx.sb("lgb", [128, NTL, 8], F32)
        q1 = cx.sb("q1", [128, NTL, 8], F32)
        m12 = cx.sb("m12", [128, 4, NTL], F32)
        stg = cx.sb("stg", [128, 4, 2, 6], F32)
        mvg = cx.sb("mvg", [128, 4, 2], F32)
        scg = cx.sb("scg", [128, 4, 2], F32)
        E1 = cx.sb("E1", [128, 8, NTL], F32)
        E2 = cx.sb("E2", [128, 8, NTL], F32)
        MM = cx.sb("MM", [128, 8, NTL], F32)
        PW = cx.sb("PW", [128, NTL, 2], F32)
        within = cx.sb("within", [128, 8, NTL], F32)
        tot = cx.sb("tot", [128, 8, NTL], F32)
        incl = cx.sb("incl", [128, 8, NTL], F32)
        posall = cx.sb("posall", [128, 8, NTL], F32)
        ptmp = cx.sb("ptmp", [128, 8, NTL], F32)
        pos1 = cx.sb("pos1", [128, NTL], F32)
        pos2 = cx.sb("pos2", [128, NTL], F32)
        pos1i = cx.sb("pos1i", [128, NTL], I32)
        pos2i = cx.sb("pos2i", [128, NTL], I32)
        sv = cx.sb("sv", [128, 8, 6], F32)
        cmp_ = cx.sb("cmp", [128, NSLOT, 8], F32)
        esl = cx.sb("esl", [128, NSLOT], F32)
        idxf = cx.sb("idxf", [128, NSLOT, NG], F32)
        idxw = cx.sb("idxw", [128, NSLOT, NG], I32)
        xT = [cx.sb("xT%d" % i, [128, 8, SLOT], BF16) for i in range(2)]
        acc = [cx.sb("acc%d" % i, [128, 4, 1024], F32) for i in range(2)]
        NWB = 3
        wgb = [cx.sb("wg%d" % i, [128, 2, 8, 256], BF16) for i in range(NWB)]
        wub = [cx.sb("wu%d" % i, [128, 2, 8, 256], BF16) for i in range(NWB)]
        wdb = [cx.sb("wd%d" % i, [128, 2, 2, 1024], BF16) for i in range(NWB)]
        sg = [cx.sb("sg%d" % i, [128, SLOT], F32) for i in range(2)]
        aT = [cx.sb("aT%d" % i, [128, G, SLOT], BF16) for i in range(2)]
        st = cx.sb("st", [128, 2, 6], F32)
        mv = cx.sb("mv", [128, 2], F32)
        sc = cx.sb("sc", [128, 2], F32)
        pg = [cx.ps("pg%d" % i, [128, 512], F32) for i in range(2)]
        pu = [cx.ps("pu%d" % i, [128, 512], F32) for i in range(2)]
        pd = [cx.ps("pd%d" % i, [128, 1024], F32) for i in range(2)]

        S.dma("sp", ident[:], W["c_ident"])
        S.dma("sp", ltm[:], W["c_ltm"])
        S.dma("sp", slot512[:], W["c_slot512"][:, 0:NSLOT])
        S.dma("sp", cgp[:], W["c_gp"])
        S.dma("sp", rwb[:], W["router_w"].rearrange("(k p) e -> p k e", p=128))
        S.dma("sp", gb[:], W["odd_ln_ffn_g"].to_broadcast([128, 1024]))
        S.dma("sp", bb[:], W["odd_ln_ffn_b"].to_broadcast([128, 1024]))
        S.add("pool", lambda e: e.memset(ones[:], 1.0), [], [ones[:]])
        S.add("pool", lambda e: e.memset(acc[0][:], 0.0), [], [acc[0][:]])
        for r0 in range(0, NROWS, 512):
            S.dma("pool", xs[r0:r0 + 512, :].rearrange("(t p) d -> p t d", p=128), acc[0][:])

        for tt in range(NTL):
            hb = hinb[tt % 2]
            S.dma("sp", hb[:], hin[tt * 128:(tt + 1) * 128, :])
            for b in range(2):
                pb = pg[b]
                for j in range(4):
                    kc = b * 4 + j
                    S.add("pe", lambda e, pb=pb, j=j, kc=kc, hb=hb: e.transpose(pb[:, j * 128:(j + 1) * 128], hb[:, kc * 128:(kc + 1) * 128], ident[:]),
                          [hb[:, kc * 128:(kc + 1) * 128], ident[:]], [pb[:, j * 128:(j + 1) * 128]])
                dstf = xTf[:, b * 4:(b + 1) * 4, :]
                src = pb[:, 0:512].rearrange("p (j t) -> p j t", j=4)
                S.add("act" if b == 0 else "dve", (lambda e, dstf=dstf, src=src: e.copy(dstf, src)) if b == 0 else
                      (lambda e, dstf=dstf, src=src: e.tensor_copy(dstf, src)), [pb[:, 0:512]], [dstf])
            pl = pu[0][:, 0:8]
            for kc in range(8):
                S.add("pe", lambda e, kc=kc, pl=pl: e.matmul(pl, xTf[:, kc, :], rwb[:, kc, :], start=(kc == 0), stop=(kc == 7)),
                      [xTf[:, kc, :], rwb[:, kc, :]], [pl])
            S.add("dve", lambda e, pl=pl, tt=tt: e.tensor_copy(lga[:, tt, :], pl), [pl], [lga[:, tt, :]])
        bcT = lambda ap2: ap2.unsqueeze(2).to_broadcast([128, NTL, 8])
        E1v = E1[:, :, :].rearrange("p e t -> p t e")
        E2v = E2[:, :, :].rearrange("p e t -> p t e")
        S.add("dve", lambda e: e.tensor_reduce(m12[:, 0, :], lga[:], AX.X, ALU.max), [lga[:]], [m12[:, 0, :]])
        S.add("dve", lambda e: e.tensor_tensor(q1[:], lga[:], bcT(m12[:, 0, :]), ALU.is_equal), [lga[:], m12[:, 0, :]], [q1[:]])
        S.add("dve", lambda e: e.scalar_tensor_tensor(lgb[:], q1[:], -1e30, lga[:], ALU.mult, ALU.add), [q1[:], lga[:]], [lgb[:]])
        S.add("pool", lambda e: e.tensor_copy(E1v, q1[:]), [q1[:]], [E1[:]])
        S.add("dve", lambda e: e.tensor_reduce(m12[:, 1, :], lgb[:], AX.X, ALU.max), [lgb[:]], [m12[:, 1, :]])
        S.add("dve", lambda e: e.tensor_tensor(E2v, lgb[:], bcT(m12[:, 1, :]), ALU.is_equal), [lgb[:], m12[:, 1, :]], [E2[:]])
        S.add("dve", lambda e: e.tensor_tensor(m12[:, 2, :], m12[:, 1, :], m12[:, 0, :], ALU.subtract), [m12[:, 0:2, :]], [m12[:, 2, :]])
        S.add("act", lambda e: e.activation(m12[:, 2, :], m12[:, 2, :], AF.Exp), [m12[:, 2, :]], [m12[:, 2, :]])
        S.add("dve", lambda e: e.tensor_scalar(m12[:, 3, :], m12[:, 2, :], 1.0, None, ALU.add), [m12[:, 2, :]], [m12[:, 3, :]])
        S.add("dve", lambda e: e.reciprocal(PW[:, :, 0], m12[:, 3, :]), [m12[:, 3, :]], [PW[:, :, 0]])
        S.add("dve", lambda e: e.tensor_tensor(PW[:, :, 1], m12[:, 2, :], PW[:, :, 0], ALU.mult), [m12[:, 2, :], PW[:, :, 0]], [PW[:, :, 1]])
        f2 = lambda t: t[:, :, :].rearrange("p e t -> p (e t)")
        S.add("dve", lambda e: e.tensor_tensor(MM[:], E1[:], E2[:], ALU.add), [E1[:], E2[:]], [MM[:]])
        NC_ = 8 * NTL
        S.add("pe", lambda e: e.matmul(pg[0][:, 0:NC_], ltm[:], f2(MM), start=True, stop=True), [ltm[:], MM[:]], [pg[0][:, 0:NC_]])
        S.add("pe", lambda e: e.matmul(pg[1][:, 0:NC_], ones[:], f2(MM), start=True, stop=True), [ones[:], MM[:]], [pg[1][:, 0:NC_]])
        S.add("dve", lambda e: e.tensor_copy(f2(within), pg[0][:, 0:NC_]), [pg[0][:, 0:NC_]], [within[:]])
        S.add("act", lambda e: e.copy(f2(tot), pg[1][:, 0:NC_]), [pg[1][:, 0:NC_]], [tot[:]])
        for ex in range(8):
            S.add("dve", lambda e, ex=ex: e.tensor_tensor_scan(incl[:, ex, :], ones[:, 0:NTL], tot[:, ex, :], 0.0, ALU.mult, ALU.add),
                  [ones[:, 0:NTL], tot[:, ex, :]], [incl[:, ex, :]])
        S.add("dve", lambda e: e.tensor_copy(sv[:, :, 0], incl[:, :, NTL - 1]), [incl[:]], [sv[:, :, 0]])
        S.add("dve", lambda e: e.tensor_tensor(cmp_[:, 0:8, :], sv[:, :, 0].unsqueeze(1).to_broadcast([128, 8, 8]),
                                               slot512[:, 0:8].unsqueeze(2).to_broadcast([128, 8, 8]), ALU.is_gt),
              [sv[:, :, 0], slot512[:, 0:8]], [cmp_[:, 0:8, :]])
        S.add("dve", lambda e: e.tensor_reduce(sv[:, :, 1], cmp_[:, 0:8, :].rearrange("p j e -> p e j"), AX.X, ALU.add),
              [cmp_[:, 0:8, :]], [sv[:, :, 1]])
        S.add("dve", lambda e: e.tensor_scalar(sv[:, :, 3], sv[:, :, 1], float(SLOT), None, ALU.mult), [sv[:, :, 1]], [sv[:, :, 3]])
        S.add("dve", lambda e: e.tensor_tensor_scan(sv[:, :, 4], ones[:, 0:8], sv[:, :, 3], 0.0, ALU.mult, ALU.add),
              [ones[:, 0:8], sv[:, :, 3]], [sv[:, :, 4]])
        S.add("dve", lambda e: e.tensor_tensor(sv[:, :, 5], sv[:, :, 4], sv[:, :, 3], ALU.subtract), [sv[:, :, 4], sv[:, :, 3]], [sv[:, :, 5]])
        S.add("dve", lambda e: e.tensor_tensor(cmp_[:], sv[:, :, 4].unsqueeze(1).to_broadcast([128, NSLOT, 8]),
                                               slot512[:, :].unsqueeze(2).to_broadcast([128, NSLOT, 8]), ALU.is_le),
              [sv[:, :, 4], slot512[:]], [cmp_[:]])
        S.add("dve", lambda e: e.tensor_reduce(esl[:], cmp_[:], AX.X, ALU.add), [cmp_[:]], [esl[:]])
        S.add("dve", lambda e: e.tensor_scalar(esl[:], esl[:], 7.0, None, ALU.min), [esl[:]], [esl[:]])
        S.add("dve", lambda e: e.scalar_tensor_tensor(idxf[:], esl[:, :].unsqueeze(2).to_broadcast([128, NSLOT, NG]), float(NG * 128),
                                                      cgp[:, :].unsqueeze(1).to_broadcast([128, NSLOT, NG]), ALU.mult, ALU.add),
              [esl[:], cgp[:]], [idxf[:]])
        S.add("dve", lambda e: e.tensor_copy(idxw[:], idxf[:]), [idxf[:]], [idxw[:]])
        S.add("dve", lambda e: e.tensor_tensor(posall[:], incl[:], tot[:], ALU.subtract), [incl[:], tot[:]], [posall[:]])
        S.add("dve", lambda e: e.tensor_tensor(posall[:], posall[:], within[:], ALU.add), [posall[:], within[:]], [posall[:]])
        S.add("dve", lambda e: e.tensor_tensor(posall[:], posall[:], sv[:, :, 5:6].to_broadcast([128, 8, NTL]), ALU.add),
              [posall[:], sv[:, :, 5:6]], [posall[:]])
        for (Ek, pk, pki) in ((E1, pos1, pos1i), (E2, pos2, pos2i)):
            S.add("dve", lambda e, Ek=Ek: e.tensor_tensor(ptmp[:], posall[:], Ek[:], ALU.mult), [posall[:], Ek[:]], [ptmp[:]])
            S.add("dve", lambda e, pk=pk: e.tensor_reduce(pk[:], ptmp[:, :, :].rearrange("p e t -> p t e"), AX.X, ALU.add), [ptmp[:]], [pk[:]])
            S.add("dve", lambda e, pk=pk, pki=pki: e.tensor_copy(pki[:], pk[:]), [pk[:]], [pki[:]])
        import os
        ESTOP = int(os.environ.get("E_STOP", "9"))
        if os.environ.get("DBG_E"):
            dd = lambda nm, shp, dt=F32: nc.dram_tensor("dbg_" + nm, shp, dt, kind="ExternalOutput").ap()
            S.dma("sp", dd("pos1i", [128, NTL], I32), pos1i[:])
            S.dma("sp", dd("pos2i", [128, NTL], I32), pos2i[:])
            S.dma("sp", dd("idxw", [128, NSLOT, NG], I32), idxw[:])
            S.dma("sp", dd("esl", [128, NSLOT]), esl[:])
            S.dma("sp", dd("sv", [128, 8, 6]), sv[:])
            S.dma("sp", dd("E1", [128, 8, NTL]), E1[:])
            S.dma("sp", dd("E2", [128, 8, NTL]), E2[:])
            S.dma("sp", dd("PW", [128, NTL, 2]), PW[:])
        for tt in range(NTL if ESTOP >= 2 else 0):
            hb = hin4[(tt // 4) % 2][:, tt % 4, :]
            S.dma("sp", hb, hin[tt * 128:(tt + 1) * 128, :])
            for pki in (pos1i, pos2i):
                S.dma("pool", xs, hb,
                      fn=lambda e, hb=hb, pki=pki, tt=tt: e.indirect_dma_start(
                          out=xs, out_offset=bass.IndirectOffsetOnAxis(ap=pki[:, tt:tt + 1], axis=0), in_=hb, in_offset=None),
                      reads=[hb, pki[:, tt:tt + 1]], writes=[xs])
        widx = 0
        cnt = 0
        def slot_prologue(s_):
            xb_ = xT[s_ % 2]
            for ti in range(4):
                hb = hin4[s_ % 2][:, ti, :]
                S.dma("sp", hb, xs[s_ * SLOT + ti * 128:s_ * SLOT + (ti + 1) * 128, :])
                emit_load_transpose(S, hb, ident, pg, xb_[:, :, ti * 128:(ti + 1) * 128])

        if ESTOP >= 3:
            slot_prologue(0)
        for s in range(NSLOT if ESTOP >= 3 else 0):
            xb = xT[s % 2]
            ac = acc[s % 2]
            for gi in range(NSG):
                if gi == 3 and s + 1 < NSLOT:
                    slot_prologue(s + 1)
                slot = widx % NWB
                widx += 1
                for sub in range(2):
                    ixa = idxw[:, s, 2 * gi + sub:2 * gi + sub + 1]
                    for (dst, srcw) in ((wgb[slot], wg_l), (wub[slot], wu_l), (wdb[slot], wd_l)):
                        dst2 = dst[:, sub].rearrange("p a b -> p (a b)")
                        S.dma("pool", dst2, srcw,
                              fn=lambda e, dst2=dst2, srcw=srcw, ixa=ixa: e.indirect_dma_start(
                                  out=dst2, out_offset=None, in_=srcw, in_offset=bass.IndirectOffsetOnAxis(ap=ixa, axis=0)),
                              reads=[srcw, ixa], writes=[dst2])
                ab = aT[cnt % 2]
                for fc in range(G):
                    sub, c = fc // 2, fc % 2
                    i2 = (cnt * G + fc) % 2
                    pgb, pub, sgb = pg[i2], pu[i2], sg[i2]
                    for kc in range(8):
                        S.add("pe", lambda e, pgb=pgb, kc=kc, sub=sub, c=c, slot=slot, xb=xb: e.matmul(
                            pgb[:, :], wgb[slot][:, sub, kc, c * 128:(c + 1) * 128], xb[:, kc, :], start=(kc == 0), stop=(kc == 7)),
                            [wgb[slot][:, sub, kc, c * 128:(c + 1) * 128], xb[:, kc, :]], [pgb[:, :]])
                    for kc in range(8):
                        S.add("pe", lambda e, pub=pub, kc=kc, sub=sub, c=c, slot=slot, xb=xb: e.matmul(
                            pub[:, :], wub[slot][:, sub, kc, c * 128:(c + 1) * 128], xb[:, kc, :], start=(kc == 0), stop=(kc == 7)),
                            [wub[slot][:, sub, kc, c * 128:(c + 1) * 128], xb[:, kc, :]], [pub[:, :]])
                    S.add("act", lambda e, sgb=sgb, pgb=pgb: e.activation(sgb[:], pgb[:, :], AF.Silu), [pgb[:, :]], [sgb[:]])
                    S.add("dve", lambda e, ab=ab, fc=fc, sgb=sgb, pub=pub: e.tensor_tensor(ab[:, fc, :], sgb[:], pub[:, :], ALU.mult),
                          [sgb[:], pub[:, :]], [ab[:, fc, :]])
                for ti in range(4):
                    pdb = pd[(cnt * 4 + ti) % 2]
                    for hd in range(2):
                        for fc in range(G):
                            S.add("pe", lambda e, pdb=pdb, hd=hd, fc=fc, ab=ab, ti=ti, slot=slot: e.matmul(
                                pdb[:, hd * 512:(hd + 1) * 512], ab[:, fc, ti * 128:(ti + 1) * 128],
                                wdb[slot][:, fc // 2, fc % 2, hd * 512:(hd + 1) * 512], start=(fc == 0), stop=(fc == G - 1)),
                                [ab[:, fc, ti * 128:(ti + 1) * 128], wdb[slot][:, fc // 2, fc % 2, hd * 512:(hd + 1) * 512]],
                                [pdb[:, hd * 512:(hd + 1) * 512]])
                    a_t = ac[:, ti, :]
                    if gi == 0:
                        S.add("act", lambda e, a_t=a_t, pdb=pdb: e.copy(a_t, pdb[:]), [pdb[:]], [a_t])
                    else:
                        S.add("dve", lambda e, a_t=a_t, pdb=pdb: e.tensor_tensor(a_t, pdb[:], a_t, ALU.add), [pdb[:], a_t], [a_t])
                cnt += 1
            S.dma("sp", ys[s * SLOT:(s + 1) * SLOT, :].rearrange("(t p) d -> p t d", p=128), ac[:])
        for g4 in range(0, NTL if ESTOP >= 4 else 0, 4):
            nn = min(4, NTL - g4)
            hq = hin4[(g4 // 4) % 2]
            for k in range(nn):
                tt = g4 + k
                S.dma("sp", hq[:, k, :], hin[tt * 128:(tt + 1) * 128, :])
                for (ab_, pki) in ((acc[0], pos1i), (acc[1], pos2i)):
                    yt = ab_[:, k, :]
                    S.dma("pool", yt, ys,
                          fn=lambda e, yt=yt, pki=pki, tt=tt: e.indirect_dma_start(
                              out=yt, out_offset=None, in_=ys, in_offset=bass.IndirectOffsetOnAxis(ap=pki[:, tt:tt + 1], axis=0)),
                          reads=[ys, pki[:, tt:tt + 1]], writes=[yt])
            for k in range(nn):
                S.add("act", lambda e, hq=hq, k=k: e.mul(hq[:, k, :], hq[:, k, :], ALPHA), [hq[:, k, :]], [hq[:, k, :]])
            for j, ab_ in enumerate((acc[0], acc[1])):
                for k in range(nn):
                    tt = g4 + k
                    S.add("dve", lambda e, hq=hq, k=k, ab_=ab_, tt=tt, j=j: e.scalar_tensor_tensor(
                        hq[:, k, :], ab_[:, k, :], PW[:, tt, j:j + 1], hq[:, k, :], ALU.mult, ALU.add),
                        [ab_[:, k, :], PW[:, tt, j:j + 1], hq[:, k, :]], [hq[:, k, :]])
            emit_ln_group(S, [(hq[:, k, :], acc[0][:, k, :]) for k in range(nn)], gb, bb, stg, mvg, scg)
            for k in range(nn):
                tt = g4 + k
                S.dma("sp", hout[tt * 128:(tt + 1) * 128, :], acc[0][:, k, :])
    S.barrier()


def ffn_stream_phase(S, nc, T, hin, hout, wg, wu, wd, F, lng, lnb, ident_d, tag):
    TB = min(512, T)
    NB = T // TB
    TPB = TB // 128
    GW = 256
    NG = F // GW
    assert NG * GW == F
    NWB = 3
    with contextlib.ExitStack() as es:
        cx = Ctx(nc, es, tag)
        ident = cx.sb("ident", [128, 128], F32)
        gb = cx.sb("gb", [128, 1024], F32)
        bb = cx.sb("bb", [128, 1024], F32)
        hin4 = [cx.sb("hin4_%d" % i, [128, TPB, 1024], F32) for i in range(2)]
        xT = [cx.sb("xT%d" % i, [128, 8, TB], BF16) for i in range(2)]
        acc = [cx.sb("acc%d" % i, [128, TPB, 1024], F32) for i in range(2)]
        wgb = [cx.sb("wg%d" % i, [128, 8, GW], BF16) for i in range(NWB)]
        wub = [cx.sb("wu%d" % i, [128, 8, GW], BF16) for i in range(NWB)]
        wdb = [cx.sb("wd%d" % i, [128, 2, 1024], BF16) for i in range(NWB)]
        sg = [cx.sb("sg%d" % i, [128, TB], F32) for i in range(2)]
        aT = [cx.sb("aT%d" % i, [128, 2, TB], BF16) for i in range(2)]
        stg = cx.sb("stg", [128, 4, 2, 6], F32)
        mvg = cx.sb("mvg", [128, 4, 2], F32)
        scg = cx.sb("scg", [128, 4, 2], F32)
        pg = [cx.ps("pg%d" % i, [128, 512], F32) for i in range(2)]
        pu = [cx.ps("pu%d" % i, [128, 512], F32) for i in range(2)]
        pd = [cx.ps("pd%d" % i, [128, 1024], F32) for i in range(2)]
        S.dma("sp", ident[:], ident_d)
        S.dma("sp", gb[:], lng.to_broadcast([128, 1024]))
        S.dma("sp", bb[:], lnb.to_broadcast([128, 1024]))

        def prologue(b_):
            for ti in range(TPB):
                hb = hin4[b_ % 2][:, ti, :]
                S.dma("sp", hb, hin[b_ * TB + ti * 128:b_ * TB + (ti + 1) * 128, :])
                emit_load_transpose(S, hb, ident, pg, xT[b_ % 2][:, :, ti * 128:(ti + 1) * 128])

        def epilogue(b_):
            hq, ac = hin4[b_ % 2], acc[b_ % 2]
            for ti in range(TPB):
                S.add("dve", lambda e, hq=hq, ac=ac, ti=ti: e.scalar_tensor_tensor(hq[:, ti, :], hq[:, ti, :], ALPHA, ac[:, ti, :], ALU.mult, ALU.add),
                      [hq[:, ti, :], ac[:, ti, :]], [hq[:, ti, :]])
            emit_ln_group(S, [(hq[:, ti, :], ac[:, ti, :]) for ti in range(TPB)], gb, bb, stg, mvg, scg)
            for ti in range(TPB):
                S.dma("sp", hout[b_ * TB + ti * 128:b_ * TB + (ti + 1) * 128, :], ac[:, ti, :])

        widx = 0
        cnt = 0
        pending = [None]
        prologue(0)
        for blk_ in range(NB):
            xb = xT[blk_ % 2]
            ac = acc[blk_ % 2]
            for gi in range(NG):
                if gi == NG // 2 and blk_ + 1 < NB:
                    prologue(blk_ + 1)
                if gi == 2 and blk_ > 0:
                    epilogue(blk_ - 1)
                slot = widx % NWB
                widx += 1
                f0 = gi * GW
                S.dma("pool", wgb[slot][:], wg[0, :, f0:f0 + GW].rearrange("(k p) f -> p k f", p=128))
                S.dma("pool", wub[slot][:], wu[0, :, f0:f0 + GW].rearrange("(k p) f -> p k f", p=128))
                S.dma("pool", wdb[slot][:], wd[0, f0:f0 + GW, :].rearrange("(c p) d -> p c d", p=128))
                ab = aT[cnt % 2]
                for fc in range(2):
                    i2 = (cnt * 2 + fc) % 2
                    pgb, pub, sgb = pg[i2], pu[i2], sg[i2]
                    for kc in range(8):
                        S.add("pe", lambda e, pgb=pgb, kc=kc, fc=fc, slot=slot, xb=xb: e.matmul(
                            pgb[:, 0:TB], wgb[slot][:, kc, fc * 128:(fc + 1) * 128], xb[:, kc, :], start=(kc == 0), stop=(kc == 7)),
                            [wgb[slot][:, kc, fc * 128:(fc + 1) * 128], xb[:, kc, :]], [pgb[:, 0:TB]])
                    for kc in range(8):
                        S.add("pe", lambda e, pub=pub, kc=kc, fc=fc, slot=slot, xb=xb: e.matmul(
                            pub[:, 0:TB], wub[slot][:, kc, fc * 128:(fc + 1) * 128], xb[:, kc, :], start=(kc == 0), stop=(kc == 7)),
                            [wub[slot][:, kc, fc * 128:(fc + 1) * 128], xb[:, kc, :]], [pub[:, 0:TB]])
                    S.add("act", lambda e, sgb=sgb, pgb=pgb: e.activation(sgb[:], pgb[:, 0:TB], AF.Silu), [pgb[:, 0:TB]], [sgb[:]])
                    S.add("dve", lambda e, ab=ab, fc=fc, sgb=sgb, pub=pub: e.tensor_tensor(ab[:, fc, :], sgb[:], pub[:, 0:TB], ALU.mult),
                          [sgb[:], pub[:, 0:TB]], [ab[:, fc, :]])

                def down_part(ab=ab, slot=slot, gi=gi, ac=ac, cnt=cnt):
                    for ti in range(TPB):
                        pdb = pd[(cnt * TPB + ti) % 2]
                        for hd in range(2):
                            for fc in range(2):
                                S.add("pe", lambda e, pdb=pdb, hd=hd, fc=fc, ab=ab, ti=ti, slot=slot: e.matmul(
                                    pdb[:, hd * 512:(hd + 1) * 512], ab[:, fc, ti * 128:(ti + 1) * 128],
                                    wdb[slot][:, fc, hd * 512:(hd + 1) * 512], start=(fc == 0), stop=(fc == 1)),
                                    [ab[:, fc, ti * 128:(ti + 1) * 128], wdb[slot][:, fc, hd * 512:(hd + 1) * 512]],
                                    [pdb[:, hd * 512:(hd + 1) * 512]])
                        a_t = ac[:, ti, :]
                        if gi == 0:
                            S.add("act", lambda e, a_t=a_t, pdb=pdb: e.copy(a_t, pdb[:]), [pdb[:]], [a_t])
                        else:
                            S.add("dve", lambda e, a_t=a_t, pdb=pdb: e.tensor_tensor(a_t, pdb[:], a_t, ALU.add), [pdb[:], a_t], [a_t])
                if pending[0] is not None:
                    pending[0]()
                pending[0] = down_part
                if gi == NG - 1:
                    pending[0]()
                    pending[0] = None
                cnt += 1
        epilogue(NB - 1)
    S.barrier()
```

```python
import contextlib
import numpy as np
import concourse.bass as bass
import concourse.mybir as mybir
from concourse.bass_utils import run_bass_kernel_spmd

F32 = mybir.dt.float32
BF16 = mybir.dt.bfloat16
AF = mybir.ActivationFunctionType
ALU = mybir.AluOpType
AX = mybir.AxisListType

D = 1024
ALPHA = (2.0 * 2) ** 0.25
LN_EPS = 1e-5
GN_EPS = 64e-5
N_CORES = 8


def _dsize(dt):
    if dt == F32:
        return 4
    if dt == BF16:
        return 2
    s = str(dt)
    if "64" in s:
        return 8
    if "32" in s:
        return 4
    if "16" in s:
        return 2
    return 1


class Op:
    __slots__ = ("eng", "fn", "deps", "stream", "inc", "count", "isdma")

    def __init__(self, eng, fn, stream, isdma):
        self.eng = eng
        self.fn = fn
        self.deps = []
        self.stream = stream
        self.inc = False
        self.count = 0
        self.isdma = isdma


def region(ap):
    name = ap.name
    space = str(ap.space)
    off = int(ap.offset)
    pat = ap.ap
    esz = _dsize(ap.dtype)
    if "SB" in space or "PSUM" in space:
        shp = ap.tensor.shape
        rowsize = 1
        for s in list(shp)[1:]:
            rowsize *= int(s)
        p0 = off // rowsize
        f0 = off % rowsize
        npart = pat[0][1]
        f1 = f0 + 1
        for st, c in pat[1:]:
            if st > 0:
                f1 += (c - 1) * st
        if "PSUM" in space:
            b0 = (f0 * esz) // 2048
            b1 = (f1 * esz + 2047) // 2048
            return (name, 0, 128, b0 * 2048, b1 * 2048, True)
        return (name, p0, p0 + npart, f0 * esz, f1 * esz)
    lo = off
    hi = off + 1
    for st, c in pat:
        if st > 0:
            hi += (c - 1) * st
        elif st < 0:
            lo += (c - 1) * st
    return (name, 0, 1, lo * esz, hi * esz)


def _ovl(a, b):
    return a[1] < b[2] and b[1] < a[2] and a[3] < b[4] and b[3] < a[4]


def _contains(a, b):
    return a[1] <= b[1] and a[2] >= b[2] and a[3] <= b[3] and a[4] >= b[4]


class Sched:
    ENGS = ["pe", "act", "dve", "pool", "sp"]

    def __init__(self, nc, n_dma=32):
        self.nc = nc
        self.ops = {e: [] for e in self.ENGS}
        self.rec = {}
        self.n_dma = n_dma
        self.dma_last = [None] * n_dma
        self.dma_ops = [[] for _ in range(n_dma)]
        self.dma_rr = 0

    def _track(self, op, reads, writes):
        deps = op.deps
        st = op.stream
        rregs = [region(ap) for ap in reads]
        wregs = [region(ap) for ap in writes]
        for rg in rregs:
            lst = self.rec.get(rg[0])
            if lst:
                ps = len(rg) > 5
                for r in lst:
                    if (r[2] or (ps and r[1].stream != st)) and _ovl(r[0], rg):
                        p = r[1]
                        if p.isdma and p.stream == st:
                            continue
                        deps.append(p)
        for rg in wregs:
            lst = self.rec.get(rg[0])
            if lst:
                for r in lst:
                    if _ovl(r[0], rg):
                        p = r[1]
                        if p.stream == st:
                            continue
                        deps.append(p)
        for p in deps:
            p.inc = True
        for rg in rregs:
            lst = self.rec.setdefault(rg[0], [])
            lst[:] = [r for r in lst if not ((not r[2]) and r[1].stream == st and _contains(rg, r[0]))]
            lst.append([rg, op, False])
        for rg in wregs:
            lst = self.rec.setdefault(rg[0], [])
            lst[:] = [r for r in lst if not _contains(rg, r[0])]
            lst.append([rg, op, True])

    def add(self, eng, fn, reads=(), writes=()):
        op = Op(eng, fn, eng, False)
        self._track(op, reads, writes)
        self.ops[eng].append(op)
        return op

    def _pick_sem(self, eng):
        half = self.n_dma // 2
        if eng == "pool":
            self.dma_rr_sw = (getattr(self, "dma_rr_sw", -1) + 1) % half
            return half + self.dma_rr_sw
        self.dma_rr = (self.dma_rr + 1) % half
        return self.dma_rr

    def dma(self, eng, out, in_, sem=None, fn=None, reads=None, writes=None, **kw):
        if sem is None:
            sem = self._pick_sem(eng)
        stream = "dma%d" % sem
        if fn is None:
            fn = (lambda e, o=out, i=in_, k=kw: e.dma_start(out=o, in_=i, **k))
        op = Op(eng, fn, stream, True)
        op.inc = True
        prev = self.dma_last[sem]
        if prev is not None:
            op.deps.append(prev)
        self._track(op, [in_] if reads is None else reads, [out] if writes is None else writes)
        self.dma_last[sem] = op
        self.dma_ops[sem].append(op)
        self.ops[eng].append(op)
        return op

    def barrier(self):
        lasts = []
        for e in ["pe", "act", "dve", "pool"]:
            for op in reversed(self.ops[e]):
                if not op.isdma and op.fn is not None:
                    lasts.append(op)
                    break
        for s in range(self.n_dma):
            if self.dma_last[s] is not None:
                lasts.append(self.dma_last[s])
        for p in lasts:
            p.inc = True
        for e in self.ENGS:
            op = Op(e, None, e, False)
            op.deps = list(lasts)
            self.ops[e].append(op)
        self.rec = {}

    def emit(self):
        nc = self.nc
        for e in ["pe", "act", "dve", "pool"]:
            c = 0
            for op in self.ops[e]:
                if op.isdma or op.fn is None:
                    continue
                if op.inc:
                    c += 1
                op.count = c
        for s in range(self.n_dma):
            c = 0
            for op in self.dma_ops[s]:
                c += 16
                op.count = c
        with contextlib.ExitStack() as es:
            sems = {}
            for e in ["pe", "act", "dve", "pool"]:
                sems[e] = es.enter_context(nc.semaphore("s_" + e))
            for s in range(self.n_dma):
                if self.dma_ops[s]:
                    sems["dma%d" % s] = es.enter_context(nc.semaphore("s_dma%d" % s))
            block = es.enter_context(nc.Block())

            def run(engname):
                def body(engine):
                    known = {}
                    for op in self.ops[engname]:
                        need = {}
                        for p in op.deps:
                            v = p.count
                            if v > need.get(p.stream, 0):
                                need[p.stream] = v
                        for stn, v in need.items():
                            if v > known.get(stn, 0):
                                engine.wait_ge(sems[stn], v)
                                known[stn] = v
                        if op.fn is None:
                            continue
                        ins = op.fn(engine)
                        if op.inc:
                            ins.then_inc(sems[op.stream], 16 if op.isdma else 1)
                return body

            block.tensor(run("pe"))
            block.scalar(run("act"))
            block.vector(run("dve"))
            block.gpsimd(run("pool"))
            block.sync(run("sp"))


class Ctx:
    def __init__(self, nc, es, tag):
        self.nc = nc
        self.es = es
        self.tag = tag

    def sb(self, name, shape, dt):
        return self.es.enter_context(self.nc.sbuf_tensor(name + self.tag, list(shape), dt))

    def ps(self, name, shape, dt):
        return self.es.enter_context(self.nc.psum_tensor(name + self.tag, list(shape), dt))


def emit_ln(S, cx, pre, o, gb, bb, st, mv, sc, eps=LN_EPS):
    for c in range(2):
        S.add("dve", lambda e, c=c: e.bn_stats(st[:, c, :], pre[:, c * 512:(c + 1) * 512]),
              [pre[:, c * 512:(c + 1) * 512]], [st[:, c, :]])
    S.add("dve", lambda e: e.bn_aggr(mv[:], st[:]), [st[:]], [mv[:]])
    S.add("act", lambda e: e.activation(sc[:, 0:1], mv[:, 1:2], AF.Sqrt, bias=eps), [mv[:, 1:2]], [sc[:, 0:1]])
    S.add("dve", lambda e: e.reciprocal(sc[:, 0:1], sc[:, 0:1]), [sc[:, 0:1]], [sc[:, 0:1]])
    S.add("dve", lambda e: e.scalar_tensor_tensor(sc[:, 1:2], mv[:, 0:1], -1.0, sc[:, 0:1], ALU.mult, ALU.mult),
          [mv[:, 0:1], sc[:, 0:1]], [sc[:, 1:2]])
    S.add("act", lambda e: e.activation(o, pre, AF.Identity, bias=sc[:, 1:2], scale=sc[:, 0:1]),
          [pre, sc[:]], [o])
    S.add("dve", lambda e: e.tensor_tensor(o, o, gb[:], ALU.mult), [o, gb[:]], [o])
    S.add("pool", lambda e: e.tensor_tensor(o, o, bb[:], ALU.add), [o, bb[:]], [o])


def emit_ln_group(S, pairs, gb, bb, stg, mvg, scg, eps=LN_EPS):
    n = len(pairs)
    for k, (pre, o) in enumerate(pairs):
        for c in range(2):
            S.add("dve", lambda e, k=k, c=c, pre=pre: e.bn_stats(stg[:, k, c, :], pre[:, c * 512:(c + 1) * 512]),
                  [pre[:, c * 512:(c + 1) * 512]], [stg[:, k, c, :]])
    for k in range(n):
        S.add("dve", lambda e, k=k: e.bn_aggr(mvg[:, k, :], stg[:, k, :, :]), [stg[:, k, :, :]], [mvg[:, k, :]])
    S.add("act", lambda e: e.activation(scg[:, 0:n, 0], mvg[:, 0:n, 1], AF.Sqrt, bias=eps), [mvg[:, 0:n, :]], [scg[:, 0:n, 0]])
    S.add("dve", lambda e: e.reciprocal(scg[:, 0:n, 0], scg[:, 0:n, 0]), [scg[:, 0:n, 0]], [scg[:, 0:n, 0]])
    S.add("dve", lambda e: e.scalar_tensor_tensor(scg[:, 0:n, 1], mvg[:, 0:n, 0], -1.0, scg[:, 0:n, 0], ALU.mult, ALU.mult),
          [mvg[:, 0:n, :], scg[:, 0:n, 0]], [scg[:, 0:n, 1]])
    for k, (pre, o) in enumerate(pairs):
        S.add("act", lambda e, k=k, pre=pre, o=o: e.activation(o, pre, AF.Identity, bias=scg[:, k, 1:2], scale=scg[:, k, 0:1]),
              [pre, scg[:, k, :]], [o])
    for k, (pre, o) in enumerate(pairs):
        S.add("dve", lambda e, o=o: e.tensor_tensor(o, o, gb[:], ALU.mult), [o, gb[:]], [o])
    for k, (pre, o) in enumerate(pairs):
        S.add("dve", lambda e, o=o: e.tensor_tensor(o, o, bb[:], ALU.add), [o, bb[:]], [o])


def emit_load_transpose(S, hin_tile, ident, pbanks, xT_dst, xTf_dst=None):
    for b in range(2):
        pb = pbanks[b]
        for j in range(4):
            kc = b * 4 + j
            S.add("pe", lambda e, pb=pb, j=j, kc=kc: e.transpose(pb[:, j * 128:(j + 1) * 128],
                                                               hin_tile[:, kc * 128:(kc + 1) * 128], ident[:]),
                  [hin_tile[:, kc * 128:(kc + 1) * 128], ident[:]], [pb[:, j * 128:(j + 1) * 128]])
        src = pb[:, 0:512].rearrange("p (j t) -> p j t", j=4)
        dst = xT_dst[:, b * 4:(b + 1) * 4, :]
        S.add("act", lambda e, dst=dst, src=src: e.copy(dst, src), [pb[:, 0:512]], [dst])
        if xTf_dst is not None:
            dstf = xTf_dst[:, b * 4:(b + 1) * 4, :]
            S.add("dve", lambda e, dstf=dstf, src=src: e.tensor_copy(dstf, src), [pb[:, 0:512]], [dstf])


def ffn_phase(S, nc, T, hin, hout, wg, wu, wd, F, G, NE, rw, lng, lnb, ident_d, tag):
    HALF = min(T, 2048)
    NH = T // HALF
    TB = min(512, HALF)
    NB = HALF // TB
    NT = HALF // 128
    TPB = TB // 128
    GW = G * 128
    NG = F // GW
    assert NG * GW == F
    with contextlib.ExitStack() as es:
        cx = Ctx(nc, es, tag)
        ident = cx.sb("ident", [128, 128], F32)
        xT = cx.sb("xT", [128, 8, HALF], BF16)
        acc = cx.sb("acc", [128, NT, 1024], F32)
        wgb = [cx.sb("wg%d" % i, [128, 8, GW], BF16) for i in range(2)]
        wub = [cx.sb("wu%d" % i, [128, 8, GW], BF16) for i in range(2)]
        wdb = [cx.sb("wd%d" % i, [128, G, 1024], BF16) for i in range(2)]
        hinb = [cx.sb("hin%d" % i, [128, 1024], F32) for i in range(2)]
        sg = [cx.sb("sg%d" % i, [128, TB], F32) for i in range(2)]
        aT = [cx.sb("aT%d" % i, [128, G, TB], BF16) for i in range(2)]
        gb = cx.sb("gb", [128, 1024], F32)
        bb = cx.sb("bb", [128, 1024], F32)
        st = cx.sb("st", [128, 2, 6], F32)
        mv = cx.sb("mv", [128, 2], F32)
        sc = cx.sb("sc", [128, 2], F32)
        ob = [cx.sb("ob%d" % i, [128, 1024], F32) for i in range(4)]
        stg = cx.sb("stg", [128, 4, 2, 6], F32)
        mvg = cx.sb("mvg", [128, 4, 2], F32)
        scg = cx.sb("scg", [128, 4, 2], F32)
        pg = [cx.ps("pg%d" % i, [128, 512], F32) for i in range(2)]
        pu = [cx.ps("pu%d" % i, [128, 512], F32) for i in range(2)]
        pd = [cx.ps("pd%d" % i, [128, 1024], F32) for i in range(2)]
        if rw is not None:
            rwb = cx.sb("rwb", [128, 8, 8], F32)
            xTf = cx.sb("xTf", [128, 8, 128], F32)
            cw = cx.sb("cw", [128, NT, 8], F32)
            lg = cx.sb("lg", [128, 8], F32)
            lg2 = cx.sb("lg2", [128, 8], F32)
            eq1 = cx.sb("eq1", [128, 8], F32)
            eq2 = cx.sb("eq2", [128, 8], F32)
            sm = cx.sb("sm", [128, 8], F32)
            S.dma("sp", rwb[:], rw.rearrange("(k p) e -> p k e", p=128))
        S.dma("sp", ident[:], ident_d)
        S.dma("sp", gb[:], lng.to_broadcast([128, 1024]))
        S.dma("sp", bb[:], lnb.to_broadcast([128, 1024]))

        widx = 0
        for hf in range(NH):
            t0 = hf * HALF
            for tt in range(NT):
                hb = hinb[tt % 2]
                S.dma("sp", hb[:], hin[t0 + tt * 128:t0 + (tt + 1) * 128, :])
                emit_load_transpose(S, hb, ident, pg, xT[:, :, tt * 128:(tt + 1) * 128],
                                    xTf if rw is not None else None)
                a_t = acc[:, tt, :]
                S.add("act", lambda e, a_t=a_t, hb=hb: e.mul(a_t, hb[:], ALPHA), [hb[:]], [a_t])
                if rw is not None:
                    pl = pu[0][:, 0:8]
                    for kc in range(8):
                        S.add("pe", lambda e, kc=kc, pl=pl: e.matmul(pl, xTf[:, kc, :], rwb[:, kc, :],
                                                                   start=(kc == 0), stop=(kc == 7)),
                              [xTf[:, kc, :], rwb[:, kc, :]], [pl])
                    S.add("dve", lambda e, pl=pl: e.tensor_copy(lg[:], pl), [pl], [lg[:]])
                    S.add("dve", lambda e: e.tensor_reduce(sm[:, 0:1], lg[:], AX.X, ALU.max), [lg[:]], [sm[:, 0:1]])
                    S.add("dve", lambda e: e.tensor_scalar(eq1[:], lg[:], sm[:, 0:1], None, ALU.is_equal),
                          [lg[:], sm[:, 0:1]], [eq1[:]])
                    S.add("dve", lambda e: e.scalar_tensor_tensor(lg2[:], eq1[:], -1e30, lg[:], ALU.mult, ALU.add),
                          [eq1[:], lg[:]], [lg2[:]])
                    S.add("dve", lambda e: e.tensor_reduce(sm[:, 1:2], lg2[:], AX.X, ALU.max), [lg2[:]], [sm[:, 1:2]])
                    S.add("dve", lambda e: e.tensor_scalar(eq2[:], lg2[:], sm[:, 1:2], None, ALU.is_equal),
                          [lg2[:], sm[:, 1:2]], [eq2[:]])
                    S.add("dve", lambda e: e.tensor_tensor(sm[:, 2:3], sm[:, 1:2], sm[:, 0:1], ALU.subtract),
                          [sm[:, 0:2]], [sm[:, 2:3]])
                    S.add("act", lambda e: e.activation(sm[:, 3:4], sm[:, 2:3], AF.Exp), [sm[:, 2:3]], [sm[:, 3:4]])
                    S.add("dve", lambda e: e.tensor_scalar(sm[:, 4:5], sm[:, 3:4], 1.0, None, ALU.add),
                          [sm[:, 3:4]], [sm[:, 4:5]])
                    S.add("dve", lambda e: e.reciprocal(sm[:, 5:6], sm[:, 4:5]), [sm[:, 4:5]], [sm[:, 5:6]])
                    S.add("dve", lambda e: e.tensor_tensor(sm[:, 6:7], sm[:, 3:4], sm[:, 5:6], ALU.mult),
                          [sm[:, 3:4], sm[:, 5:6]], [sm[:, 6:7]])
                    cwt = cw[:, tt, :]
                    S.add("dve", lambda e, cwt=cwt: e.tensor_scalar(cwt, eq1[:], sm[:, 5:6], None, ALU.mult),
                          [eq1[:], sm[:, 5:6]], [cwt])
                    S.add("dve", lambda e, cwt=cwt: e.scalar_tensor_tensor(cwt, eq2[:], sm[:, 6:7], cwt, ALU.mult, ALU.add),
                          [eq2[:], sm[:, 6:7], cwt], [cwt])
            cnt = 0
            for ex in range(NE):
                for gi in range(NG):
                    slot = widx % 2
                    widx += 1
                    f0 = gi * GW
                    S.dma("pool", wgb[slot][:], wg[ex, :, f0:f0 + GW].rearrange("(k p) f -> p k f", p=128))
                    S.dma("pool", wub[slot][:], wu[ex, :, f0:f0 + GW].rearrange("(k p) f -> p k f", p=128))
                    S.dma("pool", wdb[slot][:], wd[ex, f0:f0 + GW, :].rearrange("(c p) d -> p c d", p=128))
                    for blk in range(NB):
                        c0 = blk * TB
                        ab = aT[cnt % 2]
                        for fc in range(G):
                            i2 = (cnt * G + fc) % 2
                            pgb, pub, sgb = pg[i2], pu[i2], sg[i2]
                            for kc in range(8):
                                S.add("pe", lambda e, pgb=pgb, kc=kc, fc=fc, slot=slot, c0=c0: e.matmul(
                                    pgb[:, 0:TB], wgb[slot][:, kc, fc * 128:(fc + 1) * 128], xT[:, kc, c0:c0 + TB],
                                    start=(kc == 0), stop=(kc == 7)),
                                    [wgb[slot][:, kc, fc * 128:(fc + 1) * 128], xT[:, kc, c0:c0 + TB]], [pgb[:, 0:TB]])
                            for kc in range(8):
                                S.add("pe", lambda e, pub=pub, kc=kc, fc=fc, slot=slot, c0=c0: e.matmul(
                                    pub[:, 0:TB], wub[slot][:, kc, fc * 128:(fc + 1) * 128], xT[:, kc, c0:c0 + TB],
                                    start=(kc == 0), stop=(kc == 7)),
                                    [wub[slot][:, kc, fc * 128:(fc + 1) * 128], xT[:, kc, c0:c0 + TB]], [pub[:, 0:TB]])
                            S.add("act", lambda e, sgb=sgb, pgb=pgb: e.activation(sgb[:], pgb[:, 0:TB], AF.Silu),
                                  [pgb[:, 0:TB]], [sgb[:]])
                            S.add("dve", lambda e, ab=ab, fc=fc, sgb=sgb, pub=pub: e.tensor_tensor(
                                ab[:, fc, :], sgb[:], pub[:, 0:TB], ALU.mult), [sgb[:], pub[:, 0:TB]], [ab[:, fc, :]])
                        for ti in range(TPB):
                            tt = blk * TPB + ti
                            pdb = pd[(cnt * TPB + ti) % 2]
                            for hd in range(2):
                                for fc in range(G):
                                    S.add("pe", lambda e, pdb=pdb, hd=hd, fc=fc, ab=ab, ti=ti, slot=slot: e.matmul(
                                        pdb[:, hd * 512:(hd + 1) * 512], ab[:, fc, ti * 128:(ti + 1) * 128],
                                        wdb[slot][:, fc, hd * 512:(hd + 1) * 512], start=(fc == 0), stop=(fc == G - 1)),
                                        [ab[:, fc, ti * 128:(ti + 1) * 128], wdb[slot][:, fc, hd * 512:(hd + 1) * 512]],
                                        [pdb[:, hd * 512:(hd + 1) * 512]])
                            a_t = acc[:, tt, :]
                            if rw is not None:
                                cws = cw[:, tt, ex:ex + 1]
                                S.add("dve", lambda e, a_t=a_t, pdb=pdb, cws=cws: e.scalar_tensor_tensor(
                                    a_t, pdb[:], cws, a_t, ALU.mult, ALU.add), [pdb[:], cws, a_t], [a_t])
                            else:
                                S.add("dve", lambda e, a_t=a_t, pdb=pdb: e.tensor_tensor(a_t, pdb[:], a_t, ALU.add),
                                      [pdb[:], a_t], [a_t])
                        cnt += 1
            for t4 in range(0, NT, 4):
                nn = min(4, NT - t4)
                emit_ln_group(S, [(acc[:, t4 + k, :], ob[k][:]) for k in range(nn)], gb, bb, stg, mvg, scg)
                for k in range(nn):
                    S.dma("sp", hout[t0 + (t4 + k) * 128:t0 + (t4 + k + 1) * 128, :], ob[k][:])
    S.barrier()


def conv_phase(S, nc, T, hin, hout, w_in, conv_w, w_out, lng, lnb, ident_d, tag):
    TB = min(512, T)
    NB = T // TB
    TPB = TB // 128
    with contextlib.ExitStack() as es:
        cx = Ctx(nc, es, tag)
        ident = cx.sb("ident", [128, 128], F32)
        win = cx.sb("win", [128, 8, 3072], BF16)
        wout = cx.sb("wout", [128, 8, 1024], BF16)
        cwt = cx.sb("cwt", [128, 8, 3], F32)
        xT = [cx.sb("xT%d" % i, [128, 8, TB], BF16) for i in range(2)]
        hinb = [cx.sb("hin%d" % i, [128, TPB, 1024], F32) for i in range(2)]
        up = [cx.sb("up%d" % i, [128, TB + 2], F32) for i in range(8)]
        gcs = [cx.sb("gcs%d" % i, [128, TB], F32) for i in range(2)]
        cv = [cx.sb("cv%d" % i, [128, TB], F32) for i in range(2)]
        vT = [cx.sb("vT%d" % i, [128, 8, TB], BF16) for i in range(2)]
        pre = [cx.sb("pre%d" % i, [128, 1024], F32) for i in range(4)]
        stg = cx.sb("stg", [128, 4, 2, 6], F32)
        mvg = cx.sb("mvg", [128, 4, 2], F32)
        scg = cx.sb("scg", [128, 4, 2], F32)
        gb = cx.sb("gb", [128, 1024], F32)
        bb = cx.sb("bb", [128, 1024], F32)
        st = cx.sb("st", [128, 2, 6], F32)
        mv = cx.sb("mv", [128, 2], F32)
        sc = cx.sb("sc", [128, 2], F32)
        ob = [cx.sb("ob%d" % i, [128, 1024], F32) for i in range(4)]
        pA = [cx.ps("pA%d" % i, [128, 512], F32) for i in range(2)]
        pB = [cx.ps("pB%d" % i, [128, 512], F32) for i in range(2)]
        pC = [cx.ps("pC%d" % i, [128, 512], F32) for i in range(2)]
        pO = cx.ps("pO", [128, 1024], F32)

        S.dma("sp", ident[:], ident_d)
        S.dma("sp", gb[:], lng.to_broadcast([128, 1024]))
        S.dma("sp", bb[:], lnb.to_broadcast([128, 1024]))
        S.dma("sp", cwt[:], conv_w)
        for j in range(3):
            S.dma("pool", win[:, :, j * 1024:(j + 1) * 1024],
                  w_in[:, j * 1024:(j + 1) * 1024].rearrange("(k p) f -> p k f", p=128))
        S.dma("pool", wout[:], w_out.rearrange("(k p) f -> p k f", p=128))
        for c in range(8):
            S.add("pool", lambda e, c=c: e.memset(up[c][:, 0:2], 0.0), [], [up[c][:, 0:2]])

        for blk in range(NB):
            t0 = blk * TB
            hb = hinb[blk % 2]
            xb = xT[blk % 2]
            vb = vT[blk % 2]
            for ti in range(TPB):
                S.dma("sp", hb[:, ti, :], hin[t0 + ti * 128:t0 + (ti + 1) * 128, :])
                emit_load_transpose(S, hb[:, ti, :], ident, pA, xb[:, :, ti * 128:(ti + 1) * 128])
            for c in range(8):
                i2 = c % 2
                for (pp, col0) in ((pA[i2], 0), (pB[i2], 1024), (pC[i2], 2048)):
                    for kc in range(8):
                        S.add("pe", lambda e, pp=pp, col0=col0, kc=kc, c=c, xb=xb: e.matmul(
                            pp[:, 0:TB], win[:, kc, col0 + c * 128:col0 + (c + 1) * 128], xb[:, kc, :],
                            start=(kc == 0), stop=(kc == 7)),
                            [win[:, kc, col0 + c * 128:col0 + (c + 1) * 128], xb[:, kc, :]], [pp[:, 0:TB]])
                g_s = gcs[i2]
                upc = up[c]
                cvb = cv[i2]
                if blk > 0:
                    S.add("pool", lambda e, upc=upc: e.tensor_copy(upc[:, 0:2], upc[:, TB:TB + 2]),
                          [upc[:, TB:TB + 2]], [upc[:, 0:2]])
                S.add("act", lambda e, g_s=g_s, i2=i2: e.copy(g_s[:], pB[i2][:, 0:TB]), [pB[i2][:, 0:TB]], [g_s[:]])
                S.add("dve", lambda e, upc=upc, g_s=g_s, i2=i2: e.tensor_tensor(upc[:, 2:TB + 2], g_s[:], pC[i2][:, 0:TB], ALU.mult),
                      [g_s[:], pC[i2][:, 0:TB]], [upc[:, 2:TB + 2]])
                S.add("act", lambda e, cvb=cvb, upc=upc, c=c: e.activation(cvb[:], upc[:, 0:TB], AF.Copy, scale=cwt[:, c, 0:1]),
                      [upc[:, 0:TB], cwt[:, c, 0:1]], [cvb[:]])
                S.add("dve", lambda e, cvb=cvb, upc=upc, c=c: e.scalar_tensor_tensor(
                    cvb[:], upc[:, 1:TB + 1], cwt[:, c, 1:2], cvb[:], ALU.mult, ALU.add),
                    [upc[:, 1:TB + 1], cwt[:, c, 1:2], cvb[:]], [cvb[:]])
                S.add("dve", lambda e, cvb=cvb, upc=upc, c=c: e.scalar_tensor_tensor(
                    cvb[:], upc[:, 2:TB + 2], cwt[:, c, 2:3], cvb[:], ALU.mult, ALU.add),
                    [upc[:, 2:TB + 2], cwt[:, c, 2:3], cvb[:]], [cvb[:]])
                S.add("dve", lambda e, cvb=cvb, vb=vb, c=c, i2=i2: e.tensor_tensor(vb[:, c, :], cvb[:], pA[i2][:, 0:TB], ALU.mult),
                      [cvb[:], pA[i2][:, 0:TB]], [vb[:, c, :]])
            for ti in range(TPB):
                for hd in range(2):
                    for kc in range(8):
                        S.add("pe", lambda e, hd=hd, kc=kc, ti=ti, vb=vb: e.matmul(
                            pO[:, hd * 512:(hd + 1) * 512], vb[:, kc, ti * 128:(ti + 1) * 128],
                            wout[:, kc, hd * 512:(hd + 1) * 512], start=(kc == 0), stop=(kc == 7)),
                            [vb[:, kc, ti * 128:(ti + 1) * 128], wout[:, kc, hd * 512:(hd + 1) * 512]],
                            [pO[:, hd * 512:(hd + 1) * 512]])
                pr = pre[ti % 4]
                S.add("dve", lambda e, pr=pr, hb=hb, ti=ti: e.scalar_tensor_tensor(
                    pr[:], hb[:, ti, :], ALPHA, pO[:], ALU.mult, ALU.add), [hb[:, ti, :], pO[:]], [pr[:]])
            emit_ln_group(S, [(pre[ti % 4][:], ob[ti % 4][:]) for ti in range(TPB)], gb, bb, stg, mvg, scg)
            for ti in range(TPB):
                S.dma("sp", hout[t0 + ti * 128:t0 + (ti + 1) * 128, :], ob[ti % 4][:])
    S.barrier()


WEIGHT_SPECS = {
    "dense_w_gate": [1, 1024, 2816], "dense_w_up": [1, 1024, 2816], "dense_w_down": [1, 2816, 1024],
    "even_ln_ffn_g": [1, 1024], "even_ln_ffn_b": [1, 1024],
    "odd_w_in": [1024, 3072], "odd_conv_w": [128, 8, 3], "odd_w_out": [1024, 1024],
    "odd_ln_mix_g": [1, 1024], "odd_ln_mix_b": [1, 1024],
    "router_w": [1024, 8],
    "moe_w_gate": [8, 1024, 3584], "moe_w_up": [8, 1024, 3584], "moe_w_down": [8, 3584, 1024],
    "odd_ln_ffn_g": [1, 1024], "odd_ln_ffn_b": [1, 1024],
    "c_ident": [128, 128],
    "even_w_in": [1024, 2560], "even_sinks": [1, 8], "c_bias": [128, 8, 256],
    "rwkv_w_up": [64, 512], "rwkv_a_up": [64, 512], "rwkv_g_up": [128, 512],
    "rwkv_gn_g": [1, 512], "rwkv_gn_b": [1, 512], "even_w_out": [1024, 1024],
    "even_ln_mix_g": [1, 1024], "even_ln_mix_b": [1, 1024],
    "moe_wg_l": [14336, 2048], "moe_wu_l": [14336, 2048], "moe_wd_l": [14336, 2048],
    "c_ltm": [128, 128], "c_slot512": [128, 32], "c_gp": [128, 14],
    "c_rwv": [64, 66], "c_mug": [128, 1], "c_msu": [64, 64], "c_miu": [64, 64], "c_msl": [64, 64],
}
PHASE_INPUTS = {
    "B2": ["dense_w_gate", "dense_w_up", "dense_w_down", "even_ln_ffn_g", "even_ln_ffn_b", "c_ident"],
    "E": ["router_w", "moe_wg_l", "moe_wu_l", "moe_wd_l", "odd_ln_ffn_g", "odd_ln_ffn_b", "c_ident", "c_ltm", "c_slot512", "c_gp"],
    "A1": ["even_w_in", "even_sinks", "c_bias", "c_ident"],
    "A": ["even_w_in", "even_sinks", "c_bias", "c_ident", "rwkv_w_up", "rwkv_a_up", "rwkv_g_up", "rwkv_gn_g", "rwkv_gn_b",
          "even_w_out", "even_ln_mix_g", "even_ln_mix_b", "c_rwv", "c_mug", "c_msu", "c_miu", "c_msl"],
    "B": ["dense_w_gate", "dense_w_up", "dense_w_down", "even_ln_ffn_g", "even_ln_ffn_b", "c_ident"],
    "C": ["odd_w_in", "odd_conv_w", "odd_w_out", "odd_ln_mix_g", "odd_ln_mix_b", "c_ident"],
    "D": ["router_w", "moe_w_gate", "moe_w_up", "moe_w_down", "odd_ln_ffn_g", "odd_ln_ffn_b", "c_ident"],
}


def build(T, phases):
    nc = bass.Bass("TRN2", target_bir_lowering=False)
    x = nc.dram_tensor("x", [T, D], F32, kind="ExternalInput").ap()
    ocols = 512 if phases == ["A1"] else D
    out = nc.dram_tensor("out", [T, ocols], F32, kind="ExternalOutput").ap()
    names = []
    for p in phases:
        for n in PHASE_INPUTS[p]:
            if n not in names:
                names.append(n)
    W = {n: nc.dram_tensor(n, WEIGHT_SPECS[n], F32, kind="ExternalInput").ap() for n in names}
    hs = [x]
    for i in range(len(phases) - 1):
        hs.append(nc.dram_tensor("hscr%d" % i, [T, D], F32).ap())
    hs.append(out)
    S = Sched(nc)
    for i, p in enumerate(phases):
        hi, ho = hs[i], hs[i + 1]
        if p == "A1":
            attn_phase(S, nc, T, hi, ho, W["even_w_in"], W["even_sinks"], W["c_bias"], W["c_ident"], "_A1")
        elif p == "A":
            ya_scr = nc.dram_tensor("ya_scr", [T, 512], F32).ap()
            attn_phase(S, nc, T, hi, ya_scr, W["even_w_in"], W["even_sinks"], W["c_bias"], W["c_ident"], "_A1")
            rwkv_phase(S, nc, T, hi, ya_scr, ho, W, "_A2")
        elif p == "B2":
            ffn_stream_phase(S, nc, T, hi, ho, W["dense_w_gate"], W["dense_w_up"], W["dense_w_down"], 2816,
                             W["even_ln_ffn_g"], W["even_ln_ffn_b"], W["c_ident"], "_B2")
        elif p == "B":
            ffn_phase(S, nc, T, hi, ho, W["dense_w_gate"], W["dense_w_up"], W["dense_w_down"], 2816, 2, 1, None,
                      W["even_ln_ffn_g"], W["even_ln_ffn_b"], W["c_ident"], "_B")
        elif p == "C":
            conv_phase(S, nc, T, hi, ho, W["odd_w_in"], W["odd_conv_w"], W["odd_w_out"],
                       W["odd_ln_mix_g"], W["odd_ln_mix_b"], W["c_ident"], "_C")
        elif p == "E":
            moe_sparse_phase(S, nc, T, hi, ho, W, "_E")
        elif p == "D":
            ffn_phase(S, nc, T, hi, ho, W["moe_w_gate"], W["moe_w_up"], W["moe_w_down"], 3584, 4, 8, W["router_w"],
                      W["odd_ln_ffn_g"], W["odd_ln_ffn_b"], W["c_ident"], "_D")
    S.emit()
    return nc, names


WANT_MOE_LAYOUT = [False]


def _t5_bucket(dist):
    n = np.maximum(dist, 0)
    max_exact = 16
    lr = np.log(np.maximum(n, 1).astype(np.float32) / np.float32(max_exact)) / np.float32(np.log(128 / 16))
    large = max_exact + (lr.astype(np.float32) * np.float32(32 - max_exact)).astype(np.int32)
    large = np.minimum(large, 31)
    return np.where(n < max_exact, n, large)


def host_consts(inputs):
    c = {"c_ident": np.eye(128, dtype=np.float32)}
    s_i = np.arange(64)[:, None]
    t_i = np.arange(64)[None, :]
    c["c_msu"] = (s_i < t_i).astype(np.float32)
    c["c_miu"] = (s_i <= t_i).astype(np.float32)
    c["c_msl"] = (s_i > t_i).astype(np.float32)
    pp_ = np.arange(128)
    c["c_ltm"] = (pp_[:, None] < pp_[None, :]).astype(np.float32)
    c["c_slot512"] = np.tile((np.arange(32) * 512).astype(np.float32)[None, :], (128, 1))
    c["c_gp"] = (np.arange(14)[None, :] * 128 + pp_[:, None]).astype(np.float32)
    if "moe_w_gate" in inputs and WANT_MOE_LAYOUT[0]:
        for src, dstn in (("moe_w_gate", "moe_wg_l"), ("moe_w_up", "moe_wu_l")):
            a = np.asarray(inputs[src], np.float32).reshape(8, 8, 128, 14, 256)
            c[dstn] = np.ascontiguousarray(a.transpose(0, 3, 2, 1, 4)).reshape(14336, 2048)
        a = np.asarray(inputs["moe_w_down"], np.float32).reshape(8, 14, 2, 128, 1024)
        c["moe_wd_l"] = np.ascontiguousarray(a.transpose(0, 1, 3, 2, 4)).reshape(14336, 2048)
    if "rel_bias_table" in inputs:
        tbl = np.asarray(inputs["rel_bias_table"], np.float32)
        qi = np.arange(128)[:, None]
        ki = np.arange(256)[None, :]
        dist = qi + 128 - ki
        valid = (dist >= 0) & (dist < 128)
        g = tbl[_t5_bucket(dist)]
        g = np.where(valid[:, :, None], g, np.float32(-1e30))
        c["c_bias"] = np.ascontiguousarray(np.transpose(g, (0, 2, 1))).astype(np.float32)
    if "rwkv_mu" in inputs:
        mu = np.asarray(inputs["rwkv_mu"], np.float32).reshape(-1)
        cols = [mu[0:1664].reshape(26, 64).T]
        for nm in ("rwkv_w0", "rwkv_a0", "rwkv_k_k", "rwkv_k_a", "rwkv_r_k"):
            cols.append(np.asarray(inputs[nm], np.float32).reshape(8, 64).T)
        c["c_rwv"] = np.ascontiguousarray(np.concatenate(cols, axis=1))
        c["c_mug"] = np.ascontiguousarray(mu[1664:1792].reshape(128, 1))
    return c


def prep_weights(inputs, names):
    WANT_MOE_LAYOUT[0] = "moe_wg_l" in names
    cons = host_consts(inputs)
    outw = {}
    for n in names:
        if n in cons:
            outw[n] = cons[n]
            continue
        a = np.asarray(inputs[n], dtype=np.float32)
        shp = WEIGHT_SPECS[n]
        if n == "odd_conv_w":
            a = a.reshape(3, 8, 128).transpose(2, 1, 0)
        outw[n] = np.ascontiguousarray(a.reshape(shp))
    return outw


def run_phases(xfull, inputs, phases, T):
    nc, names = build(T, phases)
    w = prep_weights(inputs, names)
    in_maps = []
    for c in range(xfull.shape[0]):
        m = {"x": np.ascontiguousarray(xfull[c])}
        m.update(w)
        in_maps.append(m)
    res = run_bass_kernel_spmd(nc, in_maps, core_ids=list(range(len(in_maps))))
    global LAST_RES
    LAST_RES = res.results
    return np.stack([np.asarray(r["out"]) for r in res.results], axis=0)


class Banks:
    def __init__(self, banks):
        self.banks = banks
        self.i = 0

    def nxt(self):
        b = self.banks[self.i % len(self.banks)]
        self.i += 1
        return b


def attn_phase(S, nc, T, xin_d, ya_d, w_in, sinks, bias_d, ident_d, tag):
    NTL = T // 128
    with contextlib.ExitStack() as es:
        cx = Ctx(nc, es, tag)
        ident = cx.sb("ident", [128, 128], F32)
        win = cx.sb("win", [128, 8, 768], BF16)
        biasb = cx.sb("biasb", [128, 8, 256], F32)
        sinkb = cx.sb("sinkb", [128, 8], F32)
        xinb = [cx.sb("xin%d" % i, [128, 1024], F32) for i in range(2)]
        xT = [cx.sb("xT%d" % i, [128, 8, 128], BF16) for i in range(2)]
        qT = [cx.sb("qT%d" % i, [64, 8, 128], BF16) for i in range(2)]
        kT = [cx.sb("kT%d" % i, [64, 2, 128], BF16) for i in range(2)]
        vt = [cx.sb("vt%d" % i, [128, 128], BF16) for i in range(2)]
        sc = [cx.sb("sc%d" % i, [128, 8, 256], F32) for i in range(2)]
        pp = [cx.sb("pp%d" % i, [128, 8, 256], F32) for i in range(2)]
        PT = [cx.sb("PT%d" % i, [128, 16, 128], BF16) for i in range(2)]
        mx = cx.sb("mx", [128, 8], F32)
        mm = cx.sb("mm", [128, 8], F32)
        negm = cx.sb("negm", [128, 8], F32)
        rs = cx.sb("rs", [128, 8], F32)
        esb = cx.sb("esb", [128, 8], F32)
        rden = [cx.sb("rden%d" % i, [128, 8], F32) for i in range(2)]
        ya = [cx.sb("ya%d" % i, [128, 512], F32) for i in range(2)]
        PB = Banks([cx.ps("ps%d" % i, [128, 512], F32) for i in range(6)])
        bo_banks = [cx.ps("pbo%d" % i, [128, 512], F32) for i in range(2)]

        S.dma("sp", ident[:], ident_d)
        S.dma("sp", biasb[:], bias_d)
        S.dma("sp", sinkb[:], sinks.to_broadcast([128, 8]))
        S.dma("pool", win[:], w_in[:, 0:768].rearrange("(k p) f -> p k f", p=128))
        for i in range(2):
            S.add("pool", lambda e, i=i: e.memset(pp[i][:], 0.0), [], [pp[i][:]])

        for n in range(NTL):
            p = n % 2
            xb = xinb[p]
            S.dma("sp", xb[:], xin_d[n * 128:(n + 1) * 128, :])
            emit_load_transpose(S, xb, ident, [PB.nxt(), PB.nxt()], xT[p][:])
            for b in range(2):
                bk = PB.nxt()
                for hh in range(4):
                    h = b * 4 + hh
                    for kc in range(8):
                        S.add("pe", lambda e, bk=bk, hh=hh, h=h, kc=kc, p=p: e.matmul(
                            bk[0:64, hh * 128:(hh + 1) * 128], win[:, kc, h * 64:(h + 1) * 64], xT[p][:, kc, :],
                            start=(kc == 0), stop=(kc == 7)),
                            [win[:, kc, h * 64:(h + 1) * 64], xT[p][:, kc, :]], [bk[0:64, hh * 128:(hh + 1) * 128]])
                dst = qT[p][:, b * 4:(b + 1) * 4, :]
                src = bk[0:64, :].rearrange("p (j t) -> p j t", j=4)
                S.add("act", lambda e, dst=dst, src=src: e.mul(dst, src, 0.125), [bk[0:64, :]], [dst])
            bk = PB.nxt()
            for g in range(2):
                for kc in range(8):
                    S.add("pe", lambda e, bk=bk, g=g, kc=kc, p=p: e.matmul(
                        bk[0:64, g * 128:(g + 1) * 128], win[:, kc, 512 + g * 64:512 + (g + 1) * 64], xT[p][:, kc, :],
                        start=(kc == 0), stop=(kc == 7)),
                        [win[:, kc, 512 + g * 64:512 + (g + 1) * 64], xT[p][:, kc, :]], [bk[0:64, g * 128:(g + 1) * 128]])
            src = bk[0:64, 0:256].rearrange("p (j t) -> p j t", j=2)
            S.add("act", lambda e, src=src, p=p: e.copy(kT[p][:], src), [bk[0:64, 0:256]], [kT[p][:]])
            bv = PB.nxt()
            for kc in range(8):
                S.add("pe", lambda e, bv=bv, kc=kc, p=p: e.matmul(
                    bv[:, 0:128], xT[p][:, kc, :], win[:, kc, 640:768], start=(kc == 0), stop=(kc == 7)),
                    [xT[p][:, kc, :], win[:, kc, 640:768]], [bv[:, 0:128]])
            S.add("dve", lambda e, bv=bv, p=p: e.tensor_copy(vt[p][:], bv[:, 0:128]), [bv[:, 0:128]], [vt[p][:]])

            import os
            DBG = int(os.environ.get("DBG_STOP", "9"))
            DBGN = int(os.environ.get("DBGN", "0"))
            if DBG <= 1:
                continue
            bo = bo_banks[p]
            k0 = 0 if n > 0 else 128
            kcs = (0, 1) if n > 0 else (1,)
            scb, ppb, PTb = sc[p], pp[p], PT[p]
            for i in range(4):
                g = i // 2
                bs = PB.nxt()
                for hh in range(2):
                    h = 2 * i + hh
                    if n > 0:
                        S.add("pe", lambda e, bs=bs, hh=hh, h=h, g=g, p=p: e.matmul(
                            bs[:, hh * 256:hh * 256 + 128], qT[p][:, h, :], kT[1 - p][:, g, :], start=True, stop=True),
                            [qT[p][:, h, :], kT[1 - p][:, g, :]], [bs[:, hh * 256:hh * 256 + 128]])
                    S.add("pe", lambda e, bs=bs, hh=hh, h=h, g=g, p=p: e.matmul(
                        bs[:, hh * 256 + 128:hh * 256 + 256], qT[p][:, h, :], kT[p][:, g, :], start=True, stop=True),
                        [qT[p][:, h, :], kT[p][:, g, :]], [bs[:, hh * 256 + 128:hh * 256 + 256]])
                bsv = bs[:, :].rearrange("p (j t) -> p j t", j=2)[:, :, k0:256]
                scv = scb[:, 2 * i:2 * i + 2, k0:256]
                S.add("dve", lambda e, scv=scv, bsv=bsv, i=i, k0=k0: e.tensor_tensor(scv, bsv, biasb[:, 2 * i:2 * i + 2, k0:256], ALU.add),
                      [bs[:, :], biasb[:, 2 * i:2 * i + 2, :]], [scb[:, 2 * i:2 * i + 2, :]])
            S.add("dve", lambda e, scb=scb, k0=k0: e.tensor_reduce(mx[:], scb[:, :, k0:256], AX.X, ALU.max), [scb[:]], [mx[:]])
            S.add("dve", lambda e: e.tensor_tensor(mm[:], mx[:], sinkb[:], ALU.max), [mx[:], sinkb[:]], [mm[:]])
            S.add("dve", lambda e: e.tensor_scalar(negm[:], mm[:], -1.0, None, ALU.mult), [mm[:]], [negm[:]])
            for h in range(8):
                S.add("act", lambda e, h=h, scb=scb, ppb=ppb, k0=k0: e.activation(
                    ppb[:, h, k0:256], scb[:, h, k0:256], AF.Exp, bias=negm[:, h:h + 1], accum_out=rs[:, h:h + 1]),
                    [scb[:, h, :], negm[:, h:h + 1]], [ppb[:, h, :], rs[:, h:h + 1]])
            S.add("dve", lambda e: e.tensor_tensor(esb[:], sinkb[:], mm[:], ALU.subtract), [sinkb[:], mm[:]], [esb[:]])
            S.add("act", lambda e: e.activation(esb[:], esb[:], AF.Exp), [esb[:]], [esb[:]])
            S.add("dve", lambda e: e.tensor_tensor(esb[:], esb[:], rs[:], ALU.add), [esb[:], rs[:]], [esb[:]])
            S.add("dve", lambda e, p=p: e.reciprocal(rden[p][:], esb[:]), [esb[:]], [rden[p][:]])
            for i in range(4):
                bt = PB.nxt()
                for hh in range(2):
                    h = 2 * i + hh
                    for kc in (0, 1):
                        j = hh * 2 + kc
                        S.add("pe", lambda e, bt=bt, j=j, h=h, kc=kc, ppb=ppb: e.transpose(
                            bt[:, j * 128:(j + 1) * 128], ppb[:, h, kc * 128:(kc + 1) * 128], ident[:]),
                            [ppb[:, h, kc * 128:(kc + 1) * 128], ident[:]], [bt[:, j * 128:(j + 1) * 128]])
                S.add("act", lambda e, bt=bt, i=i, PTb=PTb: e.copy(PTb[:, 4 * i:4 * i + 4, :], bt[:, :].rearrange("p (j t) -> p j t", j=4)),
                      [bt[:, :]], [PTb[:, 4 * i:4 * i + 4, :]])
            for h in range(8):
                g = h // 4
                for kc in kcs:
                    vsrc = vt[1 - p] if kc == 0 else vt[p]
                    S.add("pe", lambda e, bo=bo, h=h, kc=kc, g=g, vsrc=vsrc, PTb=PTb, kcs=kcs: e.matmul(
                        bo[:, h * 64:(h + 1) * 64], PTb[:, h * 2 + kc, :], vsrc[:, g * 64:(g + 1) * 64],
                        start=(kc == kcs[0]), stop=(kc == kcs[-1])),
                        [PTb[:, h * 2 + kc, :], vsrc[:, g * 64:(g + 1) * 64]], [bo[:, h * 64:(h + 1) * 64]])
            yav = ya[p][:, :].rearrange("p (h d) -> p h d", h=8)
            bov = bo[:, :].rearrange("p (h d) -> p h d", h=8)
            rb = rden[p][:, :].unsqueeze(2).to_broadcast([128, 8, 64])
            S.add("dve", lambda e, yav=yav, bov=bov, rb=rb: e.tensor_tensor(yav, bov, rb, ALU.mult),
                  [bo[:, :], rden[p][:]], [ya[p][:]])
            S.dma("sp", ya_d[n * 128:(n + 1) * 128, :], ya[p][:])
    S.barrier()


RW_DECAY_SCALE = -0.6065306597126334


def rwkv_phase(S, nc, T, xin_d, ya_d, hout, W, tag):
    NTL = T // 128
    w_in = W["even_w_in"]
    with contextlib.ExitStack() as es:
        cx = Ctx(nc, es, tag)
        ident = cx.sb("ident", [128, 128], F32)
        id64 = ident[0:64, 0:64]
        idb = cx.sb("idb", [64, 64], BF16)
        win = cx.sb("win", [128, 8, 1792], BF16)
        wout = cx.sb("wout", [128, 8, 1024], BF16)
        gb = cx.sb("gb", [128, 1024], F32)
        bb = cx.sb("bb", [128, 1024], F32)
        gng = cx.sb("gng", [64, 512], F32)
        gnb = cx.sb("gnb", [64, 512], F32)
        rwv = cx.sb("rwv", [64, 66], F32)
        omka = cx.sb("omka", [64, 8], F32)
        mug = cx.sb("mug", [128, 1], F32)
        wup = cx.sb("wup", [128, 512], F32)
        aup = cx.sb("aup", [128, 512], F32)
        gup = cx.sb("gup", [128, 512], F32)
        msu = cx.sb("msu", [64, 64], F32)
        miu = cx.sb("miu", [64, 64], F32)
        msl = cx.sb("msl", [64, 64], F32)
        ones = cx.sb("ones", [64, 128], F32)
        onesb = cx.sb("onesb", [64, 2], BF16)
        xinb = [cx.sb("xin%d" % i, [128, 1024], F32) for i in range(2)]
        xT = [cx.sb("xT%d" % i, [128, 8, 128], BF16) for i in range(2)]
        zraw = cx.sb("zraw", [64, 26, 129], F32)
        zgraw = cx.sb("zgraw", [128, 129], F32)
        zm = cx.sb("zm", [64, 26, 64], F32)
        zgm = cx.sb("zgm", [128, 64], F32)
        th = cx.sb("th", [128, 64], F32)
        zad = cx.sb("zad", [128, 64], F32)
        H = {}
        for nm in ("lw", "a", "kkn", "rn", "k", "cum", "en", "eA", "eC", "b", "rkf"):
            H[nm] = cx.sb("h_" + nm, [64, 8, 64], F32)
        for nm in ("Bt", "Kt", "Bh", "Kh", "zvb"):
            H[nm] = cx.sb("h_" + nm, [64, 8, 64], BF16)
        M = {}
        for nm in ("Nm", "NTm", "P", "Ma0", "Ma1", "MTa0", "MTa1", "Xs", "Us", "Tb0", "Tb1"):
            M[nm] = cx.sb("m_" + nm, [64, 512], BF16)
        DB = []
        for q in range(2):
            d = {}
            d["eR"] = cx.sb("d%d_eR" % q, [64, 8, 64], F32)
            for nm in ("At", "Rt", "rk"):
                d[nm] = cx.sb("d%d_%s" % (q, nm), [64, 8, 64], BF16)
            for nm in ("BhT", "KhT", "Vt", "AKTm", "RBTm", "RKTm", "Qb"):
                d[nm] = cx.sb("d%d_%s" % (q, nm), [64, 512], BF16)
            d["sgd"] = cx.sb("d%d_sgd" % q, [128, 64], F32)
            DB.append(d)
        for nm in ("Qf", "Tf0", "Tf1", "Ys", "sq", "yn", "tmp", "ybo"):
            M[nm] = cx.sb("m_" + nm, [64, 512], F32)
        gs = cx.sb("gs", [64, 8, 8], F32)
        yab = cx.sb("yab", [128, 512], F32)
        catT = [cx.sb("catT%d" % i, [128, 8, 128], BF16) for i in range(2)]
        pre = cx.sb("pre", [128, 1024], F32)
        ob = cx.sb("ob", [128, 1024], F32)
        st = cx.sb("st", [128, 2, 6], F32)
        mv = cx.sb("mv", [128, 2], F32)
        sc = cx.sb("sc", [128, 2], F32)
        pmix = cx.ps("pmix", [128, 1024], F32)
        pbt = cx.ps("pbt", [128, 1024], BF16)
        PB = Banks([cx.ps("ps%d" % i, [128, 512], F32) for i in range(3)])
        PB2 = Banks([cx.ps("pq%d" % i, [128, 512], F32) for i in range(2)])
        bt_i = [0]

        def v3(t):
            return t[:, :].rearrange("p (h d) -> p h d", h=8)

        def bc_h(ap2):
            return ap2.unsqueeze(2).to_broadcast([64, 8, 64])

        def bc_m(ap2):
            return ap2.unsqueeze(1).to_broadcast([64, 8, 64])

        S.dma("sp", ident[:], W["c_ident"])
        S.dma("sp", gb[:], W["even_ln_mix_g"].to_broadcast([128, 1024]))
        S.dma("sp", bb[:], W["even_ln_mix_b"].to_broadcast([128, 1024]))
        S.dma("sp", gng[:], W["rwkv_gn_g"].to_broadcast([64, 512]))
        S.dma("sp", gnb[:], W["rwkv_gn_b"].to_broadcast([64, 512]))
        S.dma("sp", rwv[:], W["c_rwv"])
        S.dma("sp", mug[:], W["c_mug"])
        S.add("pool", lambda e: e.memset(wup[:], 0.0), [], [wup[:]])
        S.add("pool", lambda e: e.memset(aup[:], 0.0), [], [aup[:]])
        S.dma("sp", wup[0:64, :], W["rwkv_w_up"])
        S.dma("sp", aup[0:64, :], W["rwkv_a_up"])
        S.dma("sp", gup[:], W["rwkv_g_up"])
        S.dma("sp", msu[:], W["c_msu"])
        S.dma("sp", miu[:], W["c_miu"])
        S.dma("sp", msl[:], W["c_msl"])
        for j in range(2):
            S.dma("pool", win[:, :, j * 896:(j + 1) * 896],
                  w_in[:, 768 + j * 896:768 + (j + 1) * 896].rearrange("(k p) f -> p k f", p=128))
        S.dma("pool", wout[:], W["even_w_out"].rearrange("(k p) f -> p k f", p=128))
        S.add("pool", lambda e: e.memset(ones[:], 1.0), [], [ones[:]])
        S.add("pool", lambda e: e.memset(onesb[:], 1.0), [], [onesb[:]])
        S.add("pool", lambda e: e.memset(th[:], 0.0), [], [th[:]])
        S.add("pool", lambda e: e.memset(zad[:], 0.0), [], [zad[:]])
        S.add("pool", lambda e: e.memset(zraw[:, :, 0:1], 0.0), [], [zraw[:, :, 0:1]])
        S.add("pool", lambda e: e.memset(zgraw[:, 0:1], 0.0), [], [zgraw[:, 0:1]])
        S.add("pool", lambda e: e.memset(M["Tf0"][:], 0.0), [], [M["Tf0"][:]])
        S.add("pool", lambda e: e.memset(M["Tb0"][:], 0.0), [], [M["Tb0"][:]])
        S.add("act", lambda e: e.copy(idb[:], id64), [id64], [idb[:]])
        S.add("dve", lambda e: e.tensor_scalar(omka[:], rwv[:, 50:58], -1.0, 1.0, ALU.mult, ALU.add), [rwv[:, 50:58]], [omka[:]])
        mu_b = rwv[:, 0:26].unsqueeze(2).to_broadcast([64, 26, 64])
        w0_b = bc_h(rwv[:, 26:34])
        a0_b = bc_h(rwv[:, 34:42])
        kk_b = bc_h(rwv[:, 42:50])
        ka_b = bc_h(rwv[:, 50:58])
        rk_b = bc_h(rwv[:, 58:66])
        omka_b = bc_h(omka[:, :])

        def TT(eng, out, in0, in1, op, reads, writes):
            S.add(eng, lambda e: e.tensor_tensor(out, in0, in1, op), reads, writes)

        def headmm(bank, lhs_fn, rhs_fn):
            for h in range(8):
                lh, rh = lhs_fn(h), rhs_fn(h)
                ob_ = bank[0:64, h * 64:(h + 1) * 64]
                S.add("pe", lambda e, ob_=ob_, lh=lh, rh=rh: e.matmul(ob_, lh, rh, start=True, stop=True),
                      [lh, rh], [ob_])

        def blk(t, h):
            return t[:, h * 64:(h + 1) * 64]

        import os
        RWSTOP = int(os.environ.get("RW_STOP", "99"))
        def proj(n):
            p = n % 2
            xb = xinb[p]
            S.dma("sp", xb[:], xin_d[n * 128:(n + 1) * 128, :])
            emit_load_transpose(S, xb, ident, [PB.nxt(), PB.nxt()], xT[p][:])
            if n > 0:
                S.add("pool", lambda e: e.tensor_copy(zraw[:, :, 0:1], zraw[:, :, 128:129]), [zraw[:, :, 128:129]], [zraw[:, :, 0:1]])
                S.add("pool", lambda e: e.tensor_copy(zgraw[:, 0:1], zgraw[:, 128:129]), [zgraw[:, 128:129]], [zgraw[:, 0:1]])
            for b in range(7):
                bk = PB.nxt()
                ng = 4 if b < 6 else 2
                for j in range(ng):
                    gi = b * 4 + j
                    col0 = gi * 64
                    for kc in range(8):
                        S.add("pe", lambda e, bk=bk, j=j, col0=col0, kc=kc, p=p: e.matmul(
                            bk[0:64, j * 128:(j + 1) * 128], win[:, kc, col0:col0 + 64], xT[p][:, kc, :],
                            start=(kc == 0), stop=(kc == 7)),
                            [win[:, kc, col0:col0 + 64], xT[p][:, kc, :]], [bk[0:64, j * 128:(j + 1) * 128]])
                dst = zraw[:, b * 4:b * 4 + ng, 1:129]
                src = bk[0:64, 0:ng * 128].rearrange("p (j t) -> p j t", j=ng)
                S.add("act", lambda e, dst=dst, src=src: e.copy(dst, src), [bk[0:64, 0:ng * 128]], [dst])
            bk = PB.nxt()
            for kc in range(8):
                S.add("pe", lambda e, bk=bk, kc=kc, p=p: e.matmul(
                    bk[:, 0:128], win[:, kc, 1664:1792], xT[p][:, kc, :], start=(kc == 0), stop=(kc == 7)),
                    [win[:, kc, 1664:1792], xT[p][:, kc, :]], [bk[:, 0:128]])
            S.add("act", lambda e, bk=bk: e.copy(zgraw[:, 1:129], bk[:, 0:128]), [bk[:, 0:128]], [zgraw[:, 1:129]])

        def stage1(ci, q):
            D_ = DB[q]
            c0 = ci * 64
            zc = zraw[:, :, c0 + 1:c0 + 65]
            zp = zraw[:, :, c0:c0 + 64]
            sgd = D_["sgd"]
            TT("pool", zm[:], zp, zc, ALU.subtract, [zraw[:]], [zm[:]]); yield
            TT("dve", zm[:], zm[:], mu_b, ALU.mult, [zm[:], rwv[:, 0:26]], [zm[:]]); yield
            TT("pool", zm[:], zm[:], zc, ALU.add, [zm[:], zraw[:]], [zm[:]]); yield
            TT("pool", zgm[:], zgraw[:, c0:c0 + 64], zgraw[:, c0 + 1:c0 + 65], ALU.subtract, [zgraw[:]], [zgm[:]]); yield
            S.add("dve", lambda e, c0=c0: e.scalar_tensor_tensor(zgm[:], zgm[:], mug[:, 0:1], zgraw[:, c0 + 1:c0 + 65], ALU.mult, ALU.add),
                  [zgm[:], mug[:], zgraw[:]], [zgm[:]]); yield
            zr = zm[:, 0:8, :]
            zk = zm[:, 8:16, :]
            S.add("act", lambda e: e.activation(th[0:64, :], zm[:, 24, :], AF.Tanh), [zm[:, 24, :]], [th[0:64, :]]); yield
            S.add("pool", lambda e: e.tensor_copy(zad[0:64, :], zm[:, 25, :]), [zm[:, 25, :]], [zad[0:64, :]]); yield
            S.add("act", lambda e: e.activation(sgd[:], zgm[:], AF.Sigmoid), [zgm[:]], [sgd[:]]); yield
            S.add("act", lambda e: e.copy(H["zvb"][:], zm[:, 16:24, :]), [zm[:, 16:24, :]], [H["zvb"][:]]); yield
            bW = PB.nxt()
            headmm(bW, lambda h: wup[:, h * 64:(h + 1) * 64], lambda h: th[:])
            lw, a_, kkn, rn, k_, cum = H["lw"], H["a"], H["kkn"], H["rn"], H["k"], H["cum"]
            en, eA, eC, b_, rkf = H["en"], H["eA"], H["eC"], H["b"], H["rkf"]
            Bt, Kt, Bh, Kh, zvb = H["Bt"], H["Kt"], H["Bh"], H["Kh"], H["zvb"]
            eR, At, Rt, rk = D_["eR"], D_["At"], D_["Rt"], D_["rk"]
            TT("dve", lw[:], v3(bW)[0:64], w0_b, ALU.add, [bW[0:64, :], rwv[:, 26:34]], [lw[:]]); yield
            bA = PB.nxt()
            headmm(bA, lambda h: aup[:, h * 64:(h + 1) * 64], lambda h: zad[:])
            TT("dve", a_[:], v3(bA)[0:64], a0_b, ALU.add, [bA[0:64, :], rwv[:, 34:42]], [a_[:]]); yield
            S.add("act", lambda e: e.activation(lw[:], lw[:], AF.Sigmoid), [lw[:]], [lw[:]]); yield
            S.add("act", lambda e: e.activation(a_[:], a_[:], AF.Sigmoid), [a_[:]], [a_[:]]); yield
            TT("dve", kkn[:], zk, kk_b, ALU.mult, [zm[:, 8:16, :], rwv[:, 42:50]], [kkn[:]]); yield
            TT("pool", rn[:], kkn[:], kkn[:], ALU.mult, [kkn[:]], [rn[:]]); yield
            bS = PB.nxt()
            rn2 = rn[:, :, :].rearrange("p h d -> p (h d)")
            S.add("pe", lambda e, bS=bS, rn2=rn2: e.matmul(bS[:, :], ones[:], rn2, start=True, stop=True),
                  [ones[:], rn[:]], [bS[:, :]])
            S.add("act", lambda e, bS=bS: e.activation(rn[:], v3(bS)[0:64], AF.Sqrt), [bS[0:64, :]], [rn[:]]); yield
            for h in range(8):
                S.add("dve", lambda e, h=h: e.tensor_tensor_scan(cum[:, h, :], ones[:, 0:64], lw[:, h, :], 0.0, ALU.mult, ALU.add),
                      [ones[:, 0:64], lw[:, h, :]], [cum[:, h, :]])
                yield
            S.add("dve", lambda e: e.tensor_scalar(rn[:], rn[:], 1e-12, None, ALU.max), [rn[:]], [rn[:]]); yield
            S.add("dve", lambda e: e.reciprocal(rn[:], rn[:]), [rn[:]], [rn[:]]); yield
            TT("dve", kkn[:], kkn[:], rn[:], ALU.mult, [kkn[:], rn[:]], [kkn[:]]); yield
            TT("pool", k_[:], a_[:], ka_b, ALU.mult, [a_[:], rwv[:, 50:58]], [k_[:]]); yield
            TT("pool", k_[:], k_[:], omka_b, ALU.add, [k_[:], omka[:]], [k_[:]]); yield
            TT("dve", k_[:], k_[:], zk, ALU.mult, [k_[:], zm[:, 8:16, :]], [k_[:]]); yield
            S.add("act", lambda e: e.activation(eR[:], cum[:], AF.Exp, scale=RW_DECAY_SCALE), [cum[:]], [eR[:]]); yield
            S.add("act", lambda e: e.activation(en[:], cum[:], AF.Exp, scale=-RW_DECAY_SCALE), [cum[:]], [en[:]]); yield
            TT("pool", eA[:], cum[:], lw[:], ALU.subtract, [cum[:], lw[:]], [eA[:]]); yield
            S.add("act", lambda e: e.activation(eA[:], eA[:], AF.Exp, scale=RW_DECAY_SCALE), [eA[:]], [eA[:]]); yield
            TT("pool", eC[:], cum[:, :, 63:64].to_broadcast([64, 8, 64]), cum[:], ALU.subtract, [cum[:]], [eC[:]]); yield
            S.add("act", lambda e: e.activation(eC[:], eC[:], AF.Exp, scale=RW_DECAY_SCALE), [eC[:]], [eC[:]]); yield
            S.add("dve", lambda e: e.scalar_tensor_tensor(At[:], kkn[:], -1.0, eA[:], ALU.mult, ALU.mult), [kkn[:], eA[:]], [At[:]]); yield
            TT("pool", b_[:], kkn[:], a_[:], ALU.mult, [kkn[:], a_[:]], [b_[:]]); yield
            TT("dve", Bt[:], b_[:], en[:], ALU.mult, [b_[:], en[:]], [Bt[:]]); yield
            TT("pool", Kt[:], k_[:], en[:], ALU.mult, [k_[:], en[:]], [Kt[:]]); yield
            TT("dve", Rt[:], zr, eR[:], ALU.mult, [zm[:, 0:8, :], eR[:]], [Rt[:]]); yield
            TT("pool", Bh[:], b_[:], eC[:], ALU.mult, [b_[:], eC[:]], [Bh[:]]); yield
            TT("dve", Kh[:], k_[:], eC[:], ALU.mult, [k_[:], eC[:]], [Kh[:]]); yield
            TT("pool", rkf[:], zr, k_[:], ALU.mult, [zm[:, 0:8, :], k_[:]], [rkf[:]]); yield
            TT("pool", rk[:], rkf[:], rk_b, ALU.mult, [rkf[:], rwv[:, 58:66]], [rk[:]]); yield
            for (src3, dstn) in ((Bh, "BhT"), (Kh, "KhT"), (zvb, "Vt")):
                half = bt_i[0] % 2
                bt_i[0] += 1
                for h in range(8):
                    in_ = src3[:, h, :]
                    ob_ = pbt[0:64, half * 512 + h * 64:half * 512 + (h + 1) * 64]
                    S.add("pe", lambda e, ob_=ob_, in_=in_: e.transpose(ob_, in_, idb[:]), [in_, idb[:]], [ob_])
                dst = D_[dstn]
                srcb = pbt[0:64, half * 512:(half + 1) * 512]
                S.add("act" if dstn == "KhT" else "dve", (lambda e, dst=dst, srcb=srcb: e.copy(dst[:], srcb)) if dstn == "KhT" else
                      (lambda e, dst=dst, srcb=srcb: e.tensor_copy(dst[:], srcb)), [srcb], [dst[:]])
                yield
            Nm, NTm, P, Qf = M["Nm"], M["NTm"], M["P"], M["Qf"]
            specs = ((Bt, At, Nm, msu), (At, Bt, NTm, msl), (Kt, At, D_["AKTm"], msu), (Bt, Rt, D_["RBTm"], miu), (Kt, Rt, D_["RKTm"], miu))
            for (L3, R3, dst, msk) in specs:
                bC = PB.nxt()
                headmm(bC, lambda h, L3=L3: L3[:, h, :], lambda h, R3=R3: R3[:, h, :])
                TT("dve", v3(dst), v3(bC)[0:64], bc_m(msk[:, :]), ALU.mult, [bC[0:64, :], msk[:]], [dst[:]]); yield
            TT("pool", v3(Qf), v3(Nm), bc_m(id64), ALU.add, [Nm[:], ident[0:64, 0:64]], [Qf[:]]); yield
            TT("pool", v3(P), v3(NTm), bc_m(id64), ALU.add, [NTm[:], ident[0:64, 0:64]], [P[:]]); yield
            cM, cMT = Nm, NTm
            for lvl in range(1, 6):
                nM = M["Ma%d" % (lvl % 2)]
                nMT = M["MTa%d" % (lvl % 2)]
                bM = PB.nxt()
                headmm(bM, lambda h, cMT=cMT: blk(cMT, h), lambda h, cM=cM: blk(cM, h))
                S.add("act", lambda e, nM=nM, bM=bM: e.copy(nM[:], bM[0:64, :]), [bM[0:64, :]], [nM[:]]); yield
                if lvl < 5:
                    bMT = PB.nxt()
                    headmm(bMT, lambda h, cM=cM: blk(cM, h), lambda h, cMT=cMT: blk(cMT, h))
                    S.add("dve", lambda e, nMT=nMT, bMT=bMT: e.tensor_copy(nMT[:], bMT[0:64, :]), [bMT[0:64, :]], [nMT[:]]); yield
                bQ = PB.nxt()
                headmm(bQ, lambda h: blk(P, h), lambda h, nM=nM: blk(nM, h))
                TT("dve", Qf[:], Qf[:], bQ[0:64, :], ALU.add, [Qf[:], bQ[0:64, :]], [Qf[:]]); yield
                if lvl < 5:
                    bP = PB.nxt()
                    headmm(bP, lambda h, nM=nM: blk(nM, h), lambda h: blk(P, h))
                    TT("dve", P[:], P[:], bP[0:64, :], ALU.add, [P[:], bP[0:64, :]], [P[:]]); yield
                cM, cMT = nM, nMT
            S.add("act", lambda e: e.copy(D_["Qb"][:], Qf[:]), [Qf[:]], [D_["Qb"][:]]); yield

        def stage2(n, ci, q, gcx):
            D_ = DB[q]
            p = n % 2
            c0 = ci * 64
            Tfp, Tfn = M["Tf%d" % (gcx % 2)], M["Tf%d" % ((gcx + 1) % 2)]
            Tbp, Tbn = M["Tb%d" % (gcx % 2)], M["Tb%d" % ((gcx + 1) % 2)]
            eR, At, Rt, rk, sgd = D_["eR"], D_["At"], D_["Rt"], D_["rk"], D_["sgd"]
            BhT, KhT, Vt, AKTm, RBTm, RKTm, Qb = D_["BhT"], D_["KhT"], D_["Vt"], D_["AKTm"], D_["RBTm"], D_["RKTm"], D_["Qb"]
            Xs, Us, Ys, sq, yn, tmp, ybo = M["Xs"], M["Us"], M["Ys"], M["sq"], M["yn"], M["tmp"], M["ybo"]
            bX = PB2.nxt()
            for h in range(8):
                ob_ = bX[0:64, h * 64:(h + 1) * 64]
                S.add("pe", lambda e, ob_=ob_, h=h: e.matmul(ob_, blk(AKTm, h), blk(Vt, h), start=True, stop=False),
                      [blk(AKTm, h), blk(Vt, h)], [ob_])
                S.add("pe", lambda e, ob_=ob_, h=h, Tbp=Tbp: e.matmul(ob_, At[:, h, :], blk(Tbp, h), start=False, stop=True),
                      [At[:, h, :], blk(Tbp, h)], [ob_])
            S.add("act", lambda e, bX=bX: e.copy(Xs[:], bX[0:64, :]), [bX[0:64, :]], [Xs[:]]); yield
            bU = PB2.nxt()
            headmm(bU, lambda h: blk(Qb, h), lambda h: blk(Xs, h))
            S.add("dve", lambda e, bU=bU: e.tensor_copy(Us[:], bU[0:64, :]), [bU[0:64, :]], [Us[:]]); yield
            bTn = PB2.nxt()
            for h in range(8):
                ob_ = bTn[0:64, h * 64:(h + 1) * 64]
                S.add("pe", lambda e, ob_=ob_, h=h: e.matmul(ob_, blk(BhT, h), blk(Us, h), start=True, stop=False),
                      [blk(BhT, h), blk(Us, h)], [ob_])
                S.add("pe", lambda e, ob_=ob_, h=h: e.matmul(ob_, blk(KhT, h), blk(Vt, h), start=False, stop=True),
                      [blk(KhT, h), blk(Vt, h)], [ob_])
            TT("pool", v3(Tfn), v3(Tfp), eR[:, :, 63:64].to_broadcast([64, 8, 64]), ALU.mult, [Tfp[:], eR[:]], [Tfn[:]]); yield
            TT("dve", Tfn[:], Tfn[:], bTn[0:64, :], ALU.add, [Tfn[:], bTn[0:64, :]], [Tfn[:]]); yield
            S.add("act", lambda e, Tbn=Tbn, Tfn=Tfn: e.copy(Tbn[:], Tfn[:]), [Tfn[:]], [Tbn[:]]); yield
            bY = PB2.nxt()
            for h in range(8):
                ob_ = bY[0:64, h * 64:(h + 1) * 64]
                S.add("pe", lambda e, ob_=ob_, h=h, Tbp=Tbp: e.matmul(ob_, Rt[:, h, :], blk(Tbp, h), start=True, stop=False),
                      [Rt[:, h, :], blk(Tbp, h)], [ob_])
                S.add("pe", lambda e, ob_=ob_, h=h: e.matmul(ob_, blk(RBTm, h), blk(Us, h), start=False, stop=False),
                      [blk(RBTm, h), blk(Us, h)], [ob_])
                S.add("pe", lambda e, ob_=ob_, h=h: e.matmul(ob_, blk(RKTm, h), blk(Vt, h), start=False, stop=True),
                      [blk(RKTm, h), blk(Vt, h)], [ob_])
            S.add("act", lambda e, bY=bY: e.copy(Ys[:], bY[0:64, :]), [bY[0:64, :]], [Ys[:]]); yield
            bB = PB2.nxt()
            for h in range(8):
                ob_ = bB[0:64, h:h + 1]
                S.add("pe", lambda e, ob_=ob_, h=h: e.matmul(ob_, rk[:, h, :], onesb[:, 0:1], start=True, stop=True),
                      [rk[:, h, :], onesb[:, 0:1]], [ob_])
            S.add("dve", lambda e, bB=bB: e.tensor_copy(gs[:, :, 4], bB[0:64, 0:8]), [bB[0:64, 0:8]], [gs[:, :, 4]]); yield
            S.add("dve", lambda e: e.tensor_reduce(gs[:, :, 0], v3(Ys), AX.X, ALU.add), [Ys[:]], [gs[:, :, 0]]); yield
            TT("pool", sq[:], Ys[:], Ys[:], ALU.mult, [Ys[:]], [sq[:]]); yield
            S.add("dve", lambda e: e.tensor_reduce(gs[:, :, 1], v3(sq), AX.X, ALU.add), [sq[:]], [gs[:, :, 1]]); yield
            S.add("dve", lambda e: e.tensor_scalar(gs[:, :, 0:2], gs[:, :, 0:2], 1.0 / 64.0, None, ALU.mult), [gs[:, :, 0:2]], [gs[:, :, 0:2]]); yield
            TT("dve", gs[:, :, 2], gs[:, :, 0], gs[:, :, 0], ALU.mult, [gs[:, :, 0]], [gs[:, :, 2]]); yield
            TT("dve", gs[:, :, 1], gs[:, :, 1], gs[:, :, 2], ALU.subtract, [gs[:, :, 1], gs[:, :, 2]], [gs[:, :, 1]]); yield
            S.add("act", lambda e: e.activation(gs[:, :, 3], gs[:, :, 1], AF.Sqrt, bias=GN_EPS), [gs[:, :, 1]], [gs[:, :, 3]]); yield
            S.add("dve", lambda e: e.reciprocal(gs[:, :, 3], gs[:, :, 3]), [gs[:, :, 3]], [gs[:, :, 3]]); yield
            TT("dve", v3(yn), v3(Ys), gs[:, :, 0:1].to_broadcast([64, 8, 64]), ALU.subtract, [Ys[:], gs[:, :, 0:1]], [yn[:]]); yield
            TT("dve", v3(yn), v3(yn), gs[:, :, 3:4].to_broadcast([64, 8, 64]), ALU.mult, [yn[:], gs[:, :, 3:4]], [yn[:]]); yield
            TT("pool", yn[:], yn[:], gng[:], ALU.mult, [yn[:], gng[:]], [yn[:]]); yield
            TT("pool", yn[:], yn[:], gnb[:], ALU.add, [yn[:], gnb[:]], [yn[:]]); yield
            TT("dve", v3(tmp), v3(Vt), gs[:, :, 4:5].to_broadcast([64, 8, 64]), ALU.mult, [Vt[:], gs[:, :, 4:5]], [tmp[:]]); yield
            TT("pool", yn[:], yn[:], tmp[:], ALU.add, [yn[:], tmp[:]], [yn[:]]); yield
            bG = PB2.nxt()
            S.add("pe", lambda e, bG=bG: e.matmul(bG[0:64, :], sgd[:], gup[:], start=True, stop=True),
                  [sgd[:], gup[:]], [bG[0:64, :]])
            TT("dve", ybo[:], yn[:], bG[0:64, :], ALU.mult, [yn[:], bG[0:64, :]], [ybo[:]]); yield
            bYT = PB2.nxt()
            for j in range(4):
                ob_ = bYT[:, j * 64:(j + 1) * 64]
                in_ = ybo[:, j * 128:(j + 1) * 128]
                S.add("pe", lambda e, ob_=ob_, in_=in_: e.transpose(ob_, in_, id64), [in_, id64], [ob_])
            dst = catT[p][:, 4:8, c0:c0 + 64]
            S.add("act", lambda e, dst=dst, bYT=bYT: e.copy(dst, bYT[:, 0:256].rearrange("p (j t) -> p j t", j=4)),
                  [bYT[:, 0:256]], [dst]); yield

        def outp(n):
            p = n % 2
            xb = xinb[p]
            S.dma("sp", yab[:], ya_d[n * 128:(n + 1) * 128, :])
            bAT = PB2.nxt()
            for j in range(4):
                S.add("pe", lambda e, bAT=bAT, j=j: e.transpose(bAT[:, j * 128:(j + 1) * 128], yab[:, j * 128:(j + 1) * 128], ident[:]),
                      [yab[:, j * 128:(j + 1) * 128], ident[:]], [bAT[:, j * 128:(j + 1) * 128]])
            S.add("act", lambda e, bAT=bAT, p=p: e.copy(catT[p][:, 0:4, :], bAT[:, :].rearrange("p (j t) -> p j t", j=4)),
                  [bAT[:, :]], [catT[p][:, 0:4, :]])
            for hd in range(2):
                for kc in range(8):
                    S.add("pe", lambda e, hd=hd, kc=kc, p=p: e.matmul(
                        pmix[:, hd * 512:(hd + 1) * 512], catT[p][:, kc, :], wout[:, kc, hd * 512:(hd + 1) * 512],
                        start=(kc == 0), stop=(kc == 7)),
                        [catT[p][:, kc, :], wout[:, kc, hd * 512:(hd + 1) * 512]], [pmix[:, hd * 512:(hd + 1) * 512]])
            S.add("dve", lambda e, xb=xb: e.scalar_tensor_tensor(pre[:], xb[:], ALPHA, pmix[:], ALU.mult, ALU.add),
                  [xb[:], pmix[:]], [pre[:]])
            emit_ln(S, cx, pre[:], ob[:], gb, bb, st, mv, sc)
            S.dma("sp", hout[n * 128:(n + 1) * 128, :], ob[:])

        def interleave(ga, gb_, ra=3):
            da = db = False
            while not (da and db):
                for _ in range(ra):
                    if not da:
                        try:
                            next(ga)
                        except StopIteration:
                            da = True
                if not db:
                    try:
                        next(gb_)
                    except StopIteration:
                        db = True

        import os
        NCH = 2 * NTL
        proj(0)
        for _ in stage1(0, 0):
            pass
        for g in range(1, NCH + 1):
            if g < NCH and g % 2 == 0:
                proj(g // 2)
            g2 = stage2((g - 1) // 2, (g - 1) % 2, (g - 1) % 2, g - 1)
            if g < NCH and os.environ.get("NOPIPE") == "1":
                for _ in g2:
                    pass
                for _ in stage1(g % 2, g % 2):
                    pass
            elif g < NCH:
                interleave(stage1(g % 2, g % 2), g2, int(os.environ.get("RATIO", "2")))
            else:
                for _ in g2:
                    pass
            if (g - 1) % 2 == 1:
                outp((g - 1) // 2)
    S.barrier()


def kernel(**inputs):
    x = np.ascontiguousarray(np.asarray(inputs["x"], dtype=np.float32))
    out = run_phases(x, inputs, ["A", "B2", "C", "E"], x.shape[1])
    return np.ascontiguousarray(out.astype(np.float32))


I32 = mybir.dt.int32
SLOT = 512


def moe_sparse_phase(S, nc, T, hin, hout, W, tag):
    NTL = T // 128
    NSLOT = (2 * T) // SLOT + 8
    NROWS = NSLOT * SLOT
    wg_l, wu_l, wd_l = W["moe_wg_l"], W["moe_wu_l"], W["moe_wd_l"]
    xs = nc.dram_tensor("moe_xs" + tag, [NROWS, 1024], F32).ap()
    ys = nc.dram_tensor("moe_ys" + tag, [NROWS, 1024], F32).ap()
    G = 4
    NG = 14
    NSG = 7
    with contextlib.ExitStack() as es:
        cx = Ctx(nc, es, tag)
        ident = cx.sb("ident", [128, 128], F32)
        ltm = cx.sb("ltm", [128, 128], F32)
        ones = cx.sb("ones", [128, 128], F32)
        slot512 = cx.sb("slot512", [128, NSLOT], F32)
        cgp = cx.sb("cgp", [128, NG], F32)
        rwb = cx.sb("rwb", [128, 8, 8], F32)
        gb = cx.sb("gb", [128, 1024], F32)
        bb = cx.sb("bb", [128, 1024], F32)
        hin4 = [cx.sb("hin4_%d" % i, [128, 4, 1024], F32) for i in range(2)]
        hinb = [hin4[0][:, 0, :], hin4[0][:, 1, :]]
        xTf = cx.sb("xTf", [128, 8, 128], F32)
        lga = cx.sb("lga", [128, NTL, 8], F32)
        lgb = cx.sb("lgb", [128, NTL, 8], F32)
        q1 = cx.sb("q1", [128, NTL, 8], F32)
        m12 = cx.sb("m12", [128, 4, NTL], F32)
        stg = cx.sb("stg", [128, 4, 2, 6], F32)
        mvg = cx.sb("mvg", [128, 4, 2], F32)
        scg = cx.sb("scg", [128, 4, 2], F32)
        E1 = cx.sb("E1", [128, 8, NTL], F32)
        E2 = cx.sb("E2", [128, 8, NTL], F32)
        MM = cx.sb("MM", [128, 8, NTL], F32)
        PW = cx.sb("PW", [128, NTL, 2], F32)
        within = cx.sb("within", [128, 8, NTL], F32)
        tot = cx.sb("tot", [128, 8, NTL], F32)
        incl = cx.sb("incl", [128, 8, NTL], F32)
        posall = cx.sb("posall", [128, 8, NTL], F32)
        ptmp = cx.sb("ptmp", [128, 8, NTL], F32)
        pos1 = cx.sb("pos1", [128, NTL], F32)
        pos2 = cx.sb("pos2", [128, NTL], F32)
        pos1i = cx.sb("pos1i", [128, NTL], I32)
        pos2i = cx.sb("pos2i", [128, NTL], I32)
        sv = cx.sb("sv", [128, 8, 6], F32)
        cmp_ = cx.sb("cmp", [128, NSLOT, 8], F32)
        esl = cx.sb("esl", [128, NSLOT], F32)
        idxf = cx.sb("idxf", [128, NSLOT, NG], F32)
        idxw = cx.sb("idxw", [128, NSLOT, NG], I32)
        xT = [cx.sb("xT%d" % i, [128, 8, SLOT], BF16) for i in range(2)]
        acc = [cx.sb("acc%d" % i, [128, 4, 1024], F32) for i in range(2)]
        NWB = 3
        wgb = [cx.sb("wg%d" % i, [128, 2, 8, 256], BF16) for i in range(NWB)]
        wub = [cx.sb("wu%d" % i, [128, 2, 8, 256], BF16) for i in range(NWB)]
        wdb = [cx.sb("wd%d" % i, [128, 2, 2, 1024], BF16) for i in range(NWB)]
        sg = [cx.sb("sg%d" % i, [128, SLOT], F32) for i in range(2)]
        aT = [cx.sb("aT%d" % i, [128, G, SLOT], BF16) for i in range(2)]
        st = cx.sb("st", [128, 2, 6], F32)
        mv = cx.sb("mv", [128, 2], F32)
        sc = cx.sb("sc", [128, 2], F32)
        pg = [cx.ps("pg%d" % i, [128, 512], F32) for i in range(2)]
        pu = [cx.ps("pu%d" % i, [128, 512], F32) for i in range(2)]
        pd = [cx.ps("pd%d" % i, [128, 1024], F32) for i in range(2)]

        S.dma("sp", ident[:], W["c_ident"])
        S.dma("sp", ltm[:], W["c_ltm"])
        S.dma("sp", slot512[:], W["c_slot512"][:, 0:NSLOT])
        S.dma("sp", cgp[:], W["c_gp"])
        S.dma("sp", rwb[:], W["router_w"].rearrange("(k p) e -> p k e", p=128))
        S.dma("sp", gb[:], W["odd_ln_ffn_g"].to_broadcast([128, 1024]))
        S.dma("sp", bb[:], W["odd_ln_ffn_b"].to_broadcast([128, 1024]))
        S.add("pool", lambda e: e.memset(ones[:], 1.0), [], [ones[:]])
        S.add("pool", lambda e: e.memset(acc[0][:], 0.0), [], [acc[0][:]])
        for r0 in range(0, NROWS, 512):
            S.dma("pool", xs[r0:r0 + 512, :].rearrange("(t p) d -> p t d", p=128), acc[0][:])

        for tt in range(NTL):
            hb = hinb[tt % 2]
            S.dma("sp", hb[:], hin[tt * 128:(tt + 1) * 128, :])
            for b in range(2):
                pb = pg[b]
                for j in range(4):
                    kc = b * 4 + j
                    S.add("pe", lambda e, pb=pb, j=j, kc=kc, hb=hb: e.transpose(pb[:, j * 128:(j + 1) * 128], hb[:, kc * 128:(kc + 1) * 128], ident[:]),
                          [hb[:, kc * 128:(kc + 1) * 128], ident[:]], [pb[:, j * 128:(j + 1) * 128]])
                dstf = xTf[:, b * 4:(b + 1) * 4, :]
                src = pb[:, 0:512].rearrange("p (j t) -> p j t", j=4)
                S.add("act" if b == 0 else "dve", (lambda e, dstf=dstf, src=src: e.copy(dstf, src)) if b == 0 else
                      (lambda e, dstf=dstf, src=src: e.tensor_copy(dstf, src)), [pb[:, 0:512]], [dstf])
            pl = pu[0][:, 0:8]
            for kc in range(8):
                S.add("pe", lambda e, kc=kc, pl=pl: e.matmul(pl, xTf[:, kc, :], rwb[:, kc, :], start=(kc == 0), stop=(kc == 7)),
                      [xTf[:, kc, :], rwb[:, kc, :]], [pl])
            S.add("dve", lambda e, pl=pl, tt=tt: e.tensor_copy(lga[:, tt, :], pl), [pl], [lga[:, tt, :]])
        bcT = lambda ap2: ap2.unsqueeze(2).to_broadcast([128, NTL, 8])
        E1v = E1[:, :, :].rearrange("p e t -> p t e")
        E2v = E2[:, :, :].rearrange("p e t -> p t e")
        S.add("dve", lambda e: e.tensor_reduce(m12[:, 0, :], lga[:], AX.X, ALU.max), [lga[:]], [m12[:, 0, :]])
        S.add("dve", lambda e: e.tensor_tensor(q1[:], lga[:], bcT(m12[:, 0, :]), ALU.is_equal), [lga[:], m12[:, 0, :]], [q1[:]])
        S.add("dve", lambda e: e.scalar_tensor_tensor(lgb[:], q1[:], -1e30, lga[:], ALU.mult, ALU.add), [q1[:], lga[:]], [lgb[:]])
        S.add("pool", lambda e: e.tensor_copy(E1v, q1[:]), [q1[:]], [E1[:]])
        S.add("dve", lambda e: e.tensor_reduce(m12[:, 1, :], lgb[:], AX.X, ALU.max), [lgb[:]], [m12[:, 1, :]])
        S.add("dve", lambda e: e.tensor_tensor(E2v, lgb[:], bcT(m12[:, 1, :]), ALU.is_equal), [lgb[:], m12[:, 1, :]], [E2[:]])
        S.add("dve", lambda e: e.tensor_tensor(m12[:, 2, :], m12[:, 1, :], m12[:, 0, :], ALU.subtract), [m12[:, 0:2, :]], [m12[:, 2, :]])
        S.add("act", lambda e: e.activation(m12[:, 2, :], m12[:, 2, :], AF.Exp), [m12[:, 2, :]], [m12[:, 2, :]])
        S.add("dve", lambda e: e.tensor_scalar(m12[:, 3, :], m12[:, 2, :], 1.0, None, ALU.add), [m12[:, 2, :]], [m12[:, 3, :]])
        S.add("dve", lambda e: e.reciprocal(PW[:, :, 0], m12[:, 3, :]), [m12[:, 3, :]], [PW[:, :, 0]])
        S.add("dve", lambda e: e.tensor_tensor(PW[:, :, 1], m12[:, 2, :], PW[:, :, 0], ALU.mult), [m12[:, 2, :], PW[:, :, 0]], [PW[:, :, 1]])
        f2 = lambda t: t[:, :, :].rearrange("p e t -> p (e t)")
        S.add("dve", lambda e: e.tensor_tensor(MM[:], E1[:], E2[:], ALU.add), [E1[:], E2[:]], [MM[:]])
        NC_ = 8 * NTL
        S.add("pe", lambda e: e.matmul(pg[0][:, 0:NC_], ltm[:], f2(MM), start=True, stop=True), [ltm[:], MM[:]], [pg[0][:, 0:NC_]])
        S.add("pe", lambda e: e.matmul(pg[1][:, 0:NC_], ones[:], f2(MM), start=True, stop=True), [ones[:], MM[:]], [pg[1][:, 0:NC_]])
        S.add("dve", lambda e: e.tensor_copy(f2(within), pg[0][:, 0:NC_]), [pg[0][:, 0:NC_]], [within[:]])
        S.add("act", lambda e: e.copy(f2(tot), pg[1][:, 0:NC_]), [pg[1][:, 0:NC_]], [tot[:]])
        for ex in range(8):
            S.add("dve", lambda e, ex=ex: e.tensor_tensor_scan(incl[:, ex, :], ones[:, 0:NTL], tot[:, ex, :], 0.0, ALU.mult, ALU.add),
                  [ones[:, 0:NTL], tot[:, ex, :]], [incl[:, ex, :]])
        S.add("dve", lambda e: e.tensor_copy(sv[:, :, 0], incl[:, :, NTL - 1]), [incl[:]], [sv[:, :, 0]])
        S.add("dve", lambda e: e.tensor_tensor(cmp_[:, 0:8, :], sv[:, :, 0].unsqueeze(1).to_broadcast([128, 8, 8]),
                                               slot512[:, 0:8].unsqueeze(2).to_broadcast([128, 8, 8]), ALU.is_gt),
              [sv[:, :, 0], slot512[:, 0:8]], [cmp_[:, 0:8, :]])
        S.add("dve", lambda e: e.tensor_reduce(sv[:, :, 1], cmp_[:, 0:8, :].rearrange("p j e -> p e j"), AX.X, ALU.add),
              [cmp_[:, 0:8, :]], [sv[:, :, 1]])
        S.add("dve", lambda e: e.tensor_scalar(sv[:, :, 3], sv[:, :, 1], float(SLOT), None, ALU.mult), [sv[:, :, 1]], [sv[:, :, 3]])
        S.add("dve", lambda e: e.tensor_tensor_scan(sv[:, :, 4], ones[:, 0:8], sv[:, :, 3], 0.0, ALU.mult, ALU.add),
              [ones[:, 0:8], sv[:, :, 3]], [sv[:, :, 4]])
        S.add("dve", lambda e: e.tensor_tensor(sv[:, :, 5], sv[:, :, 4], sv[:, :, 3], ALU.subtract), [sv[:, :, 4], sv[:, :, 3]], [sv[:, :, 5]])
        S.add("dve", lambda e: e.tensor_tensor(cmp_[:], sv[:, :, 4].unsqueeze(1).to_broadcast([128, NSLOT, 8]),
                                               slot512[:, :].unsqueeze(2).to_broadcast([128, NSLOT, 8]), ALU.is_le),
              [sv[:, :, 4], slot512[:]], [cmp_[:]])
        S.add("dve", lambda e: e.tensor_reduce(esl[:], cmp_[:], AX.X, ALU.add), [cmp_[:]], [esl[:]])
        S.add("dve", lambda e: e.tensor_scalar(esl[:], esl[:], 7.0, None, ALU.min), [esl[:]], [esl[:]])
        S.add("dve", lambda e: e.scalar_tensor_tensor(idxf[:], esl[:, :].unsqueeze(2).to_broadcast([128, NSLOT, NG]), float(NG * 128),
                                                      cgp[:, :].unsqueeze(1).to_broadcast([128, NSLOT, NG]), ALU.mult, ALU.add),
              [esl[:], cgp[:]], [idxf[:]])
        S.add("dve", lambda e: e.tensor_copy(idxw[:], idxf[:]), [idxf[:]], [idxw[:]])
        S.add("dve", lambda e: e.tensor_tensor(posall[:], incl[:], tot[:], ALU.subtract), [incl[:], tot[:]], [posall[:]])
        S.add("dve", lambda e: e.tensor_tensor(posall[:], posall[:], within[:], ALU.add), [posall[:], within[:]], [posall[:]])
        S.add("dve", lambda e: e.tensor_tensor(posall[:], posall[:], sv[:, :, 5:6].to_broadcast([128, 8, NTL]), ALU.add),
              [posall[:], sv[:, :, 5:6]], [posall[:]])
        for (Ek, pk, pki) in ((E1, pos1, pos1i), (E2, pos2, pos2i)):
            S.add("dve", lambda e, Ek=Ek: e.tensor_tensor(ptmp[:], posall[:], Ek[:], ALU.mult), [posall[:], Ek[:]], [ptmp[:]])
            S.add("dve", lambda e, pk=pk: e.tensor_reduce(pk[:], ptmp[:, :, :].rearrange("p e t -> p t e"), AX.X, ALU.add), [ptmp[:]], [pk[:]])
            S.add("dve", lambda e, pk=pk, pki=pki: e.tensor_copy(pki[:], pk[:]), [pk[:]], [pki[:]])
        import os
        ESTOP = int(os.environ.get("E_STOP", "9"))
        if os.environ.get("DBG_E"):
            dd = lambda nm, shp, dt=F32: nc.dram_tensor("dbg_" + nm, shp, dt, kind="ExternalOutput").ap()
            S.dma("sp", dd("pos1i", [128, NTL], I32), pos1i[:])
            S.dma("sp", dd("pos2i", [128, NTL], I32), pos2i[:])
            S.dma("sp", dd("idxw", [128, NSLOT, NG], I32), idxw[:])
            S.dma("sp", dd("esl", [128, NSLOT]), esl[:])
            S.dma("sp", dd("sv", [128, 8, 6]), sv[:])
            S.dma("sp", dd("E1", [128, 8, NTL]), E1[:])
            S.dma("sp", dd("E2", [128, 8, NTL]), E2[:])
            S.dma("sp", dd("PW", [128, NTL, 2]), PW[:])
        for tt in range(NTL if ESTOP >= 2 else 0):
            hb = hin4[(tt // 4) % 2][:, tt % 4, :]
            S.dma("sp", hb, hin[tt * 128:(tt + 1) * 128, :])
            for pki in (pos1i, pos2i):
                S.dma("pool", xs, hb,
                      fn=lambda e, hb=hb, pki=pki, tt=tt: e.indirect_dma_start(
                          out=xs, out_offset=bass.IndirectOffsetOnAxis(ap=pki[:, tt:tt + 1], axis=0), in_=hb, in_offset=None),
                      reads=[hb, pki[:, tt:tt + 1]], writes=[xs])
        widx = 0
        cnt = 0
        def slot_prologue(s_):
            xb_ = xT[s_ % 2]
            for ti in range(4):
                hb = hin4[s_ % 2][:, ti, :]
                S.dma("sp", hb, xs[s_ * SLOT + ti * 128:s_ * SLOT + (ti + 1) * 128, :])
                emit_load_transpose(S, hb, ident, pg, xb_[:, :, ti * 128:(ti + 1) * 128])

        if ESTOP >= 3:
            slot_prologue(0)
        for s in range(NSLOT if ESTOP >= 3 else 0):
            xb = xT[s % 2]
            ac = acc[s % 2]
            for gi in range(NSG):
                if gi == 3 and s + 1 < NSLOT:
                    slot_prologue(s + 1)
                slot = widx % NWB
                widx += 1
                for sub in range(2):
                    ixa = idxw[:, s, 2 * gi + sub:2 * gi + sub + 1]
                    for (dst, srcw) in ((wgb[slot], wg_l), (wub[slot], wu_l), (wdb[slot], wd_l)):
                        dst2 = dst[:, sub].rearrange("p a b -> p (a b)")
                        S.dma("pool", dst2, srcw,
                              fn=lambda e, dst2=dst2, srcw=srcw, ixa=ixa: e.indirect_dma_start(
                                  out=dst2, out_offset=None, in_=srcw, in_offset=bass.IndirectOffsetOnAxis(ap=ixa, axis=0)),
                              reads=[srcw, ixa], writes=[dst2])
                ab = aT[cnt % 2]
                for fc in range(G):
                    sub, c = fc // 2, fc % 2
                    i2 = (cnt * G + fc) % 2
                    pgb, pub, sgb = pg[i2], pu[i2], sg[i2]
                    for kc in range(8):
                        S.add("pe", lambda e, pgb=pgb, kc=kc, sub=sub, c=c, slot=slot, xb=xb: e.matmul(
                            pgb[:, :], wgb[slot][:, sub, kc, c * 128:(c + 1) * 128], xb[:, kc, :], start=(kc == 0), stop=(kc == 7)),
                            [wgb[slot][:, sub, kc, c * 128:(c + 1) * 128], xb[:, kc, :]], [pgb[:, :]])
                    for kc in range(8):
                        S.add("pe", lambda e, pub=pub, kc=kc, sub=sub, c=c, slot=slot, xb=xb: e.matmul(
                            pub[:, :], wub[slot][:, sub, kc, c * 128:(c + 1) * 128], xb[:, kc, :], start=(kc == 0), stop=(kc == 7)),
                            [wub[slot][:, sub, kc, c * 128:(c + 1) * 128], xb[:, kc, :]], [pub[:, :]])
                    S.add("act", lambda e, sgb=sgb, pgb=pgb: e.activation(sgb[:], pgb[:, :], AF.Silu), [pgb[:, :]], [sgb[:]])
                    S.add("dve", lambda e, ab=ab, fc=fc, sgb=sgb, pub=pub: e.tensor_tensor(ab[:, fc, :], sgb[:], pub[:, :], ALU.mult),
                          [sgb[:], pub[:, :]], [ab[:, fc, :]])
                for ti in range(4):
                    pdb = pd[(cnt * 4 + ti) % 2]
                    for hd in range(2):
                        for fc in range(G):
                            S.add("pe", lambda e, pdb=pdb, hd=hd, fc=fc, ab=ab, ti=ti, slot=slot: e.matmul(
                                pdb[:, hd * 512:(hd + 1) * 512], ab[:, fc, ti * 128:(ti + 1) * 128],
                                wdb[slot][:, fc // 2, fc % 2, hd * 512:(hd + 1) * 512], start=(fc == 0), stop=(fc == G - 1)),
                                [ab[:, fc, ti * 128:(ti + 1) * 128], wdb[slot][:, fc // 2, fc % 2, hd * 512:(hd + 1) * 512]],
                                [pdb[:, hd * 512:(hd + 1) * 512]])
                    a_t = ac[:, ti, :]
                    if gi == 0:
                        S.add("act", lambda e, a_t=a_t, pdb=pdb: e.copy(a_t, pdb[:]), [pdb[:]], [a_t])
                    else:
                        S.add("dve", lambda e, a_t=a_t, pdb=pdb: e.tensor_tensor(a_t, pdb[:], a_t, ALU.add), [pdb[:], a_t], [a_t])
                cnt += 1
            S.dma("sp", ys[s * SLOT:(s + 1) * SLOT, :].rearrange("(t p) d -> p t d", p=128), ac[:])
        for g4 in range(0, NTL if ESTOP >= 4 else 0, 4):
            nn = min(4, NTL - g4)
            hq = hin4[(g4 // 4) % 2]
            for k in range(nn):
                tt = g4 + k
                S.dma("sp", hq[:, k, :], hin[tt * 128:(tt + 1) * 128, :])
                for (ab_, pki) in ((acc[0], pos1i), (acc[1], pos2i)):
                    yt = ab_[:, k, :]
                    S.dma("pool", yt, ys,
                          fn=lambda e, yt=yt, pki=pki, tt=tt: e.indirect_dma_start(
                              out=yt, out_offset=None, in_=ys, in_offset=bass.IndirectOffsetOnAxis(ap=pki[:, tt:tt + 1], axis=0)),
                          reads=[ys, pki[:, tt:tt + 1]], writes=[yt])
            for k in range(nn):
                S.add("act", lambda e, hq=hq, k=k: e.mul(hq[:, k, :], hq[:, k, :], ALPHA), [hq[:, k, :]], [hq[:, k, :]])
            for j, ab_ in enumerate((acc[0], acc[1])):
                for k in range(nn):
                    tt = g4 + k
                    S.add("dve", lambda e, hq=hq, k=k, ab_=ab_, tt=tt, j=j: e.scalar_tensor_tensor(
                        hq[:, k, :], ab_[:, k, :], PW[:, tt, j:j + 1], hq[:, k, :], ALU.mult, ALU.add),
                        [ab_[:, k, :], PW[:, tt, j:j + 1], hq[:, k, :]], [hq[:, k, :]])
            emit_ln_group(S, [(hq[:, k, :], acc[0][:, k, :]) for k in range(nn)], gb, bb, stg, mvg, scg)
            for k in range(nn):
                tt = g4 + k
                S.dma("sp", hout[tt * 128:(tt + 1) * 128, :], acc[0][:, k, :])
    S.barrier()


def ffn_stream_phase(S, nc, T, hin, hout, wg, wu, wd, F, lng, lnb, ident_d, tag):
    TB = min(512, T)
    NB = T // TB
    TPB = TB // 128
    GW = 256
    NG = F // GW
    assert NG * GW == F
    NWB = 3
    with contextlib.ExitStack() as es:
        cx = Ctx(nc, es, tag)
        ident = cx.sb("ident", [128, 128], F32)
        gb = cx.sb("gb", [128, 1024], F32)
        bb = cx.sb("bb", [128, 1024], F32)
        hin4 = [cx.sb("hin4_%d" % i, [128, TPB, 1024], F32) for i in range(2)]
        xT = [cx.sb("xT%d" % i, [128, 8, TB], BF16) for i in range(2)]
        acc = [cx.sb("acc%d" % i, [128, TPB, 1024], F32) for i in range(2)]
        wgb = [cx.sb("wg%d" % i, [128, 8, GW], BF16) for i in range(NWB)]
        wub = [cx.sb("wu%d" % i, [128, 8, GW], BF16) for i in range(NWB)]
        wdb = [cx.sb("wd%d" % i, [128, 2, 1024], BF16) for i in range(NWB)]
        sg = [cx.sb("sg%d" % i, [128, TB], F32) for i in range(2)]
        aT = [cx.sb("aT%d" % i, [128, 2, TB], BF16) for i in range(2)]
        stg = cx.sb("stg", [128, 4, 2, 6], F32)
        mvg = cx.sb("mvg", [128, 4, 2], F32)
        scg = cx.sb("scg", [128, 4, 2], F32)
        pg = [cx.ps("pg%d" % i, [128, 512], F32) for i in range(2)]
        pu = [cx.ps("pu%d" % i, [128, 512], F32) for i in range(2)]
        pd = [cx.ps("pd%d" % i, [128, 1024], F32) for i in range(2)]
        S.dma("sp", ident[:], ident_d)
        S.dma("sp", gb[:], lng.to_broadcast([128, 1024]))
        S.dma("sp", bb[:], lnb.to_broadcast([128, 1024]))

        def prologue(b_):
            for ti in range(TPB):
                hb = hin4[b_ % 2][:, ti, :]
                S.dma("sp", hb, hin[b_ * TB + ti * 128:b_ * TB + (ti + 1) * 128, :])
                emit_load_transpose(S, hb, ident, pg, xT[b_ % 2][:, :, ti * 128:(ti + 1) * 128])

        def epilogue(b_):
            hq, ac = hin4[b_ % 2], acc[b_ % 2]
            for ti in range(TPB):
                S.add("dve", lambda e, hq=hq, ac=ac, ti=ti: e.scalar_tensor_tensor(hq[:, ti, :], hq[:, ti, :], ALPHA, ac[:, ti, :], ALU.mult, ALU.add),
                      [hq[:, ti, :], ac[:, ti, :]], [hq[:, ti, :]])
            emit_ln_group(S, [(hq[:, ti, :], ac[:, ti, :]) for ti in range(TPB)], gb, bb, stg, mvg, scg)
            for ti in range(TPB):
                S.dma("sp", hout[b_ * TB + ti * 128:b_ * TB + (ti + 1) * 128, :], ac[:, ti, :])

        widx = 0
        cnt = 0
        pending = [None]
        prologue(0)
        for blk_ in range(NB):
            xb = xT[blk_ % 2]
            ac = acc[blk_ % 2]
            for gi in range(NG):
                if gi == NG // 2 and blk_ + 1 < NB:
                    prologue(blk_ + 1)
                if gi == 2 and blk_ > 0:
                    epilogue(blk_ - 1)
                slot = widx % NWB
                widx += 1
                f0 = gi * GW
                S.dma("pool", wgb[slot][:], wg[0, :, f0:f0 + GW].rearrange("(k p) f -> p k f", p=128))
                S.dma("pool", wub[slot][:], wu[0, :, f0:f0 + GW].rearrange("(k p) f -> p k f", p=128))
                S.dma("pool", wdb[slot][:], wd[0, f0:f0 + GW, :].rearrange("(c p) d -> p c d", p=128))
                ab = aT[cnt % 2]
                for fc in range(2):
                    i2 = (cnt * 2 + fc) % 2
                    pgb, pub, sgb = pg[i2], pu[i2], sg[i2]
                    for kc in range(8):
                        S.add("pe", lambda e, pgb=pgb, kc=kc, fc=fc, slot=slot, xb=xb: e.matmul(
                            pgb[:, 0:TB], wgb[slot][:, kc, fc * 128:(fc + 1) * 128], xb[:, kc, :], start=(kc == 0), stop=(kc == 7)),
                            [wgb[slot][:, kc, fc * 128:(fc + 1) * 128], xb[:, kc, :]], [pgb[:, 0:TB]])
                    for kc in range(8):
                        S.add("pe", lambda e, pub=pub, kc=kc, fc=fc, slot=slot, xb=xb: e.matmul(
                            pub[:, 0:TB], wub[slot][:, kc, fc * 128:(fc + 1) * 128], xb[:, kc, :], start=(kc == 0), stop=(kc == 7)),
                            [wub[slot][:, kc, fc * 128:(fc + 1) * 128], xb[:, kc, :]], [pub[:, 0:TB]])
                    S.add("act", lambda e, sgb=sgb, pgb=pgb: e.activation(sgb[:], pgb[:, 0:TB], AF.Silu), [pgb[:, 0:TB]], [sgb[:]])
                    S.add("dve", lambda e, ab=ab, fc=fc, sgb=sgb, pub=pub: e.tensor_tensor(ab[:, fc, :], sgb[:], pub[:, 0:TB], ALU.mult),
                          [sgb[:], pub[:, 0:TB]], [ab[:, fc, :]])

                def down_part(ab=ab, slot=slot, gi=gi, ac=ac, cnt=cnt):
                    for ti in range(TPB):
                        pdb = pd[(cnt * TPB + ti) % 2]
                        for hd in range(2):
                            for fc in range(2):
                                S.add("pe", lambda e, pdb=pdb, hd=hd, fc=fc, ab=ab, ti=ti, slot=slot: e.matmul(
                                    pdb[:, hd * 512:(hd + 1) * 512], ab[:, fc, ti * 128:(ti + 1) * 128],
                                    wdb[slot][:, fc, hd * 512:(hd + 1) * 512], start=(fc == 0), stop=(fc == 1)),
                                    [ab[:, fc, ti * 128:(ti + 1) * 128], wdb[slot][:, fc, hd * 512:(hd + 1) * 512]],
                                    [pdb[:, hd * 512:(hd + 1) * 512]])
                        a_t = ac[:, ti, :]
                        if gi == 0:
                            S.add("act", lambda e, a_t=a_t, pdb=pdb: e.copy(a_t, pdb[:]), [pdb[:]], [a_t])
                        else:
                            S.add("dve", lambda e, a_t=a_t, pdb=pdb: e.tensor_tensor(a_t, pdb[:], a_t, ALU.add), [pdb[:], a_t], [a_t])
                if pending[0] is not None:
                    pending[0]()
                pending[0] = down_part
                if gi == NG - 1:
                    pending[0]()
                    pending[0] = None
                cnt += 1
        epilogue(NB - 1)
    S.barrier()
```
